# Optimizing a Trainium2 kernel written in Bass

```python
import jax, jax.numpy as jnp
from jax import lax
import numpy as np

D_MODEL = 4096
BATCH = 4
SEQ = 4096
DEPTH = 1

CTX_LEN = 256
GRID_W = 64
HEAD_DIM = 128
N_Q_HEADS = 16
N_KV_HEADS = 4
Q_PER_KV = N_Q_HEADS // N_KV_HEADS
ATTN_WIDTH = N_Q_HEADS * HEAD_DIM
KV_WIDTH = N_KV_HEADS * HEAD_DIM
CONV_WIDTH = D_MODEL // 2
CONV_TAPS = 31
ROPE_THETA = 10000.0
ROPE_AXIS_DIM = HEAD_DIM // 2
Q_BLOCK = 128
N_GROUPS = 4
EXPERTS_PER_GROUP = 8
N_EXPERTS = N_GROUPS * EXPERTS_PER_GROUP
TOP_K = 2
EXPERT_FF = 1024
MOE_BLOCK = 128
N_MOD = 6
EPS = 1e-6

Q_OFF = 0
K_OFF = Q_OFF + ATTN_WIDTH
V_OFF = K_OFF + KV_WIDTH
GLU_OFF = V_OFF + KV_WIDTH
GATE_OFF = GLU_OFF + 2 * CONV_WIDTH
IN_WIDTH = GATE_OFF + 2 * D_MODEL

kernel_name = "hybrid_gqa_conformer_hmoe_dit"


def rmsnorm(x, g):
    xf = x.astype(jnp.float32)
    y = xf * lax.rsqrt(jnp.mean(xf * xf, axis=-1, keepdims=True) + EPS)
    return (y * g.astype(jnp.float32)).astype(x.dtype)


def modulate(x, shift, scale):
    return x * (1 + scale) + shift


def axial_rope_tables(n_tokens):
    rows = n_tokens // GRID_W
    row, col = jnp.meshgrid(jnp.arange(rows), jnp.arange(GRID_W), indexing="ij")
    pos = jnp.stack([row.reshape(-1), col.reshape(-1)], axis=-1).astype(jnp.float32)
    inv = ROPE_THETA ** (-jnp.arange(0, ROPE_AXIS_DIM, 2, dtype=jnp.float32) / ROPE_AXIS_DIM)
    ang = pos[:, :, None] * inv[None, None, :]
    return jnp.cos(ang), jnp.sin(ang)


def apply_rope(x, cos, sin):
    xf = x.astype(jnp.float32).reshape(*x.shape[:-1], 2, 2, ROPE_AXIS_DIM // 2)
    x1, x2 = xf[..., 0, :], xf[..., 1, :]
    c = cos[None, :, None]
    s = sin[None, :, None]
    out = jnp.stack([x1 * c - x2 * s, x2 * c + x1 * s], axis=-2)
    return out.reshape(x.shape).astype(x.dtype)


def attend(q, k, v):
    s = jnp.einsum("bqkgd,bskd->bkgqs", q, k, preferred_element_type=jnp.float32) * (HEAD_DIM ** -0.5)
    p = jax.nn.softmax(s, axis=-1).astype(v.dtype)
    return jnp.einsum("bkgqs,bskd->bqkgd", p, v)


def latent_attention(q, k_all, v_all):
    b, s = q.shape[:2]
    nb = s // Q_BLOCK
    qb = q.reshape(b, nb, Q_BLOCK, N_KV_HEADS, Q_PER_KV, HEAD_DIM).transpose(1, 0, 2, 3, 4, 5)
    o = lax.map(lambda qi: attend(qi, k_all, v_all), qb)
    return o.transpose(1, 0, 2, 3, 4, 5).reshape(b, s, ATTN_WIDTH)


def conformer_conv(glu, w_dw, b_dw, ln_g, ln_b):
    a, gt = jnp.split(glu, 2, axis=-1)
    u = a * jax.nn.sigmoid(gt)
    y = lax.conv_general_dilated(u, w_dw.astype(u.dtype), window_strides=(1,),
                                 padding=[(CONV_TAPS // 2, CONV_TAPS // 2)],
                                 dimension_numbers=("NWC", "WIO", "NWC"),
                                 feature_group_count=CONV_WIDTH)
    y = y.astype(jnp.float32) + b_dw.astype(jnp.float32)
    mu = jnp.mean(y, axis=-1, keepdims=True)
    var = jnp.mean(jnp.square(y - mu), axis=-1, keepdims=True)
    y = (y - mu) * lax.rsqrt(var + EPS) * ln_g.astype(jnp.float32) + ln_b.astype(jnp.float32)
    return jax.nn.silu(y).astype(glu.dtype)


def merge_branches(attn_flat, glu, gates, w_dw, b_dw, ln_g, ln_b, w_attn_out, w_conv_out, w_out):
    a = attn_flat @ w_attn_out
    cb = conformer_conv(glu, w_dw, b_dw, ln_g, ln_b) @ w_conv_out
    g_a, g_c = jnp.split(jax.nn.sigmoid(gates), 2, axis=-1)
    return (g_a * a + g_c * cb) @ w_out


def hier_moe(h, w_rg, b_rg, w_re, b_re, w_eg, w_eu, w_ed):
    n, d = h.shape
    hf = h.astype(jnp.float32)
    g_logits = hf @ w_rg.astype(jnp.float32) + b_rg.astype(jnp.float32)
    g_prob = jax.nn.softmax(g_logits, axis=-1)
    g_sel = jnp.argmax(g_logits, axis=-1)
    p_g = jnp.take_along_axis(g_prob, g_sel[:, None], axis=1)[:, 0]
    e_logits = (hf @ w_re.astype(jnp.float32) + b_re.astype(jnp.float32)).reshape(n, N_GROUPS, EXPERTS_PER_GROUP)
    e_in = jnp.take_along_axis(e_logits, g_sel[:, None, None], axis=1)[:, 0]
    top_v, top_i = lax.top_k(e_in, TOP_K)
    weights = p_g[:, None] * jax.nn.softmax(top_v, axis=-1)
    expert = g_sel[:, None].astype(jnp.int32) * EXPERTS_PER_GROUP + top_i.astype(jnp.int32)

    nk = n * TOP_K
    flat_e = expert.reshape(-1)
    flat_w = weights.reshape(-1)
    flat_t = jnp.arange(nk, dtype=jnp.int32) // TOP_K
    order = jnp.argsort(flat_e)
    se, st, sw = flat_e[order], flat_t[order], flat_w[order]
    counts = jnp.zeros((N_EXPERTS,), jnp.int32).at[flat_e].add(1)
    padded = ((counts + MOE_BLOCK - 1) // MOE_BLOCK) * MOE_BLOCK
    pend = jnp.cumsum(padded)
    pstart = pend - padded
    cstart = jnp.cumsum(counts) - counts
    dest = pstart[se] + jnp.arange(nk, dtype=jnp.int32) - cstart[se]
    nblk = (nk + N_EXPERTS * (MOE_BLOCK - 1) + MOE_BLOCK - 1) // MOE_BLOCK
    total = nblk * MOE_BLOCK
    buf_t = jnp.zeros((total,), jnp.int32).at[dest].set(st)
    buf_w = jnp.zeros((total,), jnp.float32).at[dest].set(sw)
    blk_e = jnp.minimum(jnp.searchsorted(pend, jnp.arange(nblk, dtype=jnp.int32) * MOE_BLOCK, side="right"),
                        N_EXPERTS - 1)

    def run(args):
        t, w, e = args
        xb = h[t]
        y = (jax.nn.silu(xb @ w_eg[e]) * (xb @ w_eu[e])) @ w_ed[e]
        return y * w[:, None].astype(y.dtype)

    ys = lax.map(run, (buf_t.reshape(nblk, MOE_BLOCK), buf_w.reshape(nblk, MOE_BLOCK), blk_e))
    return jnp.zeros_like(h).at[buf_t].add(ys.reshape(total, d))


def hybrid_layer(x, xc, c, c_ctx, cos, sin, norm1_g, w_mod, b_mod, w_in, q_norm_g, k_norm_g,
                 w_attn_out, conv_dw_w, conv_dw_b, conv_ln_g, conv_ln_b, w_conv_out, w_out,
                 norm2_g, w_rg, b_rg, w_re, b_re, w_eg, w_eu, w_ed, last):
    b, s, d = x.shape
    n_ctx = xc.shape[1]
    mod = (jax.nn.silu(c) @ w_mod + b_mod)[:, None, :]
    mod_c = (jax.nn.silu(c_ctx) @ w_mod + b_mod)[None, None, :]
    sh1, sc1, ga1, sh2, sc2, ga2 = jnp.split(mod, N_MOD, axis=-1)
    csh1, csc1, cga1, csh2, csc2, cga2 = jnp.split(mod_c, N_MOD, axis=-1)
    conv_args = (conv_dw_w, conv_dw_b, conv_ln_g, conv_ln_b, w_attn_out, w_conv_out, w_out)

    h = modulate(rmsnorm(x, norm1_g), sh1, sc1)
    hc = modulate(rmsnorm(xc, norm1_g), csh1, csc1)
    p = h @ w_in
    q = apply_rope(rmsnorm(p[..., Q_OFF:K_OFF].reshape(b, s, N_Q_HEADS, HEAD_DIM), q_norm_g), cos, sin)
    k = apply_rope(rmsnorm(p[..., K_OFF:V_OFF].reshape(b, s, N_KV_HEADS, HEAD_DIM), k_norm_g), cos, sin)
    v = p[..., V_OFF:GLU_OFF].reshape(b, s, N_KV_HEADS, HEAD_DIM)
    col0 = K_OFF if last else 0
    col1 = GLU_OFF if last else IN_WIDTH
    pc = hc @ w_in[:, col0:col1]
    kc = rmsnorm(pc[..., K_OFF - col0:V_OFF - col0].reshape(b, n_ctx, N_KV_HEADS, HEAD_DIM), k_norm_g)
    vc = pc[..., V_OFF - col0:GLU_OFF - col0].reshape(b, n_ctx, N_KV_HEADS, HEAD_DIM)
    k_all = jnp.concatenate([k, kc], axis=1)
    v_all = jnp.concatenate([v, vc], axis=1)
    attn = latent_attention(q.reshape(b, s, N_KV_HEADS, Q_PER_KV, HEAD_DIM), k_all, v_all)
    x = x + ga1 * merge_branches(attn, p[..., GLU_OFF:GATE_OFF], p[..., GATE_OFF:], *conv_args)
    if not last:
        qc = rmsnorm(pc[..., Q_OFF:K_OFF].reshape(b, n_ctx, N_KV_HEADS, Q_PER_KV, HEAD_DIM), q_norm_g)
        attn_c = attend(qc, kc, vc).reshape(b, n_ctx, ATTN_WIDTH)
        xc = xc + cga1 * merge_branches(attn_c, pc[..., GLU_OFF:GATE_OFF], pc[..., GATE_OFF:], *conv_args)

    h2 = modulate(rmsnorm(x, norm2_g), sh2, sc2).reshape(b * s, d)
    if last:
        y = hier_moe(h2, w_rg, b_rg, w_re, b_re, w_eg, w_eu, w_ed)
    else:
        h2c = modulate(rmsnorm(xc, norm2_g), csh2, csc2).reshape(b * n_ctx, d)
        y_all = hier_moe(jnp.concatenate([h2, h2c], axis=0), w_rg, b_rg, w_re, b_re, w_eg, w_eu, w_ed)
        y = y_all[:b * s]
        xc = xc + cga2 * y_all[b * s:].reshape(b, n_ctx, d)
    x = x + ga2 * y.reshape(b, s, d)
    return x, xc


def setup_inputs(seed: int = 0) -> dict:
    key = jax.random.key(seed)
    ks = jax.random.split(key, 32)
    f32 = jnp.float32
    L, D = DEPTH, D_MODEL

    def nrm(k, shape, scale):
        return jax.random.normal(k, shape, f32) * scale

    return {
        "x": nrm(ks[0], (BATCH, SEQ, D), 1.0),
        "c": nrm(ks[1], (BATCH, D), 1.0),
        "ctx": nrm(ks[2], (BATCH, CTX_LEN, D), 1.0),
        "c_ctx": nrm(ks[3], (D,), 1.0),
        "norm1_g": 1.0 + nrm(ks[4], (L, D), 0.02),
        "w_mod": nrm(ks[5], (L, D, N_MOD * D), 0.5 * D ** -0.5),
        "b_mod": nrm(ks[6], (L, N_MOD * D), 0.02),
        "w_in": nrm(ks[7], (L, D, IN_WIDTH), D ** -0.5),
        "q_norm_g": 1.0 + nrm(ks[8], (L, HEAD_DIM), 0.02),
        "k_norm_g": 1.0 + nrm(ks[9], (L, HEAD_DIM), 0.02),
        "w_attn_out": nrm(ks[10], (L, ATTN_WIDTH, D), ATTN_WIDTH ** -0.5),
        "conv_dw_w": nrm(ks[11], (L, CONV_TAPS, 1, CONV_WIDTH), CONV_TAPS ** -0.5),
        "conv_dw_b": nrm(ks[12], (L, CONV_WIDTH), 0.02),
        "conv_ln_g": 1.0 + nrm(ks[13], (L, CONV_WIDTH), 0.02),
        "conv_ln_b": nrm(ks[14], (L, CONV_WIDTH), 0.02),
        "w_conv_out": nrm(ks[15], (L, CONV_WIDTH, D), CONV_WIDTH ** -0.5),
        "w_out": nrm(ks[16], (L, D, D), D ** -0.5),
        "norm2_g": 1.0 + nrm(ks[17], (L, D), 0.02),
        "w_router_group": nrm(ks[18], (L, D, N_GROUPS), D ** -0.5),
        "b_router_group": nrm(ks[19], (L, N_GROUPS), 0.01),
        "w_router_expert": nrm(ks[20], (L, D, N_EXPERTS), D ** -0.5),
        "b_router_expert": nrm(ks[21], (L, N_EXPERTS), 0.01),
        "w_exp_gate": nrm(ks[22], (L, N_EXPERTS, D, EXPERT_FF), D ** -0.5),
        "w_exp_up": nrm(ks[23], (L, N_EXPERTS, D, EXPERT_FF), D ** -0.5),
        "w_exp_down": nrm(ks[24], (L, N_EXPERTS, EXPERT_FF, D), EXPERT_FF ** -0.5),
        "norm_f_g": 1.0 + nrm(ks[25], (D,), 0.02),
    }


def reference(x, c, ctx, c_ctx, norm1_g, w_mod, b_mod, w_in, q_norm_g, k_norm_g, w_attn_out,
              conv_dw_w, conv_dw_b, conv_ln_g, conv_ln_b, w_conv_out, w_out, norm2_g,
              w_router_group, b_router_group, w_router_expert, b_router_expert,
              w_exp_gate, w_exp_up, w_exp_down, norm_f_g):
    cos, sin = axial_rope_tables(x.shape[1])
    xc = ctx
    for l in range(DEPTH):
        x, xc = hybrid_layer(
            x, xc, c, c_ctx, cos, sin, norm1_g[l], w_mod[l], b_mod[l], w_in[l], q_norm_g[l], k_norm_g[l],
            w_attn_out[l], conv_dw_w[l], conv_dw_b[l], conv_ln_g[l], conv_ln_b[l], w_conv_out[l], w_out[l],
            norm2_g[l], w_router_group[l], b_router_group[l], w_router_expert[l], b_router_expert[l],
            w_exp_gate[l], w_exp_up[l], w_exp_down[l], last=(l == DEPTH - 1))
    return rmsnorm(x, norm_f_g)
```

```python
import contextlib
import numpy as np
import concourse.bass as bass
import concourse.mybir as mybir
from concourse.bass_utils import run_bass_kernel_spmd

F32 = mybir.dt.float32
BF16 = mybir.dt.bfloat16
I32 = mybir.dt.int32
ALU = mybir.AluOpType
AF = mybir.ActivationFunctionType
AX = mybir.AxisListType

D = 4096
KC = D // 128
SEQ = 4096
OWN = 2048
NCTX = 256
NKEY = SEQ + NCTX
NKT = NKEY // 128
HD = 128
NQH = 16
NKVH = 4
CW = 2048
TAPS = 31
Q_OFF, K_OFF, V_OFF, GLU_OFF, GATE_OFF, IN_W = 0, 2048, 2560, 3072, 7168, 15360
NE = 32
FF = 1024
CAP = 512
NST = CAP // 128
EPS = 1e-6
HALO = 16
UW = OWN + 2 * HALO

EPOCH = 12000
SAME_ENGINE_SYNC = True


class Sem:
    def __init__(self, fw, name, step):
        self.fw, self.name, self.step = fw, name, step
        self.count = 0
        self.handles = []

    def _handle(self, ep):
        while len(self.handles) <= ep:
            h = self.fw.stack.enter_context(self.fw.nc.semaphore(f"{self.name}_{len(self.handles)}"))
            self.handles.append(h)
        return self.handles[ep]

    def next(self, n=1):
        ep = self.count // EPOCH
        if (self.count + n - 1) // EPOCH != ep:
            self.count = (ep + 1) * EPOCH
            ep += 1
        self.count += n
        idx = self.count - ep * EPOCH
        return self._handle(ep), (self, ep, idx * self.step)

    def last_token(self):
        if self.count == 0:
            return None
        ep = (self.count - 1) // EPOCH
        return (self, ep, (self.count - ep * EPOCH) * self.step)


class Res:
    def __init__(self, name, persistent):
        self.name = name
        self.persistent = persistent
        self.last_write = None
        self.readers = []
        self.dma_sem = None


class FW:
    ENGS = ("pe", "act", "dve", "pool", "sp")

    def __init__(self, nc, stack):
        self.nc, self.stack = nc, stack
        self.ops = {e: [] for e in self.ENGS}
        self.esem = {e: Sem(self, f"s_{e}", 1) for e in ("pe", "act", "dve", "pool")}
        self.waited = {e: {} for e in self.ENGS}
        self.sem_pool = []
        self.all_dma_sems = []
        self.phase_res = []
        self.ph = None

    def begin_phase(self):
        self.ph = contextlib.ExitStack()
        self.phase_res = []

    def end_phase(self):
        self.barrier()
        self.emit()
        for r in self.phase_res:
            if r.dma_sem is not None:
                self.sem_pool.append(r.dma_sem)
                r.dma_sem = None
        self.phase_res = []
        self.ph.close()
        self.ph = None

    def res(self, name="r", persistent=False):
        r = Res(name, persistent)
        if not persistent:
            self.phase_res.append(r)
        return r

    def sb(self, name, shape, dtype, persistent=False, stack=None):
        st = stack if stack is not None else (self.stack if persistent else self.ph)
        self.nsb = getattr(self, "nsb", 0) + 1
        t = st.enter_context(self.nc.sbuf_tensor(f"sb{self.nsb}_{name}", list(shape), dtype))
        return t

    def buf(self, name, shape, dtype, persistent=False, stack=None):
        return self.sb(name, shape, dtype, persistent, stack), self.res(name, persistent or stack is not None)

    def ring(self, name, n, shape, dtype):
        return Ring([self.buf(f"{name}{i}", shape, dtype) for i in range(n)])

    def _waits_for(self, eng, toks):
        out = []
        for tok in toks:
            if tok is None:
                continue
            sem, ep, val = tok
            key = (id(sem), ep)
            if self.waited[eng].get(key, 0) >= val:
                continue
            self.waited[eng][key] = val
            out.append((sem.handles[ep], val))
        return out

    def op(self, eng, fn, reads=(), writes=(), dma=0, sem_res=None):
        toks = []
        for r in reads:
            toks.append(r.last_write)
        for w in writes:
            toks.append(w.last_write)
            toks.extend(w.readers)
        if dma:
            anchor = sem_res if sem_res is not None else writes[0]
            if anchor.dma_sem is None:
                if self.sem_pool:
                    anchor.dma_sem = self.sem_pool.pop()
                else:
                    anchor.dma_sem = Sem(self, f"d{len(self.all_dma_sems)}", 16)
                    self.all_dma_sems.append(anchor.dma_sem)
            handle, tok = anchor.dma_sem.next(dma)
            own = None
        else:
            handle, tok = self.esem[eng].next(1)
            own = self.esem[eng]
        if own is not None and not (SAME_ENGINE_SYNC and eng in ("act", "dve", "pool")):
            toks = [t for t in toks if t is None or t[0] is not own]
        waits = self._waits_for(eng, toks)
        self.ops[eng].append((waits, fn, handle, dma))
        for r in reads:
            r.readers.append(tok)
        for w in writes:
            w.last_write = tok
            w.readers = []
        return tok

    def bc_reg(self, e, val):
        if val not in self._regs:
            self._regs[val] = e.to_reg(val)
        return self._regs[val]

    def barrier(self):
        toks = [s.last_token() for s in self.esem.values()]
        toks += [s.last_token() for s in self.all_dma_sems]
        for eng in self.ENGS:
            waits = self._waits_for(eng, toks)
            self.ops[eng].append((waits, None, None, 0))

    def emit(self):
        nc = self.nc
        ops = self.ops
        self.ops = {e: [] for e in self.ENGS}
        with nc.Block() as block:
            def run(engname):
                def body(e):
                    self._regs = {}
                    for waits, fn, handle, dma in ops[engname]:
                        for h, v in waits:
                            e.wait_ge(h, v)
                        if fn is None:
                            continue
                        r = fn(e)
                        if dma:
                            assert isinstance(r, (list, tuple)) and len(r) == dma, (len(r), dma)
                            for ins in r:
                                ins.then_inc(handle, 16)
                        else:
                            r.then_inc(handle, 1)
                return body
            block.tensor(run("pe"))
            block.scalar(run("act"))
            block.vector(run("dve"))
            block.gpsimd(run("pool"))
            block.sync(run("sp"))


class Ring:
    def __init__(self, items):
        self.items = items
        self.i = 0

    def next(self):
        it = self.items[self.i % len(self.items)]
        self.i += 1
        return it


def mm_group(fw, out_ap, pairs, reads, writes):
    pairs = list(pairs)

    def fn(e):
        n = len(pairs)
        last = None
        for i, (l, r) in enumerate(pairs):
            last = e.matmul(out_ap, lhsT=l, rhs=r, start=(i == 0), stop=(i == n - 1))
        return last
    return fw.op("pe", fn, reads, writes)


def dma(fw, eng, out, in_, reads, writes, sem_res=None):
    return fw.op(eng, lambda e: [e.dma_start(out=out, in_=in_)], reads, writes, dma=1, sem_res=sem_res)


def store(fw, eng, out, in_, r_src):
    return fw.op(eng, lambda e: [e.dma_start(out=out, in_=in_)], [r_src], [], dma=1, sem_res=r_src)


def build_nc(debug=False, stop_after=None):
    nc = bass.Bass("TRN2", target_bir_lowering=False)

    def din(name, shape, dt=F32):
        return nc.dram_tensor(name, list(shape), dt, kind="ExternalInput").ap()

    def dscr(name, shape, dt=F32):
        return nc.dram_tensor(name, list(shape), dt, kind="Internal").ap()

    x_own = din("x_own", [OWN, D])
    x_oth = din("x_oth", [OWN, D])
    ctx_b = din("ctx_b", [NCTX, D])
    x_halo = din("x_halo", [2 * HALO, D])
    halo_mask = din("halo_mask", [128, 2 * HALO])
    cvec = din("cvec", [128, KC, 2])
    rope_c = din("rope_c", [128, NKEY])
    rope_s = din("rope_s", [128, NKEY])
    qk_g = din("qk_g", [128, 2])
    ident_in = din("ident", [128, 128])
    pmat_in = din("pmat", [128, 128])
    tril_in = din("tril", [128, 128])
    g1_in = din("g1", [128, KC])
    g2_in = din("g2", [128, KC])
    gf_rep_in = din("gf_rep", [128, D])
    bmod2 = din("bmod2", [2, 6 * D])
    w_mod = din("w_mod", [D, 6 * D])
    w_in = din("w_in", [D, IN_W])
    w_ao = din("w_attn_out", [NQH * HD, D])
    w_co = din("w_conv_out", [CW, D])
    w_out = din("w_out", [D, D])
    cw_in = din("conv_w", [128, 16, TAPS])
    cb_in = din("conv_b", [128, 16])
    lng_in = din("ln_g", [128, 16])
    lnb_in = din("ln_b", [128, 16])
    w_r = din("w_router", [D, 36])
    b_r_rep = din("b_router_rep", [128, 36])
    iota_cap = din("iota_cap", [128, NE])
    tok_ent = din("tok_ent", [128, 16, 2, 2], I32)
    tab_i_init = din("tab_i_init", [NE * CAP, 2], I32)
    tab_w_init = din("tab_w_init", [NE * CAP, 2])
    w_eg = din("w_exp_gate", [NE, D, FF])
    w_eu = din("w_exp_up", [NE, D, FF])
    w_ed = din("w_exp_down", [NE, FF, D])
    out = nc.dram_tensor("out", [OWN, D], F32, kind="ExternalOutput").ap()

    mod_all = dscr("mod_all", [2, 6 * D])
    qT_d = dscr("qT_d", [NQH, 128, OWN], BF16)
    uT_d = dscr("uT_d", [16, 128, UW])
    gates_d = dscr("gates_d", [64, 128, OWN])
    attnT_d = dscr("attnT_d", [16, 128, OWN], BF16)
    csT_d = dscr("csT_d", [16, 128, OWN], BF16)
    xnew_d = dscr("xnew_d", [OWN, D])
    tab_i = dscr("tab_i", [NE * CAP, 2], I32)
    tab_w = dscr("tab_w", [NE * CAP, 2])
    ybufs = [dscr(f"ybuf{i}", [2 * OWN, 1024]) for i in range(4)]

    w_mod_v = w_mod.rearrange("(kc p) n -> p kc n", p=128)
    w_in_v = w_in.rearrange("(kc p) n -> p kc n", p=128)
    w_ao_v = w_ao.rearrange("(kc p) n -> p kc n", p=128)
    w_co_v = w_co.rearrange("(kc p) n -> p kc n", p=128)
    w_out_v = w_out.rearrange("(kc p) n -> p kc n", p=128)
    w_r_v = w_r.rearrange("(kc p) n -> p kc n", p=128)

    dbg = {}

    with contextlib.ExitStack() as st:
        fw = FW(nc, st)
        r_mod_all = fw.res("mod_all", True)
        r_qT_d = fw.res("qT_d", True)
        r_uT_d = fw.res("uT_d", True)
        r_gates_d = fw.res("gates_d", True)
        r_attnT_d = fw.res("attnT_d", True)
        r_csT_d = fw.res("csT_d", True)
        r_xnew_d = [fw.res(f"xnew_d{g}", True) for g in range(4)]
        r_tab_i = fw.res("tab_i", True)
        r_tab_w = fw.res("tab_w", True)
        r_ybuf = fw.res("ybuf", True)
        r_out = fw.res("out", True)

        ident, r_ident = fw.buf("ident", [128, 128], F32, True)
        identb, r_identb = fw.buf("identb", [128, 128], BF16, True)
        pmat, r_pmat = fw.buf("pmat", [128, 128], F32, True)
        ones_f, r_ones_f = fw.buf("ones_f", [128, 128], F32, True)
        ones_b, r_ones_b = fw.buf("ones_b", [128, 128], BF16, True)
        epst, r_eps = fw.buf("epst", [128, 1], F32, True)
        qkg, r_qkg = fw.buf("qkg", [128, 2], F32, True)
        modv, r_modv = fw.buf("modv", [128, 6, KC], F32, True)
        A1, SH1, A1C, SH1C, A2, SH2 = range(6)

        banks = []
        for i in range(8):
            t = st.enter_context(nc.psum_tensor(f"bank{i}", [128, 512], F32))
            banks.append((t, fw.res(f"bank{i}", True)))

        def norm_transpose(rows, r_rows, nrows, mv_a, mv_s, dst_fn, r_dst, sc, pbanks, evac_engs=("act", "dve")):
            junk, r_junk, ss, r_ss, rstd, r_rstd, dg, r_dg, ss2, r_ss2 = sc
            for hh in range(2):
                fw.op("act", lambda e, hh=hh: e.activation(out=junk[:nrows, :], in_=rows[:nrows, hh * 2048:(hh + 1) * 2048],
                                                           func=AF.Square, accum_out=ss2[:nrows, hh:hh + 1]),
                      [r_rows], [r_junk, r_ss2])
            fw.op("dve", lambda e: e.tensor_tensor(out=ss[:nrows, :], in0=ss2[:nrows, 0:1], in1=ss2[:nrows, 1:2], op=ALU.add),
                  [r_ss2], [r_ss])
            fw.op("act", lambda e: e.activation(out=rstd[:nrows, :], in_=ss[:nrows, :], func=AF.Sqrt,
                                                scale=1.0 / D, bias=epst[:nrows, 0:1]), [r_ss, r_eps], [r_rstd])
            fw.op("dve", lambda e: e.reciprocal(out=rstd[:nrows, :], in_=rstd[:nrows, :]), [r_rstd], [r_rstd])
            fw.op("dve", lambda e: e.tensor_scalar(out=dg[:nrows, :nrows], in0=ident[:nrows, :nrows],
                                                   scalar1=rstd[:nrows, 0:1], scalar2=None, op0=ALU.mult),
                  [r_rstd, r_ident], [r_dg])
            for q in range(KC // 4):
                bk, r_bk = pbanks[q % len(pbanks)]

                def fn(e, q=q, bk=bk):
                    last = None
                    for j in range(4):
                        kc = q * 4 + j
                        last = e.matmul(bk[:, j * 128:j * 128 + nrows], lhsT=rows[:nrows, kc * 128:(kc + 1) * 128],
                                        rhs=dg[:nrows, :nrows], start=True, stop=True)
                    return last
                fw.op("pe", fn, [r_rows, r_dg], [r_bk])
                for j in range(4):
                    kc = q * 4 + j
                    eng = evac_engs[kc % len(evac_engs)]
                    if eng == "act":
                        fw.op("act", lambda e, kc=kc, j=j, bk=bk: e.activation(
                            out=dst_fn(kc), in_=bk[:, j * 128:j * 128 + nrows], func=AF.Identity,
                            scale=modv[:, mv_a, kc:kc + 1], bias=modv[:, mv_s, kc:kc + 1]),
                            [r_bk, r_modv], [r_dst])
                    else:
                        fw.op("dve", lambda e, kc=kc, j=j, bk=bk: e.tensor_scalar(
                            out=dst_fn(kc), in0=bk[:, j * 128:j * 128 + nrows],
                            scalar1=modv[:, mv_a, kc:kc + 1], scalar2=modv[:, mv_s, kc:kc + 1],
                            op0=ALU.mult, op1=ALU.add), [r_bk, r_modv], [r_dst])

        def nt_scratch():
            junk, r_junk = fw.buf("nt_junk", [128, D // 2], BF16)
            ss, r_ss = fw.buf("nt_ss", [128, 1], F32)
            ss2, r_ss2 = fw.buf("nt_ss2", [128, 2], F32)
            rstd, r_rstd = fw.buf("nt_rstd", [128, 1], F32)
            dg, r_dg = fw.buf("nt_dg", [128, 128], F32)
            return (junk, r_junk, ss, r_ss, rstd, r_rstd, dg, r_dg, ss2, r_ss2)

        fw.begin_phase()
        dma(fw, "sp", ident[:], ident_in, [], [r_ident])
        dma(fw, "sp", pmat[:], pmat_in, [], [r_pmat])
        dma(fw, "sp", qkg[:], qk_g, [], [r_qkg])
        fw.op("dve", lambda e: e.memset(ones_f[:], 1.0), [], [r_ones_f])
        fw.op("dve", lambda e: e.memset(ones_b[:], 1.0), [], [r_ones_b])
        fw.op("dve", lambda e: e.memset(epst[:], EPS), [], [r_eps])
        fw.op("dve", lambda e: e.tensor_copy(out=identb[:], in_=ident[:]), [r_ident], [r_identb])
        dma(fw, "sp", tab_i, tab_i_init, [], [r_tab_i])
        dma(fw, "sp", tab_w, tab_w_init, [], [r_tab_w])

        cv, r_cv = fw.buf("cv", [128, KC, 2], F32)
        sg, r_sg = fw.buf("sgc", [128, KC, 2], F32)
        dma(fw, "sp", cv[:], cvec, [], [r_cv])
        fw.op("act", lambda e: e.activation(out=sg[:], in_=cv[:], func=AF.Sigmoid), [r_cv], [r_sg])
        fw.op("dve", lambda e: e.tensor_tensor(out=sg[:], in0=sg[:], in1=cv[:], op=ALU.mult), [r_sg, r_cv], [r_sg])
        bmr = fw.ring("bm", 2, [2, 512], F32)
        mrr = fw.ring("modrow", 2, [2, 512], F32)
        wring = fw.ring("wm", 3, [128, 8, 512], F32)
        NCH = 6 * D // 512
        for ch in range(NCH):
            bk, r_bk = banks[ch % 2]
            tiles = []
            for k4 in range(4):
                wt, r_wt = wring.next()
                dma(fw, "sp", wt[:], w_mod_v[:, k4 * 8:(k4 + 1) * 8, ch * 512:(ch + 1) * 512], [], [r_wt])
                tiles.append((wt, r_wt))

                def fn(e, k4=k4, wt=wt, bk=bk):
                    last = None
                    for j in range(8):
                        kc = k4 * 8 + j
                        last = e.matmul(bk[0:2, :], lhsT=sg[:, kc, :], rhs=wt[:, j, :],
                                        start=(kc == 0), stop=(kc == KC - 1))
                    return last
                fw.op("pe", fn, [r_sg, r_wt], [r_bk])
            bm, r_bm = bmr.next()
            dma(fw, "sp", bm[:], bmod2[:, ch * 512:(ch + 1) * 512], [], [r_bm])
            mr, r_mr = mrr.next()
            fw.op("dve", lambda e, bk=bk, bm=bm, mr=mr: e.tensor_tensor(out=mr[:], in0=bk[0:2, :], in1=bm[:], op=ALU.add),
                  [r_bk, r_bm], [r_mr])
            store(fw, "sp", mod_all[:, ch * 512:(ch + 1) * 512], mr[:], r_mr)
        fw.barrier()
        mraw, r_mraw = fw.buf("mraw", [128, 6, KC], F32)
        g12, r_g12 = fw.buf("g12", [128, 2, KC], F32)
        dma(fw, "sp", g12[:, 0, :], g1_in, [], [r_g12])
        dma(fw, "sp", g12[:, 1, :], g2_in, [r_g12], [r_g12])

        def ld_mod(slot, row, j):
            src = mod_all[row, j * D:(j + 1) * D].rearrange("(kc p) -> p kc", p=128)
            fw.op("sp", lambda e: [e.dma_start(out=mraw[:, slot, :], in_=src, allow_slow_non_contiguous=True)],
                  [r_mraw], [r_mraw], dma=1)
        ld_mod(0, 0, 1)
        ld_mod(1, 0, 0)
        ld_mod(2, 1, 1)
        ld_mod(3, 1, 0)
        ld_mod(4, 0, 4)
        ld_mod(5, 0, 3)
        for (dst_a, dst_s, s_sc, s_sh, gi) in ((A1, SH1, 0, 1, 0), (A1C, SH1C, 2, 3, 0), (A2, SH2, 4, 5, 1)):
            fw.op("dve", lambda e, dst_a=dst_a, s_sc=s_sc, gi=gi: e.scalar_tensor_tensor(
                out=modv[:, dst_a, :], in0=mraw[:, s_sc, :], scalar=1.0, in1=g12[:, gi, :],
                op0=ALU.add, op1=ALU.mult), [r_mraw, r_g12], [r_modv])
            fw.op("dve", lambda e, dst_s=dst_s, s_sh=s_sh: e.tensor_copy(out=modv[:, dst_s, :], in_=mraw[:, s_sh, :]),
                  [r_mraw], [r_modv])
        if debug:
            dbg["modv"] = _dump(nc, fw, "dbg_modv", modv, r_modv, [128, 6, KC], F32)
        fw.end_phase()
        if stop_after == 0:
            return _finish(nc, fw, dbg, out, r_out)

        kv_stack = contextlib.ExitStack()
        KT, r_KT = fw.buf("KT", [128, NKVH, NKEY], BF16, stack=kv_stack)
        VA, r_VA = fw.buf("VA", [128, NKT, NKVH, HD + 2], BF16, stack=kv_stack)
        fw.begin_phase()
        fw.op("pool", lambda e: e.memset(VA[:, :, :, HD:HD + 2], 1.0), [], [r_VA])
        sc = nt_scratch()
        xring = fw.ring("xrow", 1, [128, D], F32)
        hT, r_hT = fw.buf("hT", [128, KC, 512], BF16)
        wring = fw.ring("wb", 2, [128, KC, 256], BF16)
        rc, r_rc = fw.buf("ropec", [128, 512], F32)
        rs, r_rs = fw.buf("ropes", [128, 512], F32)
        qg, r_qg = fw.buf("qg", [128, 512], F32)
        sq, r_sq = fw.buf("sq", [128, 512], BF16)
        rr, r_rr = fw.buf("rr", [128, 512], F32)
        t1, r_t1 = fw.buf("t1", [128, 512], F32)
        t2, r_t2 = fw.buf("t2", [128, 512], F32)
        qst = fw.ring("qst", 2, [128, 512], BF16)
        ust = fw.ring("ust", 2, [128, 512], F32)
        gst = fw.ring("gst", 2, [128, 512], F32)
        sgt, r_sgt = fw.buf("sgt", [128, 512], F32)
        hm, r_hm = fw.buf("hm", [128, 2 * HALO], F32)
        dma(fw, "sp", hm[:], halo_mask, [], [r_hm])

        groups = []
        for g in range(4):
            groups.append(dict(src=x_own[g * 512:(g + 1) * 512, :], n=512, a=A1, s=SH1, key0=g * 512,
                               full=True, own0=g * 512))
        for g in range(4):
            groups.append(dict(src=x_oth[g * 512:(g + 1) * 512, :], n=512, a=A1, s=SH1, key0=OWN + g * 512,
                               full=False))
        groups.append(dict(src=ctx_b, n=NCTX, a=A1C, s=SH1C, key0=SEQ, full=False))
        groups.append(dict(src=x_halo, n=2 * HALO, a=A1, s=SH1, key0=None, full=False, halo=True))

        def load_w(c0):
            wt, r_wt = wring.next()
            dma(fw, "pool", wt[:], w_in_v[:, :, c0:c0 + 256], [], [r_wt])
            return wt, r_wt

        def proj_fm(wt, r_wt, sub, n, bk, r_bk):
            mm_group(fw, bk[:, 0:n], [(wt[:, kc, sub * 128:(sub + 1) * 128], hT[:, kc, 0:n]) for kc in range(KC)],
                     [r_wt, r_hT], [r_bk])

        bank_i = [0]

        def next_bank(lo=0, hi=4):
            b = banks[lo + bank_i[0] % (hi - lo)]
            bank_i[0] += 1
            return b

        def do_group(G):
                n = G["n"]
                ntile = (n + 127) // 128
                for tt in range(ntile):
                    nr = min(128, n - tt * 128)
                    xr, r_xr = xring.next()
                    dma(fw, "sp", xr[:nr, :], G["src"][tt * 128:tt * 128 + nr, :], [], [r_xr])
                    norm_transpose(xr, r_xr, nr, G["a"], G["s"],
                                   lambda kc, tt=tt, nr=nr: hT[:, kc, tt * 128:tt * 128 + nr], r_hT, sc, banks[4:8])
                halo = G.get("halo", False)
                if not halo:
                    key0 = G["key0"]
                    dma(fw, "sp", rc[:, 0:n], rope_c[:, key0:key0 + n], [], [r_rc])
                    dma(fw, "sp", rs[:, 0:n], rope_s[:, key0:key0 + n], [], [r_rs])
                    heads = []
                    if G["full"]:
                        heads += [("q", h) for h in range(NQH)]
                    heads += [("k", h) for h in range(NKVH)]
                    for hi in range(0, len(heads), 2):
                        kind, h0 = heads[hi]
                        c0 = (Q_OFF if kind == "q" else K_OFF) + h0 * HD
                        wt, r_wt = load_w(c0)
                        for sub in range(2):
                            kind, h = heads[hi + sub]
                            gcol = 0 if kind == "q" else 1
                            bk, r_bk = next_bank()
                            proj_fm(wt, r_wt, sub, n, bk, r_bk)
                            fw.op("act", lambda e, bk=bk, gcol=gcol: e.activation(
                                out=qg[:, 0:n], in_=bk[:, 0:n], func=AF.Identity, scale=qkg[:, gcol:gcol + 1]),
                                [r_bk, r_qkg], [r_qg])
                            fw.op("act", lambda e, bk=bk: e.activation(out=sq[:, 0:n], in_=bk[:, 0:n], func=AF.Square),
                                  [r_bk], [r_sq])
                            b2, r_b2 = next_bank()
                            mm_group(fw, b2[:, 0:n], [(ones_b[:], sq[:, 0:n])], [r_ones_b, r_sq], [r_b2])
                            b3, r_b3 = next_bank()
                            mm_group(fw, b3[:, 0:n], [(pmat[:], qg[:, 0:n])], [r_pmat, r_qg], [r_b3])
                            fw.op("act", lambda e, b2=b2: e.activation(out=rr[:, 0:n], in_=b2[:, 0:n], func=AF.Sqrt,
                                                                       scale=1.0 / HD, bias=epst[:, 0:1]),
                                  [r_b2, r_eps], [r_rr])
                            fw.op("dve", lambda e: e.reciprocal(out=rr[:, 0:n], in_=rr[:, 0:n]), [r_rr], [r_rr])
                            fw.op("dve", lambda e: e.tensor_tensor(out=t1[:, 0:n], in0=qg[:, 0:n], in1=rc[:, 0:n], op=ALU.mult),
                                  [r_qg, r_rc], [r_t1])
                            fw.op("dve", lambda e, b3=b3: e.tensor_tensor(out=t2[:, 0:n], in0=b3[:, 0:n], in1=rs[:, 0:n],
                                                                          op=ALU.mult), [r_b3, r_rs], [r_t2])
                            fw.op("dve", lambda e: e.tensor_tensor(out=t1[:, 0:n], in0=t1[:, 0:n], in1=t2[:, 0:n], op=ALU.add),
                                  [r_t1, r_t2], [r_t1])
                            if kind == "k":
                                fw.op("dve", lambda e, h=h, key0=key0: e.tensor_tensor(
                                    out=KT[:, h, key0:key0 + n], in0=t1[:, 0:n], in1=rr[:, 0:n], op=ALU.mult),
                                    [r_t1, r_rr], [r_KT])
                            else:
                                qs, r_qs = qst.next()
                                fw.op("dve", lambda e, qs=qs: e.tensor_tensor(out=qs[:, 0:n], in0=t1[:, 0:n], in1=rr[:, 0:n],
                                                                               op=ALU.mult), [r_t1, r_rr], [r_qs])
                                o0 = G["own0"]
                                store(fw, "pool", qT_d[h, :, o0:o0 + n], qs[:, 0:n], r_qs)
                    for vb in range(2):
                        wt, r_wt = load_w(V_OFF + vb * 256)
                        for tt in range(ntile):
                            bk, r_bk = next_bank()
                            mm_group(fw, bk[:, 0:256], [(hT[:, kc, tt * 128:(tt + 1) * 128], wt[:, kc, :]) for kc in range(KC)],
                                     [r_wt, r_hT], [r_bk])
                            kt = (key0 // 128) + tt
                            eng = "act" if tt % 2 == 0 else "dve"
                            src = bk[:, 0:256].rearrange("p (h d) -> p h d", h=2)
                            if eng == "act":
                                fw.op("act", lambda e, kt=kt, vb=vb, src=src: e.activation(
                                    out=VA[:, kt, vb * 2:vb * 2 + 2, 0:HD], in_=src, func=AF.Identity), [r_bk], [r_VA])
                            else:
                                fw.op("dve", lambda e, kt=kt, vb=vb, src=src: e.tensor_copy(
                                    out=VA[:, kt, vb * 2:vb * 2 + 2, 0:HD], in_=src), [r_bk], [r_VA])
                if G["full"] or halo:
                    if halo:
                        ucol0 = None
                    else:
                        ucol0 = HALO + G["own0"]
                    for cb in range(8):
                        wa, r_wa = load_w(GLU_OFF + cb * 256)
                        wg, r_wg = load_w(GLU_OFF + CW + cb * 256)
                        for sub in range(2):
                            cc = cb * 2 + sub
                            ba, r_ba = next_bank()
                            proj_fm(wa, r_wa, sub, n, ba, r_ba)
                            bg, r_bg = next_bank()
                            proj_fm(wg, r_wg, sub, n, bg, r_bg)
                            fw.op("act", lambda e, bg=bg: e.activation(out=sgt[:, 0:n], in_=bg[:, 0:n], func=AF.Sigmoid),
                                  [r_bg], [r_sgt])
                            us, r_us = ust.next()
                            fw.op("dve", lambda e, ba=ba, us=us: e.tensor_tensor(out=us[:, 0:n], in0=ba[:, 0:n], in1=sgt[:, 0:n],
                                                                               op=ALU.mult), [r_ba, r_sgt], [r_us])
                            if halo:
                                fw.op("dve", lambda e, us=us: e.tensor_tensor(out=us[:, 0:n], in0=us[:, 0:n], in1=hm[:, 0:n],
                                                                               op=ALU.mult), [r_us, r_hm], [r_us])
                                fw.op("pool", lambda e, us=us, cc=cc: [
                                    e.dma_start(out=uT_d[cc, :, 0:HALO], in_=us[:, 0:HALO]),
                                    e.dma_start(out=uT_d[cc, :, HALO + OWN:UW], in_=us[:, HALO:2 * HALO])],
                                    [r_us], [], dma=2, sem_res=r_us)
                            else:
                                store(fw, "pool", uT_d[cc, :, ucol0:ucol0 + n], us[:, 0:n], r_us)
                if G["full"]:
                    o0 = G["own0"]
                    for gb in range(32):
                        wt, r_wt = load_w(GATE_OFF + gb * 256)
                        for sub in range(2):
                            ch = gb * 2 + sub
                            bk, r_bk = next_bank()
                            proj_fm(wt, r_wt, sub, n, bk, r_bk)
                            gs, r_gs = gst.next()
                            fw.op("act", lambda e, bk=bk, gs=gs: e.activation(out=gs[:, 0:n], in_=bk[:, 0:n], func=AF.Sigmoid),
                                  [r_bk], [r_gs])
                            store(fw, "pool", gates_d[ch, :, o0:o0 + n], gs[:, 0:n], r_gs)

        for G in groups:
            do_group(G)
        if debug:
            dbg["KT"] = _dump(nc, fw, "dbg_KT", KT, r_KT, [128, NKVH, NKEY], BF16)
            dbg["VA"] = _dump(nc, fw, "dbg_VA", VA, r_VA, [128, NKT, NKVH, HD + 2], BF16)
        fw.end_phase()
        if stop_after == 1:
            kv_stack.close()
            return _finish(nc, fw, dbg, out, r_out)

        fw.begin_phase()
        qT, r_qT = fw.buf("qT", [128, NQH, 512], BF16)
        pring = fw.ring("pexp", 3, [128, 512], BF16)
        atm = fw.ring("atm", 2, [128, 128], BF16)
        rden = fw.ring("rden", 2, [128, 1], F32)
        aT, r_aT = fw.buf("aT", [128, NQH, 512], BF16)
        uin = fw.ring("uin", 2, [128, 512 + 2 * HALO], F32)
        yc, r_yc = fw.buf("yc", [128, 16, 512], F32)
        ysq, r_ysq = fw.buf("ysq", [128, 512], F32)
        mean, r_mean = fw.buf("mean", [128, 512], F32)
        var, r_var = fw.buf("var", [128, 512], F32)
        zt, r_zt = fw.buf("zt", [128, 512], F32)
        cst = fw.ring("cst", 2, [128, 512], BF16)
        cw, r_cw = fw.buf("cw", [128, 16, TAPS], F32)
        cbv, r_cbv = fw.buf("cbv", [128, 16], F32)
        lng, r_lng = fw.buf("lng", [128, 16], F32)
        lnb, r_lnb = fw.buf("lnb", [128, 16], F32)
        dma(fw, "sp", cw[:], cw_in, [], [r_cw])
        dma(fw, "sp", cbv[:], cb_in, [], [r_cbv])
        dma(fw, "sp", lng[:], lng_in, [], [r_lng])
        dma(fw, "sp", lnb[:], lnb_in, [], [r_lnb])
        SCALE = float(HD) ** -0.5
        s_banks = [banks[0], banks[1]]
        o_banks = [(banks[2], banks[3]), (banks[4], banks[5])]
        tr_bank = banks[6]
        st_bank = banks[7]

        for g in range(4):
            o0 = g * 512
            dma(fw, "sp", qT[:], qT_d[:, :, o0:o0 + 512].rearrange("h p t -> p h t"), [], [r_qT])
            for h in range(NQH):
                kvh = h // (NQH // NKVH)
                ob = o_banks[h % 2]
                pend = None
                for kc in range(NKT + 1):
                    if kc < NKT:
                        sb_, r_sb = s_banks[kc % 2]
                        mm_group(fw, sb_[:, :], [(KT[:, kvh, kc * 128:(kc + 1) * 128], qT[:, h, :])], [r_KT, r_qT], [r_sb])
                        pb, r_pb = pring.next()
                        fw.op("act", lambda e, sb_=sb_, pb=pb: e.activation(out=pb[:], in_=sb_[:, :], func=AF.Exp,
                                                                             scale=SCALE), [r_sb], [r_pb])
                        cur = (kc, pb, r_pb)
                    else:
                        cur = None
                    if pend is not None:
                        pkc, ppb, r_ppb = pend

                        def fn(e, pkc=pkc, ppb=ppb, kvh=kvh, ob=ob):
                            last = None
                            for sub in range(4):
                                bk = ob[sub // 2][0]
                                c0 = (sub % 2) * 256
                                last = e.matmul(bk[:, c0:c0 + HD + 1], lhsT=ppb[:, sub * 128:(sub + 1) * 128],
                                                rhs=VA[:, pkc, kvh, 0:HD + 1], start=(pkc == 0), stop=(pkc == NKT - 1))
                            return last
                        fw.op("pe", fn, [r_ppb, r_VA], [ob[0][1], ob[1][1]])
                    pend = cur
                for sub in range(4):
                    bk, r_bk = ob[sub // 2]
                    c0 = (sub % 2) * 256
                    rd, r_rd = rden.next()
                    fw.op("dve", lambda e, bk=bk, c0=c0, rd=rd: e.reciprocal(out=rd[:], in_=bk[:, c0 + HD:c0 + HD + 1]),
                          [r_bk], [r_rd])
                    am, r_am = atm.next()
                    fw.op("dve", lambda e, bk=bk, c0=c0, rd=rd, am=am: e.tensor_scalar(
                        out=am[:], in0=bk[:, c0:c0 + HD], scalar1=rd[:, 0:1], scalar2=None, op0=ALU.mult),
                        [r_bk, r_rd], [r_am])
                    tb, r_tb = tr_bank
                    tbv = tb[:].bitcast(BF16)
                    fw.op("pe", lambda e, am=am, tbv=tbv: e.transpose(tbv[:, 0:128], am[:], identb[:]),
                          [r_am, r_identb], [r_tb])
                    fw.op("act", lambda e, tbv=tbv, h=h, sub=sub: e.activation(
                        out=aT[:, h, sub * 128:(sub + 1) * 128], in_=tbv[:, 0:128], func=AF.Identity), [r_tb], [r_aT])
            store(fw, "pool", attnT_d[:, :, o0:o0 + 512].rearrange("h p t -> p h t"), aT[:], r_aT)

            sbk, r_sbk = st_bank
            for cc in range(16):
                ui, r_ui = uin.next()
                dma(fw, "sp", ui[:], uT_d[cc, :, o0:o0 + 512 + 2 * HALO], [], [r_ui])
                fw.op("dve", lambda e, ui=ui, cc=cc: e.tensor_scalar(
                    out=yc[:, cc, :], in0=ui[:, 1:513], scalar1=cw[:, cc, 0:1], scalar2=cbv[:, cc:cc + 1],
                    op0=ALU.mult, op1=ALU.add), [r_ui, r_cw, r_cbv], [r_yc])
                for k in range(1, TAPS):
                    fw.op("dve", lambda e, ui=ui, cc=cc, k=k: e.scalar_tensor_tensor(
                        out=yc[:, cc, :], in0=ui[:, k + 1:k + 513], scalar=cw[:, cc, k:k + 1], in1=yc[:, cc, :],
                        op0=ALU.mult, op1=ALU.add), [r_ui, r_cw, r_yc], [r_yc])
            mm_group(fw, sbk[:, :], [(ones_f[:], yc[:, cc, :]) for cc in range(16)], [r_ones_f, r_yc], [r_sbk])
            fw.op("act", lambda e, sbk=sbk: e.activation(out=mean[:], in_=sbk[:, :], func=AF.Identity, scale=1.0 / CW),
                  [r_sbk], [r_mean])
            for cc in range(16):
                fw.op("dve", lambda e, cc=cc: e.tensor_tensor(out=yc[:, cc, :], in0=yc[:, cc, :], in1=mean[:], op=ALU.subtract),
                      [r_yc, r_mean], [r_yc])
            for cc in range(16):
                fw.op("act", lambda e, cc=cc: e.activation(out=ysq[:], in_=yc[:, cc, :], func=AF.Square), [r_yc], [r_ysq])
                fw.op("pe", lambda e, cc=cc, sbk=sbk: e.matmul(sbk[:, :], lhsT=ones_f[:], rhs=ysq[:], start=(cc == 0),
                                                            stop=(cc == 15)), [r_ones_f, r_ysq], [r_sbk])
            fw.op("act", lambda e, sbk=sbk: e.activation(out=var[:], in_=sbk[:, :], func=AF.Sqrt, scale=1.0 / CW,
                                                         bias=epst[:, 0:1]), [r_sbk, r_eps], [r_var])
            fw.op("dve", lambda e: e.reciprocal(out=var[:], in_=var[:]), [r_var], [r_var])
            for cc in range(16):
                fw.op("dve", lambda e, cc=cc: e.tensor_tensor(out=zt[:], in0=yc[:, cc, :], in1=var[:], op=ALU.mult),
                      [r_yc, r_var], [r_zt])
                cs, r_cs = cst.next()
                fw.op("act", lambda e, cc=cc, cs=cs: e.activation(out=cs[:], in_=zt[:], func=AF.Silu,
                                                                 scale=lng[:, cc:cc + 1], bias=lnb[:, cc:cc + 1]),
                      [r_zt, r_lng, r_lnb], [r_cs])
                store(fw, "pool", csT_d[cc, :, o0:o0 + 512], cs[:], r_cs)
        fw.end_phase()
        kv_stack.close()
        if stop_after == 2:
            return _finish(nc, fw, dbg, out, r_out)

        fw.begin_phase()
        aT, r_aT = fw.buf("aT3", [128, 16, 512], BF16)
        cT, r_cT = fw.buf("cT3", [128, 16, 512], BF16)
        mT, r_mT = fw.buf("mT", [128, KC, 512], BF16)
        wring = fw.ring("wb3", 3, [128, 8192], BF16)
        gin = fw.ring("gin", 4, [128, 512], F32)
        tm1, r_tm1 = fw.buf("tm1", [128, 512], F32)
        tm2, r_tm2 = fw.buf("tm2", [128, 512], F32)
        xin = fw.ring("xin", 2, [128, 4, 256], F32)
        xo = fw.ring("xo", 2, [128, 4, 256], F32)
        gar = fw.ring("gar", 2, [128, 256], F32)
        for g in range(4):
            o0 = g * 512
            dma(fw, "sp", aT[:], attnT_d[:, :, o0:o0 + 512].rearrange("h p t -> p h t"), [], [r_aT])
            dma(fw, "sp", cT[:], csT_d[:, :, o0:o0 + 512].rearrange("h p t -> p h t"), [], [r_cT])
            for ob4 in range(8):
                wa, r_wa = wring.next()
                wav = wa[:].rearrange("p (k n) -> p k n", k=16)
                dma(fw, "pool", wav, w_ao_v[:, :, ob4 * 512:(ob4 + 1) * 512], [], [r_wa])
                wc, r_wc = wring.next()
                wcv = wc[:].rearrange("p (k n) -> p k n", k=16)
                dma(fw, "pool", wcv, w_co_v[:, :, ob4 * 512:(ob4 + 1) * 512], [], [r_wc])
                for sub in range(4):
                    oc = ob4 * 4 + sub
                    ba, r_ba = banks[(oc * 2) % 4]
                    bc, r_bc = banks[(oc * 2 + 1) % 4]
                    mm_group(fw, ba[:, :], [(wav[:, kc, sub * 128:(sub + 1) * 128], aT[:, kc, :]) for kc in range(16)],
                             [r_wa, r_aT], [r_ba])
                    mm_group(fw, bc[:, :], [(wcv[:, kc, sub * 128:(sub + 1) * 128], cT[:, kc, :]) for kc in range(16)],
                             [r_wc, r_cT], [r_bc])
                    ga_, r_ga = gin.next()
                    gc_, r_gc = gin.next()
                    dma(fw, "sp", ga_[:], gates_d[oc, :, o0:o0 + 512], [], [r_ga])
                    dma(fw, "sp", gc_[:], gates_d[32 + oc, :, o0:o0 + 512], [], [r_gc])
                    fw.op("dve", lambda e, ba=ba, ga_=ga_: e.tensor_tensor(out=tm1[:], in0=ba[:, :], in1=ga_[:], op=ALU.mult),
                          [r_ba, r_ga], [r_tm1])
                    fw.op("dve", lambda e, bc=bc, gc_=gc_: e.tensor_tensor(out=tm2[:], in0=bc[:, :], in1=gc_[:], op=ALU.mult),
                          [r_bc, r_gc], [r_tm2])
                    fw.op("dve", lambda e, oc=oc: e.tensor_tensor(out=mT[:, oc, :], in0=tm1[:], in1=tm2[:], op=ALU.add),
                          [r_tm1, r_tm2], [r_mT])
            for nb in range(16):
                wo, r_wo = wring.next()
                wov = wo[:].rearrange("p (k n) -> p k n", k=KC)
                dma(fw, "pool", wov, w_out_v[:, :, nb * 256:(nb + 1) * 256], [], [r_wo])
                xi, r_xi = xin.next()
                dma(fw, "sp", xi[:], x_own[o0:o0 + 512, nb * 256:(nb + 1) * 256].rearrange("(t p) n -> p t n", p=128),
                    [], [r_xi])
                gr, r_gr = gar.next()
                dma(fw, "sp", gr[:], mod_all[0:1, 2 * D + nb * 256:2 * D + (nb + 1) * 256].broadcast_to([128, 256]),
                    [], [r_gr])
                xo_, r_xo = xo.next()
                for tt in range(4):
                    bk, r_bk = banks[4 + (nb * 4 + tt) % 4]
                    mm_group(fw, bk[:, 0:256], [(mT[:, kc, tt * 128:(tt + 1) * 128], wov[:, kc, :]) for kc in range(KC)],
                             [r_wo, r_mT], [r_bk])
                    fw.op("dve", lambda e, bk=bk, tt=tt, gr=gr, xo_=xo_: e.tensor_tensor(
                        out=xo_[:, tt, :], in0=bk[:, 0:256], in1=gr[:], op=ALU.mult), [r_bk, r_gr], [r_xo])
                    fw.op("dve", lambda e, tt=tt, xi=xi, xo_=xo_: e.tensor_tensor(
                        out=xo_[:, tt, :], in0=xo_[:, tt, :], in1=xi[:, tt, :], op=ALU.add), [r_xo, r_xi], [r_xo])
                store(fw, "act", xnew_d[o0:o0 + 512, nb * 256:(nb + 1) * 256].rearrange("(t p) n -> p t n", p=128), xo_[:],
                      r_xo)
        fw.end_phase()
        if stop_after == 3:
            return _finish(nc, fw, dbg, out, r_out)

        fw.begin_phase()
        sc = nt_scratch()
        xring = fw.ring("xrow4", 2, [128, D], F32)
        h2T, r_h2T = fw.buf("h2T", [128, KC, 128], F32)
        wr, r_wr = fw.buf("wr", [128, KC, 36], F32)
        brr, r_brr = fw.buf("brr", [128, 36], F32)
        iot, r_iot = fw.buf("iot", [128, NE], F32)
        tril, r_tril = fw.buf("tril", [128, 128], F32)
        trilb, r_trilb = fw.buf("trilb", [128, 128], BF16)
        tke, r_tke = fw.buf("tke", [128, 16, 2, 2], I32)
        Mb, r_Mb = fw.buf("Mb", [128, 16, NE], BF16)
        dma(fw, "sp", wr[:], w_r_v, [], [r_wr])
        dma(fw, "sp", brr[:], b_r_rep, [], [r_brr])
        dma(fw, "sp", iot[:], iota_cap, [], [r_iot])
        dma(fw, "sp", tril[:], tril_in, [], [r_tril])
        dma(fw, "sp", tke[:], tok_ent, [], [r_tke])
        fw.op("dve", lambda e: e.tensor_copy(out=trilb[:], in_=tril[:]), [r_tril], [r_trilb])

        def sbuf1(name, shape, dt=F32):
            return fw.buf(name, shape, dt)
        L, r_L = sbuf1("L", [128, 36])
        gmax, r_gmax = sbuf1("gmax", [128, 1])
        ngmax, r_ngmax = sbuf1("ngmax", [128, 1])
        gmask, r_gmask = sbuf1("gmask", [128, 4])
        gexp, r_gexp = sbuf1("gexp", [128, 4])
        gsum, r_gsum = sbuf1("gsum", [128, 1])
        pg, r_pg = sbuf1("pg", [128, 1])
        pen, r_pen = sbuf1("pen", [128, 4])
        em, r_em = sbuf1("em", [128, NE])
        em2, r_em2 = sbuf1("em2", [128, NE])
        m1, r_m1 = sbuf1("m1", [128, 1])
        m2, r_m2 = sbuf1("m2", [128, 1])
        mk1, r_mk1 = sbuf1("mk1", [128, NE])
        mk2, r_mk2 = sbuf1("mk2", [128, NE])
        dd, r_dd = sbuf1("dd", [128, 1])
        e2, r_e2 = sbuf1("e2", [128, 1])
        wts, r_wts = sbuf1("wts", [128, 2])
        Msum, r_Msum = sbuf1("Msum", [128, NE])
        cum, r_cum = sbuf1("cum", [128, NE])
        tmp32, r_tmp32 = sbuf1("tmp32", [128, NE])
        pos, r_pos = sbuf1("pos", [128, 2])
        eb, r_eb = sbuf1("eb", [128, 2])
        ovf, r_ovf = sbuf1("ovf", [128, 2])
        dstf, r_dstf = sbuf1("dstf", [128, 2])
        dring = fw.ring("dsti", 2, [128, 2], I32)
        wring2 = fw.ring("wts2", 2, [128, 4], F32)

        for tt in range(16):
            g = tt // 4
            xr, r_xr = xring.next()
            dma(fw, "sp", xr[:], xnew_d[tt * 128:(tt + 1) * 128, :], [], [r_xr])
            norm_transpose(xr, r_xr, 128, A2, SH2, lambda kc: h2T[:, kc, :], r_h2T, sc, banks[4:8])
            lb, r_lb = banks[tt % 2]
            mm_group(fw, lb[:, 0:36], [(h2T[:, kc, :], wr[:, kc, :]) for kc in range(KC)], [r_h2T, r_wr], [r_lb])
            V = "dve"
            fw.op(V, lambda e, lb=lb: e.tensor_tensor(out=L[:], in0=lb[:, 0:36], in1=brr[:], op=ALU.add), [r_lb, r_brr], [r_L])
            fw.op(V, lambda e: e.tensor_reduce(out=gmax[:], in_=L[:, 0:4], axis=AX.X, op=ALU.max), [r_L], [r_gmax])
            fw.op(V, lambda e: e.tensor_scalar(out=gmask[:], in0=L[:, 0:4], scalar1=gmax[:, 0:1], scalar2=None,
                                               op0=ALU.is_equal), [r_L, r_gmax], [r_gmask])
            fw.op(V, lambda e: e.tensor_scalar(out=ngmax[:], in0=gmax[:], scalar1=-1.0, scalar2=None, op0=ALU.mult),
                  [r_gmax], [r_ngmax])
            fw.op("act", lambda e: e.activation(out=gexp[:], in_=L[:, 0:4], func=AF.Exp, bias=ngmax[:, 0:1],
                                                accum_out=gsum[:]), [r_L, r_ngmax], [r_gexp, r_gsum])
            fw.op(V, lambda e: e.reciprocal(out=pg[:], in_=gsum[:]), [r_gsum], [r_pg])
            fw.op(V, lambda e: e.tensor_scalar(out=pen[:], in0=gmask[:], scalar1=-1.0, scalar2=1e30, op0=ALU.add,
                                               op1=ALU.mult), [r_gmask], [r_pen])
            fw.op(V, lambda e: e.tensor_tensor(out=em[:].rearrange("p (g k) -> p g k", g=4),
                                               in0=L[:, 4:36].rearrange("p (g k) -> p g k", g=4),
                                               in1=pen[:].unsqueeze(2).broadcast_to([128, 4, 8]), op=ALU.add),
                  [r_L, r_pen], [r_em])
            fw.op(V, lambda e: e.tensor_reduce(out=m1[:], in_=em[:], axis=AX.X, op=ALU.max), [r_em], [r_m1])
            fw.op(V, lambda e: e.tensor_scalar(out=mk1[:], in0=em[:], scalar1=m1[:, 0:1], scalar2=None, op0=ALU.is_equal),
                  [r_em, r_m1], [r_mk1])
            fw.op(V, lambda e: e.scalar_tensor_tensor(out=em2[:], in0=mk1[:], scalar=-1e30, in1=em[:], op0=ALU.mult,
                                                      op1=ALU.add), [r_mk1, r_em], [r_em2])
            fw.op(V, lambda e: e.tensor_reduce(out=m2[:], in_=em2[:], axis=AX.X, op=ALU.max), [r_em2], [r_m2])
            fw.op(V, lambda e: e.tensor_scalar(out=mk2[:], in0=em2[:], scalar1=m2[:, 0:1], scalar2=None, op0=ALU.is_equal),
                  [r_em2, r_m2], [r_mk2])
            fw.op(V, lambda e: e.tensor_tensor(out=dd[:], in0=m2[:], in1=m1[:], op=ALU.subtract), [r_m1, r_m2], [r_dd])
            fw.op("act", lambda e: e.activation(out=e2[:], in_=dd[:], func=AF.Exp), [r_dd], [r_e2])
            wt2, r_wt2 = wring2.next()
            fw.op(V, lambda e: e.tensor_scalar(out=e2[:], in0=e2[:], scalar1=1.0, scalar2=None, op0=ALU.add), [r_e2], [r_e2])
            fw.op(V, lambda e: e.reciprocal(out=e2[:], in_=e2[:]), [r_e2], [r_e2])
            fw.op(V, lambda e, wt2=wt2: e.memset(wt2[:], 0.0), [], [r_wt2])
            fw.op(V, lambda e, wt2=wt2: e.tensor_tensor(out=wt2[:, 0:1], in0=e2[:], in1=pg[:], op=ALU.mult), [r_e2, r_pg], [r_wt2])
            fw.op(V, lambda e, wt2=wt2: e.tensor_tensor(out=wt2[:, 2:3], in0=pg[:], in1=wt2[:, 0:1], op=ALU.subtract),
                  [r_pg, r_wt2], [r_wt2])
            fw.op(V, lambda e: e.tensor_tensor(out=Msum[:], in0=mk1[:], in1=mk2[:], op=ALU.add), [r_mk1, r_mk2], [r_Msum])
            fw.op(V, lambda e, tt=tt: e.tensor_copy(out=Mb[:, tt, :], in_=Msum[:]), [r_Msum], [r_Mb])
            cb_, r_cb = banks[2 + tt % 2]
            pairs = [(trilb[:], Mb[:, tt, :])] + [(ones_b[:], Mb[:, j, :]) for j in range(tt)]
            mm_group(fw, cb_[:, 0:NE], pairs, [r_trilb, r_ones_b, r_Mb], [r_cb])
            fw.op(V, lambda e, cb_=cb_: e.tensor_copy(out=cum[:], in_=cb_[:, 0:NE]), [r_cb], [r_cum])
            for k, (mk, r_mk) in enumerate(((mk1, r_mk1), (mk2, r_mk2))):
                fw.op(V, lambda e, mk=mk: e.tensor_tensor(out=tmp32[:], in0=mk[:], in1=cum[:], op=ALU.mult), [r_mk, r_cum], [r_tmp32])
                fw.op(V, lambda e, k=k: e.tensor_reduce(out=pos[:, k:k + 1], in_=tmp32[:], axis=AX.X, op=ALU.add), [r_tmp32], [r_pos])
                fw.op(V, lambda e, mk=mk: e.tensor_tensor(out=tmp32[:], in0=mk[:], in1=iot[:], op=ALU.mult), [r_mk, r_iot], [r_tmp32])
                fw.op(V, lambda e, k=k: e.tensor_reduce(out=eb[:, k:k + 1], in_=tmp32[:], axis=AX.X, op=ALU.add), [r_tmp32], [r_eb])
            fw.op(V, lambda e: e.tensor_scalar(out=ovf[:], in0=pos[:], scalar1=float(CAP) - 0.5, scalar2=1.0e6, op0=ALU.is_gt,
                                               op1=ALU.mult), [r_pos], [r_ovf])
            fw.op(V, lambda e: e.tensor_tensor(out=dstf[:], in0=pos[:], in1=eb[:], op=ALU.add), [r_pos, r_eb], [r_dstf])
            fw.op(V, lambda e: e.tensor_tensor(out=dstf[:], in0=dstf[:], in1=ovf[:], op=ALU.add), [r_dstf, r_ovf], [r_dstf])
            di, r_di = dring.next()
            fw.op(V, lambda e, di=di: e.tensor_copy(out=di[:], in_=dstf[:]), [r_dstf], [r_di])
            for k in range(2):
                fw.op("pool", lambda e, di=di, k=k, tt=tt: [e.indirect_dma_start(
                    out=tab_i, out_offset=bass.IndirectOffsetOnAxis(ap=di[:, k:k + 1], axis=0),
                    in_=tke[:, tt, k, :], in_offset=None, bounds_check=fw.bc_reg(e, NE * CAP - 1), oob_is_err=False)],
                    [r_di, r_tke], [r_tab_i], dma=1)
                fw.op("pool", lambda e, di=di, k=k, wt2=wt2: [e.indirect_dma_start(
                    out=tab_w, out_offset=bass.IndirectOffsetOnAxis(ap=di[:, k:k + 1], axis=0),
                    in_=wt2[:, 2 * k:2 * k + 2], in_offset=None, bounds_check=fw.bc_reg(e, NE * CAP - 1), oob_is_err=False)],
                    [r_di, r_wt2], [r_tab_w], dma=1)
            if tt % 4 == 3:
                fw.emit()
        fw.end_phase()
        if stop_after == 4:
            return _finish(nc, fw, dbg, out, r_out)

        fw.begin_phase()
        sc = nt_scratch()
        xg = fw.ring("xg", 2, [128, D], F32)
        gT, r_gT = fw.buf("gT", [128, KC, CAP], BF16)
        actT, r_actT = fw.buf("actT", [128, FF // 128, CAP], BF16)
        wring = fw.ring("wbC", 3, [128, 8192], BF16)
        idxr = fw.ring("idx", 2 * NST, [128, 2], I32)
        wtr = fw.ring("wtc", 2 * NST, [128, 2], F32)
        sgr = fw.ring("sgr", 2, [128, CAP], F32)
        ypc = fw.ring("ypc", 3, [128, 1024], F32)
        for ex in range(NE):
            idxs = []
            for stl in range(NST):
                ix, r_ix = idxr.next()
                w1, r_w1 = wtr.next()
                r0 = ex * CAP + stl * 128
                dma(fw, "sp", ix[:], tab_i[r0:r0 + 128, :], [r_tab_i], [r_ix])
                dma(fw, "sp", w1[:], tab_w[r0:r0 + 128, :], [r_tab_w], [r_w1])
                idxs.append((ix, r_ix, w1, r_w1))
                xr, r_xr = xg.next()
                fw.op("pool", lambda e, xr=xr, ix=ix: [e.indirect_dma_start(
                    out=xr[:], out_offset=None, in_=xnew_d,
                    in_offset=bass.IndirectOffsetOnAxis(ap=ix[:, 0:1], axis=0))],
                    [r_ix], [r_xr], dma=1)
                norm_transpose(xr, r_xr, 128, A2, SH2, lambda kc, stl=stl: gT[:, kc, stl * 128:(stl + 1) * 128], r_gT,
                               sc, banks[4:8])
            for fb in range(4):
                wg_, r_wg = wring.next()
                wgv = wg_[:].rearrange("p (k n) -> p k n", k=KC)
                dma(fw, "pool", wgv, w_eg[ex].rearrange("(kc p) n -> p kc n", p=128)[:, :, fb * 256:(fb + 1) * 256], [], [r_wg])
                wu_, r_wu = wring.next()
                wuv = wu_[:].rearrange("p (k n) -> p k n", k=KC)
                dma(fw, "pool", wuv, w_eu[ex].rearrange("(kc p) n -> p kc n", p=128)[:, :, fb * 256:(fb + 1) * 256], [], [r_wu])
                for sub in range(2):
                    fc = fb * 2 + sub
                    bg, r_bg = banks[(fc * 2) % 4]
                    bu, r_bu = banks[(fc * 2 + 1) % 4]
                    mm_group(fw, bg[:, 0:CAP], [(wgv[:, kc, sub * 128:(sub + 1) * 128], gT[:, kc, :]) for kc in range(KC)],
                             [r_wg, r_gT], [r_bg])
                    mm_group(fw, bu[:, 0:CAP], [(wuv[:, kc, sub * 128:(sub + 1) * 128], gT[:, kc, :]) for kc in range(KC)],
                             [r_wu, r_gT], [r_bu])
                    sg_, r_sg_ = sgr.next()
                    fw.op("act", lambda e, bg=bg, sg_=sg_: e.activation(out=sg_[:], in_=bg[:, 0:CAP], func=AF.Silu), [r_bg], [r_sg_])
                    fw.op("dve", lambda e, bu=bu, sg_=sg_, fc=fc: e.tensor_tensor(out=actT[:, fc, :], in0=bu[:, 0:CAP], in1=sg_[:],
                                                                                op=ALU.mult), [r_bu, r_sg_], [r_actT])
            for db in range(4):
                wd_, r_wd = wring.next()
                wdv = wd_[:].rearrange("p (k n) -> p k n", k=FF // 128)
                dma(fw, "pool", wdv, w_ed[ex].rearrange("(kc p) n -> p kc n", p=128)[:, :, db * 1024:(db + 1) * 1024], [], [r_wd])
                for stl in range(NST):
                    ix, r_ix, w1, r_w1 = idxs[stl]
                    yp, r_yp = ypc.next()
                    for hf in range(2):
                        bk, r_bk = banks[4 + (db * 2 * NST + stl * 2 + hf) % 4]
                        mm_group(fw, bk[:, :], [(actT[:, kc, stl * 128:(stl + 1) * 128], wdv[:, kc, hf * 512:(hf + 1) * 512])
                                               for kc in range(FF // 128)], [r_wd, r_actT], [r_bk])
                        if hf == 0:
                            fw.op("act", lambda e, bk=bk, yp=yp, w1=w1: e.activation(
                                out=yp[:, 0:512], in_=bk[:, :], func=AF.Identity, scale=w1[:, 0:1]), [r_bk, r_w1], [r_yp])
                        else:
                            fw.op("dve", lambda e, bk=bk, yp=yp, w1=w1: e.tensor_scalar(
                                out=yp[:, 512:1024], in0=bk[:, :], scalar1=w1[:, 0:1], scalar2=None, op0=ALU.mult),
                                [r_bk, r_w1], [r_yp])
                    fw.op("pool", lambda e, yp=yp, ix=ix, db=db: [e.indirect_dma_start(
                        out=ybufs[db], out_offset=bass.IndirectOffsetOnAxis(ap=ix[:, 1:2], axis=0),
                        in_=yp[:], in_offset=None, bounds_check=fw.bc_reg(e, 2 * OWN - 1), oob_is_err=False)],
                        [r_yp, r_ix], [], dma=1, sem_res=r_yp)
            if ex % 4 == 3:
                fw.emit()
        fw.end_phase()
        if stop_after == 5:
            return _finish(nc, fw, dbg, out, r_out)

        fw.begin_phase()
        ga2, r_ga2 = fw.buf("ga2", [128, D], F32)
        gfr, r_gfr = fw.buf("gfr", [128, D], F32)
        dma(fw, "sp", ga2[:], mod_all[0:1, 5 * D:6 * D].broadcast_to([128, D]), [], [r_ga2])
        dma(fw, "sp", gfr[:], gf_rep_in, [], [r_gfr])
        xr_ = fw.ring("xd", 2, [128, D], F32)
        y1r = fw.ring("y1", 2, [128, D], F32)
        y2r = fw.ring("y2", 2, [128, D], F32)
        junk, r_junk = fw.buf("junkd", [128, D], BF16)
        ssr = fw.ring("ssd", 2, [128, 1], F32)
        for tt in range(16):
            xr, r_xr = xr_.next()
            y1, r_y1 = y1r.next()
            y2, r_y2 = y2r.next()
            dma(fw, "sp", xr[:], xnew_d[tt * 128:(tt + 1) * 128, :], [], [r_xr])
            fw.op("sp", lambda e, y1=y1, tt=tt: [e.dma_start(out=y1[:, i * 1024:(i + 1) * 1024],
                                                             in_=ybufs[i][tt * 128:(tt + 1) * 128, :]) for i in range(4)],
                  [], [r_y1], dma=4)
            fw.op("sp", lambda e, y2=y2, tt=tt: [e.dma_start(out=y2[:, i * 1024:(i + 1) * 1024],
                                                             in_=ybufs[i][OWN + tt * 128:OWN + (tt + 1) * 128, :]) for i in range(4)],
                  [], [r_y2], dma=4)
            fw.op("pool", lambda e, y1=y1, y2=y2: e.tensor_tensor(out=y1[:], in0=y1[:], in1=y2[:], op=ALU.add), [r_y1, r_y2], [r_y1])
            fw.op("dve", lambda e, y1=y1: e.tensor_tensor(out=y1[:], in0=y1[:], in1=ga2[:], op=ALU.mult), [r_y1, r_ga2], [r_y1])
            fw.op("pool", lambda e, y1=y1, xr=xr: e.tensor_tensor(out=xr[:], in0=xr[:], in1=y1[:], op=ALU.add), [r_y1, r_xr], [r_xr])
            ss, r_ss = ssr.next()
            fw.op("act", lambda e, xr=xr, ss=ss: e.activation(out=junk[:], in_=xr[:], func=AF.Square, accum_out=ss[:]),
                  [r_xr], [r_junk, r_ss])
            fw.op("act", lambda e, ss=ss: e.activation(out=ss[:], in_=ss[:], func=AF.Sqrt, scale=1.0 / D, bias=epst[:, 0:1]),
                  [r_ss, r_eps], [r_ss])
            fw.op("dve", lambda e, ss=ss: e.reciprocal(out=ss[:], in_=ss[:]), [r_ss], [r_ss])
            fw.op("dve", lambda e, xr=xr, ss=ss, y2=y2: e.scalar_tensor_tensor(
                out=y2[:], in0=xr[:], scalar=ss[:, 0:1], in1=gfr[:], op0=ALU.mult, op1=ALU.mult), [r_xr, r_ss, r_gfr, r_y2], [r_y2])
            store(fw, "pool", out[tt * 128:(tt + 1) * 128, :], y2[:], r_y2)
        fw.end_phase()
        if debug:
            fw.begin_phase()
            def ddump(name, src, shape, dt):
                d = nc.dram_tensor(name, list(shape), dt, kind="ExternalOutput").ap()
                dma(fw, "sp", d, src, [], [fw.res(name, True)])
            ddump("dbg_qT", qT_d[:, :, 0:512], [NQH, 128, 512], BF16)
            ddump("dbg_uT", uT_d[0:2], [2, 128, UW], F32)
            ddump("dbg_gates", gates_d[0:2, :, 0:512], [2, 128, 512], F32)
            ddump("dbg_attnT", attnT_d[:, :, 0:512], [16, 128, 512], BF16)
            ddump("dbg_csT", csT_d[:, :, 0:512], [16, 128, 512], BF16)
            ddump("dbg_xnew", xnew_d[0:256, :], [256, D], F32)
            ddump("dbg_tab_i", tab_i, [NE * CAP, 2], I32)
            ddump("dbg_tab_w", tab_w, [NE * CAP, 2], F32)
            ddump("dbg_ybuf0", ybufs[0][0:128, :], [128, 1024], F32)
            ddump("dbg_ybuf1", ybufs[0][OWN:OWN + 128, :], [128, 1024], F32)
            fw.end_phase()
        return _finish(nc, fw, dbg, out, r_out)


def _dump(nc, fw, name, t, r_t, shape, dt):
    d = nc.dram_tensor(name, list(shape), dt, kind="ExternalOutput").ap()
    r_d = fw.res(name, True)
    dma(fw, "sp", d, t[:], [r_t], [r_d])
    return d


def _finish(nc, fw, dbg, out, r_out):
    return nc


def _rope_tables(hf):
    inv = (10000.0 ** (-np.arange(0, 64, 2, dtype=np.float32) / 64.0)).astype(np.float32)
    tok = np.concatenate([np.arange(hf * OWN, (hf + 1) * OWN), np.arange((1 - hf) * OWN, (2 - hf) * OWN)])
    row = (tok // 64).astype(np.float32)
    col = (tok % 64).astype(np.float32)
    C = np.ones((128, NKEY), np.float32)
    S = np.zeros((128, NKEY), np.float32)
    for d in range(128):
        a = d // 64
        r = d % 64
        j = r % 32
        half = r // 32
        pos = row if a == 0 else col
        ang = (pos * inv[j]).astype(np.float32)
        C[d, :SEQ] = np.cos(ang)
        S[d, :SEQ] = np.sin(ang) * (-1.0 if half == 0 else 1.0)
    return C, S


def _consts():
    ident = np.eye(128, dtype=np.float32)
    pm = np.zeros((128, 128), np.float32)
    for m in range(128):
        r = m % 64
        partner = m + 32 if (r // 32) == 0 else m - 32
        pm[partner, m] = 1.0
    tril = np.zeros((128, 128), np.float32)
    for k in range(128):
        tril[k, k + 1:] = 1.0
    iota_cap = np.tile((np.arange(NE, dtype=np.float32) * CAP)[None, :], (128, 1))
    tok_ent = np.zeros((128, 16, 2, 2), np.int32)
    for tt in range(16):
        t = tt * 128 + np.arange(128)
        for k in range(2):
            tok_ent[:, tt, k, 0] = t
            tok_ent[:, tt, k, 1] = k * OWN + t
    tab_i_init = np.zeros((NE * CAP, 2), np.int32)
    tab_i_init[:, 1] = 1 << 24
    tab_w_init = np.zeros((NE * CAP, 2), np.float32)
    return dict(ident=ident, pmat=pm, tril=tril, iota_cap=iota_cap, tok_ent=tok_ent,
                tab_i_init=tab_i_init, tab_w_init=tab_w_init)


_NC_CACHE = {}


def kernel(x, c, ctx, c_ctx, norm1_g, w_mod, b_mod, w_in, q_norm_g, k_norm_g, w_attn_out,
           conv_dw_w, conv_dw_b, conv_ln_g, conv_ln_b, w_conv_out, w_out, norm2_g,
           w_router_group, b_router_group, w_router_expert, b_router_expert,
           w_exp_gate, w_exp_up, w_exp_down, norm_f_g, _debug=False, _stop_after=None):
    f = lambda a: np.ascontiguousarray(np.asarray(a, dtype=np.float32))
    x, c, ctx, c_ctx = f(x), f(c), f(ctx), f(c_ctx)
    fm = lambda v: np.ascontiguousarray(f(v).reshape(-1, 128).T)
    consts = _consts()
    shared = dict(
        qk_g=np.ascontiguousarray(np.stack([f(q_norm_g)[0], f(k_norm_g)[0]], axis=1)),
        g1=fm(norm1_g[0]), g2=fm(norm2_g[0]),
        gf_rep=np.ascontiguousarray(np.tile(f(norm_f_g)[None, :], (128, 1))),
        bmod2=np.ascontiguousarray(np.tile(f(b_mod)[0][None, :], (2, 1))),
        w_mod=f(w_mod)[0], w_in=f(w_in)[0], w_attn_out=f(w_attn_out)[0], w_conv_out=f(w_conv_out)[0],
        w_out=f(w_out)[0],
        conv_w=np.ascontiguousarray(f(conv_dw_w)[0, :, 0, :].T.reshape(16, 128, TAPS).transpose(1, 0, 2)),
        conv_b=fm(conv_dw_b[0]), ln_g=fm(conv_ln_g[0]), ln_b=fm(conv_ln_b[0]),
        w_router=np.ascontiguousarray(np.concatenate([f(w_router_group)[0], f(w_router_expert)[0]], axis=1)),
        b_router_rep=np.ascontiguousarray(np.tile(np.concatenate([f(b_router_group)[0], f(b_router_expert)[0]])[None, :],
                                                  (128, 1))),
        w_exp_gate=f(w_exp_gate)[0], w_exp_up=f(w_exp_up)[0], w_exp_down=f(w_exp_down)[0],
        **consts,
    )
    in_maps = []
    for core in range(8):
        b, hf = core // 2, core % 2
        own = x[b, hf * OWN:(hf + 1) * OWN]
        oth = x[b, (1 - hf) * OWN:(2 - hf) * OWN]
        halo = np.zeros((2 * HALO, D), np.float32)
        mask = np.zeros((128, 2 * HALO), np.float32)
        if hf == 1:
            halo[:HALO] = x[b, OWN - HALO:OWN]
            mask[:, :HALO] = 1.0
        else:
            halo[HALO:] = x[b, OWN:OWN + HALO]
            mask[:, HALO:] = 1.0
        C, S = _rope_tables(hf)
        cv = np.ascontiguousarray(np.stack([c[b].reshape(KC, 128).T, c_ctx.reshape(KC, 128).T], axis=2))
        m = dict(shared)
        m.update(x_own=np.ascontiguousarray(own), x_oth=np.ascontiguousarray(oth), ctx_b=np.ascontiguousarray(ctx[b]),
                 x_halo=halo, halo_mask=mask, cvec=cv, rope_c=C, rope_s=S)
        in_maps.append(m)
    key = (_debug, _stop_after)
    if key not in _NC_CACHE:
        _NC_CACHE[key] = build_nc(debug=_debug, stop_after=_stop_after)
    nc = _NC_CACHE[key]
    res = run_bass_kernel_spmd(nc, in_maps, core_ids=list(range(8)))
    if _debug:
        return res
    outp = np.empty((4, SEQ, D), np.float32)
    for core in range(8):
        b, hf = core // 2, core % 2
        outp[b, hf * OWN:(hf + 1) * OWN] = np.asarray(res.results[core]["out"], dtype=np.float32)
    return outp
```

```python
import contextlib
import numpy as np
import concourse.bass as bass
import concourse.mybir as mybir
from concourse.bass_utils import run_bass_kernel_spmd

F32 = mybir.dt.float32
BF16 = mybir.dt.bfloat16
I32 = mybir.dt.int32
ALU = mybir.AluOpType
AF = mybir.ActivationFunctionType
AX = mybir.AxisListType

D = 4096
KC = D // 128
SEQ = 4096
OWN = 2048
NCTX = 256
NKEY = SEQ + NCTX
NKT = NKEY // 128
HD = 128
NQH = 16
NKVH = 4
CW = 2048
TAPS = 31
Q_OFF, K_OFF, V_OFF, GLU_OFF, GATE_OFF, IN_W = 0, 2048, 2560, 3072, 7168, 15360
NE = 32
FF = 1024
CAP = 512
NST = CAP // 128
EPS = 1e-6
HALO = 16
UW = OWN + 2 * HALO

EPOCH = 12000
SAME_ENGINE_SYNC = True


class Sem:
    def __init__(self, fw, name, step):
        self.fw, self.name, self.step = fw, name, step
        self.count = 0
        self.handles = []

    def _handle(self, ep):
        while len(self.handles) <= ep:
            h = self.fw.stack.enter_context(self.fw.nc.semaphore(f"{self.name}_{len(self.handles)}"))
            self.handles.append(h)
        return self.handles[ep]

    def next(self, n=1):
        ep = self.count // EPOCH
        if (self.count + n - 1) // EPOCH != ep:
            self.count = (ep + 1) * EPOCH
            ep += 1
        self.count += n
        idx = self.count - ep * EPOCH
        return self._handle(ep), (self, ep, idx * self.step)

    def last_token(self):
        if self.count == 0:
            return None
        ep = (self.count - 1) // EPOCH
        return (self, ep, (self.count - ep * EPOCH) * self.step)


class Res:
    def __init__(self, name, persistent):
        self.name = name
        self.persistent = persistent
        self.last_write = None
        self.readers = []
        self.dma_sem = None


class FW:
    ENGS = ("pe", "act", "dve", "pool", "sp")

    def __init__(self, nc, stack):
        self.nc, self.stack = nc, stack
        self.ops = {e: [] for e in self.ENGS}
        self.esem = {e: Sem(self, f"s_{e}", 1) for e in ("pe", "act", "dve", "pool")}
        self.waited = {e: {} for e in self.ENGS}
        self.sem_pool = []
        self.all_dma_sems = []
        self.phase_res = []
        self.ph = None

    def begin_phase(self):
        self.ph = contextlib.ExitStack()
        self.phase_res = []

    def end_phase(self):
        self.barrier()
        self.emit()
        for r in self.phase_res:
            if r.dma_sem is not None:
                self.sem_pool.append(r.dma_sem)
                r.dma_sem = None
        self.phase_res = []
        self.ph.close()
        self.ph = None

    def res(self, name="r", persistent=False):
        r = Res(name, persistent)
        if not persistent:
            self.phase_res.append(r)
        return r

    def sb(self, name, shape, dtype, persistent=False, stack=None):
        st = stack if stack is not None else (self.stack if persistent else self.ph)
        self.nsb = getattr(self, "nsb", 0) + 1
        t = st.enter_context(self.nc.sbuf_tensor(f"sb{self.nsb}_{name}", list(shape), dtype))
        return t

    def buf(self, name, shape, dtype, persistent=False, stack=None):
        return self.sb(name, shape, dtype, persistent, stack), self.res(name, persistent or stack is not None)

    def ring(self, name, n, shape, dtype):
        return Ring([self.buf(f"{name}{i}", shape, dtype) for i in range(n)])

    def _waits_for(self, eng, toks):
        out = []
        for tok in toks:
            if tok is None:
                continue
            sem, ep, val = tok
            key = (id(sem), ep)
            if self.waited[eng].get(key, 0) >= val:
                continue
            self.waited[eng][key] = val
            out.append((sem.handles[ep], val))
        return out

    def op(self, eng, fn, reads=(), writes=(), dma=0, sem_res=None):
        toks = []
        for r in reads:
            toks.append(r.last_write)
        for w in writes:
            toks.append(w.last_write)
            toks.extend(w.readers)
        if dma:
            anchor = sem_res if sem_res is not None else writes[0]
            if anchor.dma_sem is None:
                if self.sem_pool:
                    anchor.dma_sem = self.sem_pool.pop()
                else:
                    anchor.dma_sem = Sem(self, f"d{len(self.all_dma_sems)}", 16)
                    self.all_dma_sems.append(anchor.dma_sem)
            handle, tok = anchor.dma_sem.next(dma)
            own = None
        else:
            handle, tok = self.esem[eng].next(1)
            own = self.esem[eng]
        if own is not None and not (SAME_ENGINE_SYNC and eng in ("act", "dve", "pool")):
            toks = [t for t in toks if t is None or t[0] is not own]
        waits = self._waits_for(eng, toks)
        self.ops[eng].append((waits, fn, handle, dma))
        for r in reads:
            r.readers.append(tok)
        for w in writes:
            w.last_write = tok
            w.readers = []
        return tok

    def bc_reg(self, e, val):
        if val not in self._regs:
            self._regs[val] = e.to_reg(val)
        return self._regs[val]

    def barrier(self):
        toks = [s.last_token() for s in self.esem.values()]
        toks += [s.last_token() for s in self.all_dma_sems]
        for eng in self.ENGS:
            waits = self._waits_for(eng, toks)
            self.ops[eng].append((waits, None, None, 0))

    def emit(self):
        nc = self.nc
        ops = self.ops
        self.ops = {e: [] for e in self.ENGS}
        with nc.Block() as block:
            def run(engname):
                def body(e):
                    self._regs = {}
                    for waits, fn, handle, dma in ops[engname]:
                        for h, v in waits:
                            e.wait_ge(h, v)
                        if fn is None:
                            continue
                        r = fn(e)
                        if dma:
                            assert isinstance(r, (list, tuple)) and len(r) == dma, (len(r), dma)
                            for ins in r:
                                ins.then_inc(handle, 16)
                        else:
                            r.then_inc(handle, 1)
                return body
            block.tensor(run("pe"))
            block.scalar(run("act"))
            block.vector(run("dve"))
            block.gpsimd(run("pool"))
            block.sync(run("sp"))


class Ring:
    def __init__(self, items):
        self.items = items
        self.i = 0

    def next(self):
        it = self.items[self.i % len(self.items)]
        self.i += 1
        return it


def mm_group(fw, out_ap, pairs, reads, writes):
    pairs = list(pairs)

    def fn(e):
        n = len(pairs)
        last = None
        for i, (l, r) in enumerate(pairs):
            last = e.matmul(out_ap, lhsT=l, rhs=r, start=(i == 0), stop=(i == n - 1))
        return last
    return fw.op("pe", fn, reads, writes)


def dma(fw, eng, out, in_, reads, writes, sem_res=None):
    return fw.op(eng, lambda e: [e.dma_start(out=out, in_=in_)], reads, writes, dma=1, sem_res=sem_res)


def store(fw, eng, out, in_, r_src):
    return fw.op(eng, lambda e: [e.dma_start(out=out, in_=in_)], [r_src], [], dma=1, sem_res=r_src)


def build_nc(debug=False, stop_after=None):
    nc = bass.Bass("TRN2", target_bir_lowering=False)

    def din(name, shape, dt=F32):
        return nc.dram_tensor(name, list(shape), dt, kind="ExternalInput").ap()

    def dscr(name, shape, dt=F32):
        return nc.dram_tensor(name, list(shape), dt, kind="Internal").ap()

    x_own = din("x_own", [OWN, D])
    x_oth = din("x_oth", [OWN, D])
    ctx_b = din("ctx_b", [NCTX, D])
    x_halo = din("x_halo", [2 * HALO, D])
    halo_mask = din("halo_mask", [128, 2 * HALO])
    cvec = din("cvec", [128, KC, 2])
    rope_c = din("rope_c", [128, NKEY])
    rope_s = din("rope_s", [128, NKEY])
    qk_g = din("qk_g", [128, 2])
    ident_in = din("ident", [128, 128])
    pmat_in = din("pmat", [128, 128])
    tril_in = din("tril", [128, 128])
    g1_in = din("g1", [128, KC])
    g2_in = din("g2", [128, KC])
    gf_rep_in = din("gf_rep", [128, D])
    bmod2 = din("bmod2", [2, 6 * D])
    w_mod = din("w_mod", [D, 6 * D])
    w_in = din("w_in", [D, IN_W])
    w_ao = din("w_attn_out", [NQH * HD, D])
    w_co = din("w_conv_out", [CW, D])
    w_out = din("w_out", [D, D])
    cw_in = din("conv_w", [128, 16, TAPS])
    cb_in = din("conv_b", [128, 16])
    lng_in = din("ln_g", [128, 16])
    lnb_in = din("ln_b", [128, 16])
    w_r = din("w_router", [D, 36])
    b_r_rep = din("b_router_rep", [128, 36])
    iota_cap = din("iota_cap", [128, NE])
    tok_ent = din("tok_ent", [128, 16, 2, 2], I32)
    tab_i_init = din("tab_i_init", [NE * CAP, 2], I32)
    tab_w_init = din("tab_w_init", [NE * CAP, 2])
    w_eg = din("w_exp_gate", [NE, D, FF])
    w_eu = din("w_exp_up", [NE, D, FF])
    w_ed = din("w_exp_down", [NE, FF, D])
    out = nc.dram_tensor("out", [OWN, D], F32, kind="ExternalOutput").ap()

    mod_all = dscr("mod_all", [2, 6 * D])
    qT_d = dscr("qT_d", [NQH, 128, OWN], BF16)
    uT_d = dscr("uT_d", [16, 128, UW])
    gates_d = dscr("gates_d", [64, 128, OWN])
    attnT_d = dscr("attnT_d", [16, 128, OWN], BF16)
    csT_d = dscr("csT_d", [16, 128, OWN], BF16)
    xnew_d = dscr("xnew_d", [OWN, D])
    tab_i = dscr("tab_i", [NE * CAP, 2], I32)
    tab_w = dscr("tab_w", [NE * CAP, 2])
    ybufs = [dscr(f"ybuf{i}", [2 * OWN, 1024]) for i in range(4)]

    w_mod_v = w_mod.rearrange("(kc p) n -> p kc n", p=128)
    w_in_v = w_in.rearrange("(kc p) n -> p kc n", p=128)
    w_ao_v = w_ao.rearrange("(kc p) n -> p kc n", p=128)
    w_co_v = w_co.rearrange("(kc p) n -> p kc n", p=128)
    w_out_v = w_out.rearrange("(kc p) n -> p kc n", p=128)
    w_r_v = w_r.rearrange("(kc p) n -> p kc n", p=128)

    dbg = {}

    with contextlib.ExitStack() as st:
        fw = FW(nc, st)
        r_mod_all = fw.res("mod_all", True)
        r_qT_d = fw.res("qT_d", True)
        r_uT_d = fw.res("uT_d", True)
        r_gates_d = fw.res("gates_d", True)
        r_attnT_d = fw.res("attnT_d", True)
        r_csT_d = fw.res("csT_d", True)
        r_xnew_d = [fw.res(f"xnew_d{g}", True) for g in range(4)]
        r_tab_i = fw.res("tab_i", True)
        r_tab_w = fw.res("tab_w", True)
        r_ybuf = fw.res("ybuf", True)
        r_out = fw.res("out", True)

        ident, r_ident = fw.buf("ident", [128, 128], F32, True)
        identb, r_identb = fw.buf("identb", [128, 128], BF16, True)
        pmat, r_pmat = fw.buf("pmat", [128, 128], F32, True)
        ones_f, r_ones_f = fw.buf("ones_f", [128, 128], F32, True)
        ones_b, r_ones_b = fw.buf("ones_b", [128, 128], BF16, True)
        epst, r_eps = fw.buf("epst", [128, 1], F32, True)
        qkg, r_qkg = fw.buf("qkg", [128, 2], F32, True)
        modv, r_modv = fw.buf("modv", [128, 6, KC], F32, True)
        A1, SH1, A1C, SH1C, A2, SH2 = range(6)

        banks = []
        for i in range(8):
            t = st.enter_context(nc.psum_tensor(f"bank{i}", [128, 512], F32))
            banks.append((t, fw.res(f"bank{i}", True)))

        def norm_transpose(rows, r_rows, nrows, mv_a, mv_s, dst_fn, r_dst, sc, pbanks, evac_engs=("act", "dve")):
            junk, r_junk, ss, r_ss, rstd, r_rstd, dg, r_dg, ss2, r_ss2 = sc
            for hh in range(2):
                fw.op("act", lambda e, hh=hh: e.activation(out=junk[:nrows, :], in_=rows[:nrows, hh * 2048:(hh + 1) * 2048],
                                                           func=AF.Square, accum_out=ss2[:nrows, hh:hh + 1]),
                      [r_rows], [r_junk, r_ss2])
            fw.op("dve", lambda e: e.tensor_tensor(out=ss[:nrows, :], in0=ss2[:nrows, 0:1], in1=ss2[:nrows, 1:2], op=ALU.add),
                  [r_ss2], [r_ss])
            fw.op("act", lambda e: e.activation(out=rstd[:nrows, :], in_=ss[:nrows, :], func=AF.Sqrt,
                                                scale=1.0 / D, bias=epst[:nrows, 0:1]), [r_ss, r_eps], [r_rstd])
            fw.op("dve", lambda e: e.reciprocal(out=rstd[:nrows, :], in_=rstd[:nrows, :]), [r_rstd], [r_rstd])
            fw.op("dve", lambda e: e.tensor_scalar(out=dg[:nrows, :nrows], in0=ident[:nrows, :nrows],
                                                   scalar1=rstd[:nrows, 0:1], scalar2=None, op0=ALU.mult),
                  [r_rstd, r_ident], [r_dg])
            for q in range(KC // 4):
                bk, r_bk = pbanks[q % len(pbanks)]

                def fn(e, q=q, bk=bk):
                    last = None
                    for j in range(4):
                        kc = q * 4 + j
                        last = e.matmul(bk[:, j * 128:j * 128 + nrows], lhsT=rows[:nrows, kc * 128:(kc + 1) * 128],
                                        rhs=dg[:nrows, :nrows], start=True, stop=True)
                    return last
                fw.op("pe", fn, [r_rows, r_dg], [r_bk])
                for j in range(4):
                    kc = q * 4 + j
                    eng = evac_engs[kc % len(evac_engs)]
                    if eng == "act":
                        fw.op("act", lambda e, kc=kc, j=j, bk=bk: e.activation(
                            out=dst_fn(kc), in_=bk[:, j * 128:j * 128 + nrows], func=AF.Identity,
                            scale=modv[:, mv_a, kc:kc + 1], bias=modv[:, mv_s, kc:kc + 1]),
                            [r_bk, r_modv], [r_dst])
                    else:
                        fw.op("dve", lambda e, kc=kc, j=j, bk=bk: e.tensor_scalar(
                            out=dst_fn(kc), in0=bk[:, j * 128:j * 128 + nrows],
                            scalar1=modv[:, mv_a, kc:kc + 1], scalar2=modv[:, mv_s, kc:kc + 1],
                            op0=ALU.mult, op1=ALU.add), [r_bk, r_modv], [r_dst])

        def nt_scratch():
            junk, r_junk = fw.buf("nt_junk", [128, D // 2], BF16)
            ss, r_ss = fw.buf("nt_ss", [128, 1], F32)
            ss2, r_ss2 = fw.buf("nt_ss2", [128, 2], F32)
            rstd, r_rstd = fw.buf("nt_rstd", [128, 1], F32)
            dg, r_dg = fw.buf("nt_dg", [128, 128], F32)
            return (junk, r_junk, ss, r_ss, rstd, r_rstd, dg, r_dg, ss2, r_ss2)

        fw.begin_phase()
        dma(fw, "sp", ident[:], ident_in, [], [r_ident])
        dma(fw, "sp", pmat[:], pmat_in, [], [r_pmat])
        dma(fw, "sp", qkg[:], qk_g, [], [r_qkg])
        fw.op("dve", lambda e: e.memset(ones_f[:], 1.0), [], [r_ones_f])
        fw.op("dve", lambda e: e.memset(ones_b[:], 1.0), [], [r_ones_b])
        fw.op("dve", lambda e: e.memset(epst[:], EPS), [], [r_eps])
        fw.op("dve", lambda e: e.tensor_copy(out=identb[:], in_=ident[:]), [r_ident], [r_identb])
        dma(fw, "sp", tab_i, tab_i_init, [], [r_tab_i])
        dma(fw, "sp", tab_w, tab_w_init, [], [r_tab_w])

        cv, r_cv = fw.buf("cv", [128, KC, 2], F32)
        sg, r_sg = fw.buf("sgc", [128, KC, 2], F32)
        dma(fw, "sp", cv[:], cvec, [], [r_cv])
        fw.op("act", lambda e: e.activation(out=sg[:], in_=cv[:], func=AF.Sigmoid), [r_cv], [r_sg])
        fw.op("dve", lambda e: e.tensor_tensor(out=sg[:], in0=sg[:], in1=cv[:], op=ALU.mult), [r_sg, r_cv], [r_sg])
        bmr = fw.ring("bm", 2, [2, 512], F32)
        mrr = fw.ring("modrow", 2, [2, 512], F32)
        wring = fw.ring("wm", 3, [128, 8, 512], F32)
        NCH = 6 * D // 512
        for ch in range(NCH):
            bk, r_bk = banks[ch % 2]
            tiles = []
            for k4 in range(4):
                wt, r_wt = wring.next()
                dma(fw, "sp", wt[:], w_mod_v[:, k4 * 8:(k4 + 1) * 8, ch * 512:(ch + 1) * 512], [], [r_wt])
                tiles.append((wt, r_wt))

                def fn(e, k4=k4, wt=wt, bk=bk):
                    last = None
                    for j in range(8):
                        kc = k4 * 8 + j
                        last = e.matmul(bk[0:2, :], lhsT=sg[:, kc, :], rhs=wt[:, j, :],
                                        start=(kc == 0), stop=(kc == KC - 1))
                    return last
                fw.op("pe", fn, [r_sg, r_wt], [r_bk])
            bm, r_bm = bmr.next()
            dma(fw, "sp", bm[:], bmod2[:, ch * 512:(ch + 1) * 512], [], [r_bm])
            mr, r_mr = mrr.next()
            fw.op("dve", lambda e, bk=bk, bm=bm, mr=mr: e.tensor_tensor(out=mr[:], in0=bk[0:2, :], in1=bm[:], op=ALU.add),
                  [r_bk, r_bm], [r_mr])
            store(fw, "sp", mod_all[:, ch * 512:(ch + 1) * 512], mr[:], r_mr)
        fw.barrier()
        mraw, r_mraw = fw.buf("mraw", [128, 6, KC], F32)
        g12, r_g12 = fw.buf("g12", [128, 2, KC], F32)
        dma(fw, "sp", g12[:, 0, :], g1_in, [], [r_g12])
        dma(fw, "sp", g12[:, 1, :], g2_in, [r_g12], [r_g12])

        def ld_mod(slot, row, j):
            src = mod_all[row, j * D:(j + 1) * D].rearrange("(kc p) -> p kc", p=128)
            fw.op("sp", lambda e: [e.dma_start(out=mraw[:, slot, :], in_=src, allow_slow_non_contiguous=True)],
                  [r_mraw], [r_mraw], dma=1)
        ld_mod(0, 0, 1)
        ld_mod(1, 0, 0)
        ld_mod(2, 1, 1)
        ld_mod(3, 1, 0)
        ld_mod(4, 0, 4)
        ld_mod(5, 0, 3)
        for (dst_a, dst_s, s_sc, s_sh, gi) in ((A1, SH1, 0, 1, 0), (A1C, SH1C, 2, 3, 0), (A2, SH2, 4, 5, 1)):
            fw.op("dve", lambda e, dst_a=dst_a, s_sc=s_sc, gi=gi: e.scalar_tensor_tensor(
                out=modv[:, dst_a, :], in0=mraw[:, s_sc, :], scalar=1.0, in1=g12[:, gi, :],
                op0=ALU.add, op1=ALU.mult), [r_mraw, r_g12], [r_modv])
            fw.op("dve", lambda e, dst_s=dst_s, s_sh=s_sh: e.tensor_copy(out=modv[:, dst_s, :], in_=mraw[:, s_sh, :]),
                  [r_mraw], [r_modv])
        if debug:
            dbg["modv"] = _dump(nc, fw, "dbg_modv", modv, r_modv, [128, 6, KC], F32)
        fw.end_phase()
        if stop_after == 0:
            return _finish(nc, fw, dbg, out, r_out)

        kv_stack = contextlib.ExitStack()
        KT, r_KT = fw.buf("KT", [128, NKVH, NKEY], BF16, stack=kv_stack)
        VA, r_VA = fw.buf("VA", [128, NKT, NKVH, HD + 2], BF16, stack=kv_stack)
        fw.begin_phase()
        fw.op("pool", lambda e: e.memset(VA[:, :, :, HD:HD + 2], 1.0), [], [r_VA])
        sc = nt_scratch()
        xring = fw.ring("xrow", 1, [128, D], F32)
        hT, r_hT = fw.buf("hT", [128, KC, 512], BF16)
        wring = fw.ring("wb", 3, [128, KC, 256], BF16)
        rc, r_rc = fw.buf("ropec", [128, 512], F32)
        rs, r_rs = fw.buf("ropes", [128, 512], F32)
        qg, r_qg = fw.buf("qg", [128, 512], F32)
        sq, r_sq = fw.buf("sq", [128, 512], BF16)
        rr, r_rr = fw.buf("rr", [128, 512], F32)
        t1, r_t1 = fw.buf("t1", [128, 512], F32)
        t2, r_t2 = fw.buf("t2", [128, 512], F32)
        qst = fw.ring("qst", 2, [128, 512], BF16)
        ust = fw.ring("ust", 2, [128, 512], F32)
        gst = fw.ring("gst", 2, [128, 512], F32)
        sgt, r_sgt = fw.buf("sgt", [128, 512], F32)
        hm, r_hm = fw.buf("hm", [128, 2 * HALO], F32)
        dma(fw, "sp", hm[:], halo_mask, [], [r_hm])

        groups = []
        for g in range(4):
            groups.append(dict(src=x_own[g * 512:(g + 1) * 512, :], n=512, a=A1, s=SH1, key0=g * 512,
                               full=True, own0=g * 512))
        for g in range(4):
            groups.append(dict(src=x_oth[g * 512:(g + 1) * 512, :], n=512, a=A1, s=SH1, key0=OWN + g * 512,
                               full=False))
        groups.append(dict(src=ctx_b, n=NCTX, a=A1C, s=SH1C, key0=SEQ, full=False))
        groups.append(dict(src=x_halo, n=2 * HALO, a=A1, s=SH1, key0=None, full=False, halo=True))

        def load_w(c0):
            wt, r_wt = wring.next()
            dma(fw, "pool", wt[:], w_in_v[:, :, c0:c0 + 256], [], [r_wt])
            return wt, r_wt

        def proj_fm(wt, r_wt, sub, n, bk, r_bk):
            mm_group(fw, bk[:, 0:n], [(wt[:, kc, sub * 128:(sub + 1) * 128], hT[:, kc, 0:n]) for kc in range(KC)],
                     [r_wt, r_hT], [r_bk])

        bank_i = [0]

        def next_bank(lo=0, hi=4):
            b = banks[lo + bank_i[0] % (hi - lo)]
            bank_i[0] += 1
            return b

        def do_group(G):
                n = G["n"]
                ntile = (n + 127) // 128
                for tt in range(ntile):
                    nr = min(128, n - tt * 128)
                    xr, r_xr = xring.next()
                    dma(fw, "sp", xr[:nr, :], G["src"][tt * 128:tt * 128 + nr, :], [], [r_xr])
                    norm_transpose(xr, r_xr, nr, G["a"], G["s"],
                                   lambda kc, tt=tt, nr=nr: hT[:, kc, tt * 128:tt * 128 + nr], r_hT, sc, banks[4:8])
                halo = G.get("halo", False)
                if not halo:
                    key0 = G["key0"]
                    dma(fw, "sp", rc[:, 0:n], rope_c[:, key0:key0 + n], [], [r_rc])
                    dma(fw, "sp", rs[:, 0:n], rope_s[:, key0:key0 + n], [], [r_rs])
                    heads = []
                    if G["full"]:
                        heads += [("q", h) for h in range(NQH)]
                    heads += [("k", h) for h in range(NKVH)]
                    for hi in range(0, len(heads), 2):
                        kind, h0 = heads[hi]
                        c0 = (Q_OFF if kind == "q" else K_OFF) + h0 * HD
                        wt, r_wt = load_w(c0)
                        for sub in range(2):
                            kind, h = heads[hi + sub]
                            gcol = 0 if kind == "q" else 1
                            bk, r_bk = next_bank()
                            proj_fm(wt, r_wt, sub, n, bk, r_bk)
                            fw.op("act", lambda e, bk=bk, gcol=gcol: e.activation(
                                out=qg[:, 0:n], in_=bk[:, 0:n], func=AF.Identity, scale=qkg[:, gcol:gcol + 1]),
                                [r_bk, r_qkg], [r_qg])
                            fw.op("act", lambda e, bk=bk: e.activation(out=sq[:, 0:n], in_=bk[:, 0:n], func=AF.Square),
                                  [r_bk], [r_sq])
                            b2, r_b2 = next_bank()
                            mm_group(fw, b2[:, 0:n], [(ones_b[:], sq[:, 0:n])], [r_ones_b, r_sq], [r_b2])
                            b3, r_b3 = next_bank()
                            mm_group(fw, b3[:, 0:n], [(pmat[:], qg[:, 0:n])], [r_pmat, r_qg], [r_b3])
                            fw.op("act", lambda e, b2=b2: e.activation(out=rr[:, 0:n], in_=b2[:, 0:n], func=AF.Sqrt,
                                                                       scale=1.0 / HD, bias=epst[:, 0:1]),
                                  [r_b2, r_eps], [r_rr])
                            fw.op("dve", lambda e: e.reciprocal(out=rr[:, 0:n], in_=rr[:, 0:n]), [r_rr], [r_rr])
                            fw.op("dve", lambda e: e.tensor_tensor(out=t1[:, 0:n], in0=qg[:, 0:n], in1=rc[:, 0:n], op=ALU.mult),
                                  [r_qg, r_rc], [r_t1])
                            fw.op("dve", lambda e, b3=b3: e.tensor_tensor(out=t2[:, 0:n], in0=b3[:, 0:n], in1=rs[:, 0:n],
                                                                          op=ALU.mult), [r_b3, r_rs], [r_t2])
                            fw.op("dve", lambda e: e.tensor_tensor(out=t1[:, 0:n], in0=t1[:, 0:n], in1=t2[:, 0:n], op=ALU.add),
                                  [r_t1, r_t2], [r_t1])
                            if kind == "k":
                                fw.op("dve", lambda e, h=h, key0=key0: e.tensor_tensor(
                                    out=KT[:, h, key0:key0 + n], in0=t1[:, 0:n], in1=rr[:, 0:n], op=ALU.mult),
                                    [r_t1, r_rr], [r_KT])
                            else:
                                qs, r_qs = qst.next()
                                fw.op("dve", lambda e, qs=qs: e.tensor_tensor(out=qs[:, 0:n], in0=t1[:, 0:n], in1=rr[:, 0:n],
                                                                               op=ALU.mult), [r_t1, r_rr], [r_qs])
                                o0 = G["own0"]
                                store(fw, "pool", qT_d[h, :, o0:o0 + n], qs[:, 0:n], r_qs)
                    for vb in range(2):
                        wt, r_wt = load_w(V_OFF + vb * 256)
                        for tt in range(ntile):
                            bk, r_bk = next_bank()
                            mm_group(fw, bk[:, 0:256], [(hT[:, kc, tt * 128:(tt + 1) * 128], wt[:, kc, :]) for kc in range(KC)],
                                     [r_wt, r_hT], [r_bk])
                            kt = (key0 // 128) + tt
                            eng = "act" if tt % 2 == 0 else "dve"
                            src = bk[:, 0:256].rearrange("p (h d) -> p h d", h=2)
                            if eng == "act":
                                fw.op("act", lambda e, kt=kt, vb=vb, src=src: e.activation(
                                    out=VA[:, kt, vb * 2:vb * 2 + 2, 0:HD], in_=src, func=AF.Identity), [r_bk], [r_VA])
                            else:
                                fw.op("dve", lambda e, kt=kt, vb=vb, src=src: e.tensor_copy(
                                    out=VA[:, kt, vb * 2:vb * 2 + 2, 0:HD], in_=src), [r_bk], [r_VA])
                if G["full"] or halo:
                    if halo:
                        ucol0 = None
                    else:
                        ucol0 = HALO + G["own0"]
                    for cb in range(8):
                        wa, r_wa = load_w(GLU_OFF + cb * 256)
                        wg, r_wg = load_w(GLU_OFF + CW + cb * 256)
                        for sub in range(2):
                            cc = cb * 2 + sub
                            ba, r_ba = next_bank()
                            proj_fm(wa, r_wa, sub, n, ba, r_ba)
                            bg, r_bg = next_bank()
                            proj_fm(wg, r_wg, sub, n, bg, r_bg)
                            fw.op("act", lambda e, bg=bg: e.activation(out=sgt[:, 0:n], in_=bg[:, 0:n], func=AF.Sigmoid),
                                  [r_bg], [r_sgt])
                            us, r_us = ust.next()
                            fw.op("dve", lambda e, ba=ba, us=us: e.tensor_tensor(out=us[:, 0:n], in0=ba[:, 0:n], in1=sgt[:, 0:n],
                                                                               op=ALU.mult), [r_ba, r_sgt], [r_us])
                            if halo:
                                fw.op("dve", lambda e, us=us: e.tensor_tensor(out=us[:, 0:n], in0=us[:, 0:n], in1=hm[:, 0:n],
                                                                               op=ALU.mult), [r_us, r_hm], [r_us])
                                fw.op("pool", lambda e, us=us, cc=cc: [
                                    e.dma_start(out=uT_d[cc, :, 0:HALO], in_=us[:, 0:HALO]),
                                    e.dma_start(out=uT_d[cc, :, HALO + OWN:UW], in_=us[:, HALO:2 * HALO])],
                                    [r_us], [], dma=2, sem_res=r_us)
                            else:
                                store(fw, "pool", uT_d[cc, :, ucol0:ucol0 + n], us[:, 0:n], r_us)
                if G["full"]:
                    o0 = G["own0"]
                    for gb in range(32):
                        wt, r_wt = load_w(GATE_OFF + gb * 256)
                        for sub in range(2):
                            ch = gb * 2 + sub
                            bk, r_bk = next_bank()
                            proj_fm(wt, r_wt, sub, n, bk, r_bk)
                            gs, r_gs = gst.next()
                            fw.op("act", lambda e, bk=bk, gs=gs: e.activation(out=gs[:, 0:n], in_=bk[:, 0:n], func=AF.Sigmoid),
                                  [r_bk], [r_gs])
                            store(fw, "pool", gates_d[ch, :, o0:o0 + n], gs[:, 0:n], r_gs)

        for G in groups:
            do_group(G)
        if debug:
            dbg["KT"] = _dump(nc, fw, "dbg_KT", KT, r_KT, [128, NKVH, NKEY], BF16)
            dbg["VA"] = _dump(nc, fw, "dbg_VA", VA, r_VA, [128, NKT, NKVH, HD + 2], BF16)
        fw.end_phase()
        if stop_after == 1:
            kv_stack.close()
            return _finish(nc, fw, dbg, out, r_out)

        fw.begin_phase()
        qT, r_qT = fw.buf("qT", [128, NQH, 512], BF16)
        pring = fw.ring("pexp", 3, [128, 512], BF16)
        atm = fw.ring("atm", 2, [128, 128], BF16)
        rden = fw.ring("rden", 2, [128, 1], F32)
        aT, r_aT = fw.buf("aT", [128, NQH, 512], BF16)
        uin = fw.ring("uin", 2, [128, 512 + 2 * HALO], F32)
        yc, r_yc = fw.buf("yc", [128, 16, 512], F32)
        ysq, r_ysq = fw.buf("ysq", [128, 512], F32)
        mean, r_mean = fw.buf("mean", [128, 512], F32)
        var, r_var = fw.buf("var", [128, 512], F32)
        zt, r_zt = fw.buf("zt", [128, 512], F32)
        cst = fw.ring("cst", 2, [128, 512], BF16)
        cw, r_cw = fw.buf("cw", [128, 16, TAPS], F32)
        cbv, r_cbv = fw.buf("cbv", [128, 16], F32)
        lng, r_lng = fw.buf("lng", [128, 16], F32)
        lnb, r_lnb = fw.buf("lnb", [128, 16], F32)
        dma(fw, "sp", cw[:], cw_in, [], [r_cw])
        dma(fw, "sp", cbv[:], cb_in, [], [r_cbv])
        dma(fw, "sp", lng[:], lng_in, [], [r_lng])
        dma(fw, "sp", lnb[:], lnb_in, [], [r_lnb])
        SCALE = float(HD) ** -0.5
        s_banks = [banks[0], banks[1]]
        o_banks = [(banks[2], banks[3]), (banks[4], banks[5])]
        tr_bank = banks[6]
        st_bank = banks[7]

        def conv_chunk(cc, o0):
            ui, r_ui = uin.next()
            dma(fw, "sp", ui[:], uT_d[cc, :, o0:o0 + 512 + 2 * HALO], [], [r_ui])
            fw.op("dve", lambda e, ui=ui, cc=cc: e.tensor_scalar(
                out=yc[:, cc, :], in0=ui[:, 1:513], scalar1=cw[:, cc, 0:1], scalar2=cbv[:, cc:cc + 1],
                op0=ALU.mult, op1=ALU.add), [r_ui, r_cw, r_cbv], [r_yc])
            for k in range(1, TAPS):
                fw.op("dve", lambda e, ui=ui, cc=cc, k=k: e.scalar_tensor_tensor(
                    out=yc[:, cc, :], in0=ui[:, k + 1:k + 513], scalar=cw[:, cc, k:k + 1], in1=yc[:, cc, :],
                    op0=ALU.mult, op1=ALU.add), [r_ui, r_cw, r_yc], [r_yc])

        for g in range(4):
            o0 = g * 512
            dma(fw, "sp", qT[:], qT_d[:, :, o0:o0 + 512].rearrange("h p t -> p h t"), [], [r_qT])
            for h in range(NQH):
                kvh = h // (NQH // NKVH)
                ob = o_banks[h % 2]
                pend = None
                for kc in range(NKT + 1):
                    if kc < NKT:
                        sb_, r_sb = s_banks[kc % 2]
                        mm_group(fw, sb_[:, :], [(KT[:, kvh, kc * 128:(kc + 1) * 128], qT[:, h, :])], [r_KT, r_qT], [r_sb])
                        pb, r_pb = pring.next()
                        fw.op("act", lambda e, sb_=sb_, pb=pb: e.activation(out=pb[:], in_=sb_[:, :], func=AF.Exp,
                                                                             scale=SCALE), [r_sb], [r_pb])
                        cur = (kc, pb, r_pb)
                    else:
                        cur = None
                    if pend is not None:
                        pkc, ppb, r_ppb = pend

                        def fn(e, pkc=pkc, ppb=ppb, kvh=kvh, ob=ob):
                            last = None
                            for sub in range(4):
                                bk = ob[sub // 2][0]
                                c0 = (sub % 2) * 256
                                last = e.matmul(bk[:, c0:c0 + HD + 1], lhsT=ppb[:, sub * 128:(sub + 1) * 128],
                                                rhs=VA[:, pkc, kvh, 0:HD + 1], start=(pkc == 0), stop=(pkc == NKT - 1))
                            return last
                        fw.op("pe", fn, [r_ppb, r_VA], [ob[0][1], ob[1][1]])
                    pend = cur
                for sub in range(4):
                    bk, r_bk = ob[sub // 2]
                    c0 = (sub % 2) * 256
                    rd, r_rd = rden.next()
                    fw.op("dve", lambda e, bk=bk, c0=c0, rd=rd: e.reciprocal(out=rd[:], in_=bk[:, c0 + HD:c0 + HD + 1]),
                          [r_bk], [r_rd])
                    am, r_am = atm.next()
                    fw.op("dve", lambda e, bk=bk, c0=c0, rd=rd, am=am: e.tensor_scalar(
                        out=am[:], in0=bk[:, c0:c0 + HD], scalar1=rd[:, 0:1], scalar2=None, op0=ALU.mult),
                        [r_bk, r_rd], [r_am])
                    tb, r_tb = tr_bank
                    tbv = tb[:].bitcast(BF16)
                    fw.op("pe", lambda e, am=am, tbv=tbv: e.transpose(tbv[:, 0:128], am[:], identb[:]),
                          [r_am, r_identb], [r_tb])
                    fw.op("act", lambda e, tbv=tbv, h=h, sub=sub: e.activation(
                        out=aT[:, h, sub * 128:(sub + 1) * 128], in_=tbv[:, 0:128], func=AF.Identity), [r_tb], [r_aT])
                conv_chunk(h, o0)
            store(fw, "pool", attnT_d[:, :, o0:o0 + 512].rearrange("h p t -> p h t"), aT[:], r_aT)

            sbk, r_sbk = st_bank
            mm_group(fw, sbk[:, :], [(ones_f[:], yc[:, cc, :]) for cc in range(16)], [r_ones_f, r_yc], [r_sbk])
            fw.op("act", lambda e, sbk=sbk: e.activation(out=mean[:], in_=sbk[:, :], func=AF.Identity, scale=1.0 / CW),
                  [r_sbk], [r_mean])
            for cc in range(16):
                fw.op("dve", lambda e, cc=cc: e.tensor_tensor(out=yc[:, cc, :], in0=yc[:, cc, :], in1=mean[:], op=ALU.subtract),
                      [r_yc, r_mean], [r_yc])
            for cc in range(16):
                fw.op("act", lambda e, cc=cc: e.activation(out=ysq[:], in_=yc[:, cc, :], func=AF.Square), [r_yc], [r_ysq])
                fw.op("pe", lambda e, cc=cc, sbk=sbk: e.matmul(sbk[:, :], lhsT=ones_f[:], rhs=ysq[:], start=(cc == 0),
                                                            stop=(cc == 15)), [r_ones_f, r_ysq], [r_sbk])
            fw.op("act", lambda e, sbk=sbk: e.activation(out=var[:], in_=sbk[:, :], func=AF.Sqrt, scale=1.0 / CW,
                                                         bias=epst[:, 0:1]), [r_sbk, r_eps], [r_var])
            fw.op("dve", lambda e: e.reciprocal(out=var[:], in_=var[:]), [r_var], [r_var])
            for cc in range(16):
                fw.op("dve", lambda e, cc=cc: e.tensor_tensor(out=zt[:], in0=yc[:, cc, :], in1=var[:], op=ALU.mult),
                      [r_yc, r_var], [r_zt])
                cs, r_cs = cst.next()
                fw.op("act", lambda e, cc=cc, cs=cs: e.activation(out=cs[:], in_=zt[:], func=AF.Silu,
                                                                 scale=lng[:, cc:cc + 1], bias=lnb[:, cc:cc + 1]),
                      [r_zt, r_lng, r_lnb], [r_cs])
                store(fw, "pool", csT_d[cc, :, o0:o0 + 512], cs[:], r_cs)
        fw.end_phase()
        kv_stack.close()
        if stop_after == 2:
            return _finish(nc, fw, dbg, out, r_out)

        fw.begin_phase()
        aT, r_aT = fw.buf("aT3", [128, 16, 512], BF16)
        cT, r_cT = fw.buf("cT3", [128, 16, 512], BF16)
        mT, r_mT = fw.buf("mT", [128, KC, 512], BF16)
        wring = fw.ring("wb3", 3, [128, 8192], BF16)
        gin = fw.ring("gin", 4, [128, 512], F32)
        tm1, r_tm1 = fw.buf("tm1", [128, 512], F32)
        tm2, r_tm2 = fw.buf("tm2", [128, 512], F32)
        xin = fw.ring("xin", 2, [128, 4, 256], F32)
        xo = fw.ring("xo", 2, [128, 4, 256], F32)
        gar = fw.ring("gar", 2, [128, 256], F32)
        for g in range(4):
            o0 = g * 512
            dma(fw, "sp", aT[:], attnT_d[:, :, o0:o0 + 512].rearrange("h p t -> p h t"), [], [r_aT])
            dma(fw, "sp", cT[:], csT_d[:, :, o0:o0 + 512].rearrange("h p t -> p h t"), [], [r_cT])
            for ob4 in range(8):
                wa, r_wa = wring.next()
                wav = wa[:].rearrange("p (k n) -> p k n", k=16)
                dma(fw, "pool", wav, w_ao_v[:, :, ob4 * 512:(ob4 + 1) * 512], [], [r_wa])
                wc, r_wc = wring.next()
                wcv = wc[:].rearrange("p (k n) -> p k n", k=16)
                dma(fw, "pool", wcv, w_co_v[:, :, ob4 * 512:(ob4 + 1) * 512], [], [r_wc])
                for sub in range(4):
                    oc = ob4 * 4 + sub
                    ba, r_ba = banks[(oc * 2) % 4]
                    bc, r_bc = banks[(oc * 2 + 1) % 4]
                    mm_group(fw, ba[:, :], [(wav[:, kc, sub * 128:(sub + 1) * 128], aT[:, kc, :]) for kc in range(16)],
                             [r_wa, r_aT], [r_ba])
                    mm_group(fw, bc[:, :], [(wcv[:, kc, sub * 128:(sub + 1) * 128], cT[:, kc, :]) for kc in range(16)],
                             [r_wc, r_cT], [r_bc])
                    ga_, r_ga = gin.next()
                    gc_, r_gc = gin.next()
                    dma(fw, "sp", ga_[:], gates_d[oc, :, o0:o0 + 512], [], [r_ga])
                    dma(fw, "sp", gc_[:], gates_d[32 + oc, :, o0:o0 + 512], [], [r_gc])
                    fw.op("dve", lambda e, ba=ba, ga_=ga_: e.tensor_tensor(out=tm1[:], in0=ba[:, :], in1=ga_[:], op=ALU.mult),
                          [r_ba, r_ga], [r_tm1])
                    fw.op("dve", lambda e, bc=bc, gc_=gc_: e.tensor_tensor(out=tm2[:], in0=bc[:, :], in1=gc_[:], op=ALU.mult),
                          [r_bc, r_gc], [r_tm2])
                    fw.op("dve", lambda e, oc=oc: e.tensor_tensor(out=mT[:, oc, :], in0=tm1[:], in1=tm2[:], op=ALU.add),
                          [r_tm1, r_tm2], [r_mT])
            for nb in range(16):
                wo, r_wo = wring.next()
                wov = wo[:].rearrange("p (k n) -> p k n", k=KC)
                dma(fw, "pool", wov, w_out_v[:, :, nb * 256:(nb + 1) * 256], [], [r_wo])
                xi, r_xi = xin.next()
                dma(fw, "sp", xi[:], x_own[o0:o0 + 512, nb * 256:(nb + 1) * 256].rearrange("(t p) n -> p t n", p=128),
                    [], [r_xi])
                gr, r_gr = gar.next()
                dma(fw, "sp", gr[:], mod_all[0:1, 2 * D + nb * 256:2 * D + (nb + 1) * 256].broadcast_to([128, 256]),
                    [], [r_gr])
                xo_, r_xo = xo.next()
                for tt in range(4):
                    bk, r_bk = banks[4 + (nb * 4 + tt) % 4]
                    mm_group(fw, bk[:, 0:256], [(mT[:, kc, tt * 128:(tt + 1) * 128], wov[:, kc, :]) for kc in range(KC)],
                             [r_wo, r_mT], [r_bk])
                    fw.op("dve", lambda e, bk=bk, tt=tt, gr=gr, xo_=xo_: e.tensor_tensor(
                        out=xo_[:, tt, :], in0=bk[:, 0:256], in1=gr[:], op=ALU.mult), [r_bk, r_gr], [r_xo])
                    fw.op("dve", lambda e, tt=tt, xi=xi, xo_=xo_: e.tensor_tensor(
                        out=xo_[:, tt, :], in0=xo_[:, tt, :], in1=xi[:, tt, :], op=ALU.add), [r_xo, r_xi], [r_xo])
                store(fw, "act", xnew_d[o0:o0 + 512, nb * 256:(nb + 1) * 256].rearrange("(t p) n -> p t n", p=128), xo_[:],
                      r_xo)
        fw.end_phase()
        if stop_after == 3:
            return _finish(nc, fw, dbg, out, r_out)

        fw.begin_phase()
        sc = nt_scratch()
        xring = fw.ring("xrow4", 2, [128, D], F32)
        h2T, r_h2T = fw.buf("h2T", [128, KC, 128], F32)
        wr, r_wr = fw.buf("wr", [128, KC, 36], F32)
        brr, r_brr = fw.buf("brr", [128, 36], F32)
        iot, r_iot = fw.buf("iot", [128, NE], F32)
        tril, r_tril = fw.buf("tril", [128, 128], F32)
        trilb, r_trilb = fw.buf("trilb", [128, 128], BF16)
        tke, r_tke = fw.buf("tke", [128, 16, 2, 2], I32)
        Mb, r_Mb = fw.buf("Mb", [128, 16, NE], BF16)
        dma(fw, "sp", wr[:], w_r_v, [], [r_wr])
        dma(fw, "sp", brr[:], b_r_rep, [], [r_brr])
        dma(fw, "sp", iot[:], iota_cap, [], [r_iot])
        dma(fw, "sp", tril[:], tril_in, [], [r_tril])
        dma(fw, "sp", tke[:], tok_ent, [], [r_tke])
        fw.op("dve", lambda e: e.tensor_copy(out=trilb[:], in_=tril[:]), [r_tril], [r_trilb])

        def sbuf1(name, shape, dt=F32):
            return fw.buf(name, shape, dt)
        L, r_L = sbuf1("L", [128, 36])
        gmax, r_gmax = sbuf1("gmax", [128, 1])
        ngmax, r_ngmax = sbuf1("ngmax", [128, 1])
        gmask, r_gmask = sbuf1("gmask", [128, 4])
        gexp, r_gexp = sbuf1("gexp", [128, 4])
        gsum, r_gsum = sbuf1("gsum", [128, 1])
        pg, r_pg = sbuf1("pg", [128, 1])
        pen, r_pen = sbuf1("pen", [128, 4])
        em, r_em = sbuf1("em", [128, NE])
        em2, r_em2 = sbuf1("em2", [128, NE])
        m1, r_m1 = sbuf1("m1", [128, 1])
        m2, r_m2 = sbuf1("m2", [128, 1])
        mk1, r_mk1 = sbuf1("mk1", [128, NE])
        mk2, r_mk2 = sbuf1("mk2", [128, NE])
        dd, r_dd = sbuf1("dd", [128, 1])
        e2, r_e2 = sbuf1("e2", [128, 1])
        wts, r_wts = sbuf1("wts", [128, 2])
        Msum, r_Msum = sbuf1("Msum", [128, NE])
        cum, r_cum = sbuf1("cum", [128, NE])
        tmp32, r_tmp32 = sbuf1("tmp32", [128, NE])
        pos, r_pos = sbuf1("pos", [128, 2])
        eb, r_eb = sbuf1("eb", [128, 2])
        ovf, r_ovf = sbuf1("ovf", [128, 2])
        dstf, r_dstf = sbuf1("dstf", [128, 2])
        dring = fw.ring("dsti", 2, [128, 2], I32)
        wring2 = fw.ring("wts2", 2, [128, 4], F32)

        for tt in range(16):
            g = tt // 4
            xr, r_xr = xring.next()
            dma(fw, "sp", xr[:], xnew_d[tt * 128:(tt + 1) * 128, :], [], [r_xr])
            norm_transpose(xr, r_xr, 128, A2, SH2, lambda kc: h2T[:, kc, :], r_h2T, sc, banks[4:8])
            lb, r_lb = banks[tt % 2]
            mm_group(fw, lb[:, 0:36], [(h2T[:, kc, :], wr[:, kc, :]) for kc in range(KC)], [r_h2T, r_wr], [r_lb])
            V = "dve"
            fw.op(V, lambda e, lb=lb: e.tensor_tensor(out=L[:], in0=lb[:, 0:36], in1=brr[:], op=ALU.add), [r_lb, r_brr], [r_L])
            fw.op(V, lambda e: e.tensor_reduce(out=gmax[:], in_=L[:, 0:4], axis=AX.X, op=ALU.max), [r_L], [r_gmax])
            fw.op(V, lambda e: e.tensor_scalar(out=gmask[:], in0=L[:, 0:4], scalar1=gmax[:, 0:1], scalar2=None,
                                               op0=ALU.is_equal), [r_L, r_gmax], [r_gmask])
            fw.op(V, lambda e: e.tensor_scalar(out=ngmax[:], in0=gmax[:], scalar1=-1.0, scalar2=None, op0=ALU.mult),
                  [r_gmax], [r_ngmax])
            fw.op("act", lambda e: e.activation(out=gexp[:], in_=L[:, 0:4], func=AF.Exp, bias=ngmax[:, 0:1],
                                                accum_out=gsum[:]), [r_L, r_ngmax], [r_gexp, r_gsum])
            fw.op(V, lambda e: e.reciprocal(out=pg[:], in_=gsum[:]), [r_gsum], [r_pg])
            fw.op(V, lambda e: e.tensor_scalar(out=pen[:], in0=gmask[:], scalar1=-1.0, scalar2=1e30, op0=ALU.add,
                                               op1=ALU.mult), [r_gmask], [r_pen])
            fw.op(V, lambda e: e.tensor_tensor(out=em[:].rearrange("p (g k) -> p g k", g=4),
                                               in0=L[:, 4:36].rearrange("p (g k) -> p g k", g=4),
                                               in1=pen[:].unsqueeze(2).broadcast_to([128, 4, 8]), op=ALU.add),
                  [r_L, r_pen], [r_em])
            fw.op(V, lambda e: e.tensor_reduce(out=m1[:], in_=em[:], axis=AX.X, op=ALU.max), [r_em], [r_m1])
            fw.op(V, lambda e: e.tensor_scalar(out=mk1[:], in0=em[:], scalar1=m1[:, 0:1], scalar2=None, op0=ALU.is_equal),
                  [r_em, r_m1], [r_mk1])
            fw.op(V, lambda e: e.scalar_tensor_tensor(out=em2[:], in0=mk1[:], scalar=-1e30, in1=em[:], op0=ALU.mult,
                                                      op1=ALU.add), [r_mk1, r_em], [r_em2])
            fw.op(V, lambda e: e.tensor_reduce(out=m2[:], in_=em2[:], axis=AX.X, op=ALU.max), [r_em2], [r_m2])
            fw.op(V, lambda e: e.tensor_scalar(out=mk2[:], in0=em2[:], scalar1=m2[:, 0:1], scalar2=None, op0=ALU.is_equal),
                  [r_em2, r_m2], [r_mk2])
            fw.op(V, lambda e: e.tensor_tensor(out=dd[:], in0=m2[:], in1=m1[:], op=ALU.subtract), [r_m1, r_m2], [r_dd])
            fw.op("act", lambda e: e.activation(out=e2[:], in_=dd[:], func=AF.Exp), [r_dd], [r_e2])
            wt2, r_wt2 = wring2.next()
            fw.op(V, lambda e: e.tensor_scalar(out=e2[:], in0=e2[:], scalar1=1.0, scalar2=None, op0=ALU.add), [r_e2], [r_e2])
            fw.op(V, lambda e: e.reciprocal(out=e2[:], in_=e2[:]), [r_e2], [r_e2])
            fw.op(V, lambda e, wt2=wt2: e.memset(wt2[:], 0.0), [], [r_wt2])
            fw.op(V, lambda e, wt2=wt2: e.tensor_tensor(out=wt2[:, 0:1], in0=e2[:], in1=pg[:], op=ALU.mult), [r_e2, r_pg], [r_wt2])
            fw.op(V, lambda e, wt2=wt2: e.tensor_tensor(out=wt2[:, 2:3], in0=pg[:], in1=wt2[:, 0:1], op=ALU.subtract),
                  [r_pg, r_wt2], [r_wt2])
            fw.op(V, lambda e: e.tensor_tensor(out=Msum[:], in0=mk1[:], in1=mk2[:], op=ALU.add), [r_mk1, r_mk2], [r_Msum])
            fw.op(V, lambda e, tt=tt: e.tensor_copy(out=Mb[:, tt, :], in_=Msum[:]), [r_Msum], [r_Mb])
            cb_, r_cb = banks[2 + tt % 2]
            pairs = [(trilb[:], Mb[:, tt, :])] + [(ones_b[:], Mb[:, j, :]) for j in range(tt)]
            mm_group(fw, cb_[:, 0:NE], pairs, [r_trilb, r_ones_b, r_Mb], [r_cb])
            fw.op(V, lambda e, cb_=cb_: e.tensor_copy(out=cum[:], in_=cb_[:, 0:NE]), [r_cb], [r_cum])
            for k, (mk, r_mk) in enumerate(((mk1, r_mk1), (mk2, r_mk2))):
                fw.op(V, lambda e, mk=mk: e.tensor_tensor(out=tmp32[:], in0=mk[:], in1=cum[:], op=ALU.mult), [r_mk, r_cum], [r_tmp32])
                fw.op(V, lambda e, k=k: e.tensor_reduce(out=pos[:, k:k + 1], in_=tmp32[:], axis=AX.X, op=ALU.add), [r_tmp32], [r_pos])
                fw.op(V, lambda e, mk=mk: e.tensor_tensor(out=tmp32[:], in0=mk[:], in1=iot[:], op=ALU.mult), [r_mk, r_iot], [r_tmp32])
                fw.op(V, lambda e, k=k: e.tensor_reduce(out=eb[:, k:k + 1], in_=tmp32[:], axis=AX.X, op=ALU.add), [r_tmp32], [r_eb])
            fw.op(V, lambda e: e.tensor_scalar(out=ovf[:], in0=pos[:], scalar1=float(CAP) - 0.5, scalar2=1.0e6, op0=ALU.is_gt,
                                               op1=ALU.mult), [r_pos], [r_ovf])
            fw.op(V, lambda e: e.tensor_tensor(out=dstf[:], in0=pos[:], in1=eb[:], op=ALU.add), [r_pos, r_eb], [r_dstf])
            fw.op(V, lambda e: e.tensor_tensor(out=dstf[:], in0=dstf[:], in1=ovf[:], op=ALU.add), [r_dstf, r_ovf], [r_dstf])
            di, r_di = dring.next()
            fw.op(V, lambda e, di=di: e.tensor_copy(out=di[:], in_=dstf[:]), [r_dstf], [r_di])
            for k in range(2):
                fw.op("pool", lambda e, di=di, k=k, tt=tt: [e.indirect_dma_start(
                    out=tab_i, out_offset=bass.IndirectOffsetOnAxis(ap=di[:, k:k + 1], axis=0),
                    in_=tke[:, tt, k, :], in_offset=None, bounds_check=fw.bc_reg(e, NE * CAP - 1), oob_is_err=False)],
                    [r_di, r_tke], [r_tab_i], dma=1)
                fw.op("pool", lambda e, di=di, k=k, wt2=wt2: [e.indirect_dma_start(
                    out=tab_w, out_offset=bass.IndirectOffsetOnAxis(ap=di[:, k:k + 1], axis=0),
                    in_=wt2[:, 2 * k:2 * k + 2], in_offset=None, bounds_check=fw.bc_reg(e, NE * CAP - 1), oob_is_err=False)],
                    [r_di, r_wt2], [r_tab_w], dma=1)
            if tt % 4 == 3:
                fw.emit()
        fw.end_phase()
        if stop_after == 4:
            return _finish(nc, fw, dbg, out, r_out)

        fw.begin_phase()
        sc = nt_scratch()
        xg = fw.ring("xg", 2, [128, D], F32)
        gT, r_gT = fw.buf("gT", [128, KC, CAP], BF16)
        actT, r_actT = fw.buf("actT", [128, FF // 128, CAP], BF16)
        wring = fw.ring("wbC", 3, [128, 8192], BF16)
        idxr = fw.ring("idx", 2 * NST, [128, 2], I32)
        wtr = fw.ring("wtc", 2 * NST, [128, 2], F32)
        sgr = fw.ring("sgr", 2, [128, CAP], F32)
        ypc = fw.ring("ypc", 3, [128, 1024], F32)
        for ex in range(NE):
            idxs = []
            for stl in range(NST):
                ix, r_ix = idxr.next()
                w1, r_w1 = wtr.next()
                r0 = ex * CAP + stl * 128
                dma(fw, "sp", ix[:], tab_i[r0:r0 + 128, :], [r_tab_i], [r_ix])
                dma(fw, "sp", w1[:], tab_w[r0:r0 + 128, :], [r_tab_w], [r_w1])
                idxs.append((ix, r_ix, w1, r_w1))
                xr, r_xr = xg.next()
                fw.op("pool", lambda e, xr=xr, ix=ix: [e.indirect_dma_start(
                    out=xr[:], out_offset=None, in_=xnew_d,
                    in_offset=bass.IndirectOffsetOnAxis(ap=ix[:, 0:1], axis=0))],
                    [r_ix], [r_xr], dma=1)
                norm_transpose(xr, r_xr, 128, A2, SH2, lambda kc, stl=stl: gT[:, kc, stl * 128:(stl + 1) * 128], r_gT,
                               sc, banks[4:8])
            for fb in range(4):
                wg_, r_wg = wring.next()
                wgv = wg_[:].rearrange("p (k n) -> p k n", k=KC)
                dma(fw, "pool", wgv, w_eg[ex].rearrange("(kc p) n -> p kc n", p=128)[:, :, fb * 256:(fb + 1) * 256], [], [r_wg])
                wu_, r_wu = wring.next()
                wuv = wu_[:].rearrange("p (k n) -> p k n", k=KC)
                dma(fw, "pool", wuv, w_eu[ex].rearrange("(kc p) n -> p kc n", p=128)[:, :, fb * 256:(fb + 1) * 256], [], [r_wu])
                for sub in range(2):
                    fc = fb * 2 + sub
                    bg, r_bg = banks[(fc * 2) % 4]
                    bu, r_bu = banks[(fc * 2 + 1) % 4]
                    mm_group(fw, bg[:, 0:CAP], [(wgv[:, kc, sub * 128:(sub + 1) * 128], gT[:, kc, :]) for kc in range(KC)],
                             [r_wg, r_gT], [r_bg])
                    mm_group(fw, bu[:, 0:CAP], [(wuv[:, kc, sub * 128:(sub + 1) * 128], gT[:, kc, :]) for kc in range(KC)],
                             [r_wu, r_gT], [r_bu])
                    sg_, r_sg_ = sgr.next()
                    fw.op("act", lambda e, bg=bg, sg_=sg_: e.activation(out=sg_[:], in_=bg[:, 0:CAP], func=AF.Silu), [r_bg], [r_sg_])
                    fw.op("dve", lambda e, bu=bu, sg_=sg_, fc=fc: e.tensor_tensor(out=actT[:, fc, :], in0=bu[:, 0:CAP], in1=sg_[:],
                                                                                op=ALU.mult), [r_bu, r_sg_], [r_actT])
            for db in range(4):
                wd_, r_wd = wring.next()
                wdv = wd_[:].rearrange("p (k n) -> p k n", k=FF // 128)
                dma(fw, "pool", wdv, w_ed[ex].rearrange("(kc p) n -> p kc n", p=128)[:, :, db * 1024:(db + 1) * 1024], [], [r_wd])
                for stl in range(NST):
                    ix, r_ix, w1, r_w1 = idxs[stl]
                    yp, r_yp = ypc.next()
                    for hf in range(2):
                        bk, r_bk = banks[4 + (db * 2 * NST + stl * 2 + hf) % 4]
                        mm_group(fw, bk[:, :], [(actT[:, kc, stl * 128:(stl + 1) * 128], wdv[:, kc, hf * 512:(hf + 1) * 512])
                                               for kc in range(FF // 128)], [r_wd, r_actT], [r_bk])
                        if hf == 0:
                            fw.op("act", lambda e, bk=bk, yp=yp, w1=w1: e.activation(
                                out=yp[:, 0:512], in_=bk[:, :], func=AF.Identity, scale=w1[:, 0:1]), [r_bk, r_w1], [r_yp])
                        else:
                            fw.op("dve", lambda e, bk=bk, yp=yp, w1=w1: e.tensor_scalar(
                                out=yp[:, 512:1024], in0=bk[:, :], scalar1=w1[:, 0:1], scalar2=None, op0=ALU.mult),
                                [r_bk, r_w1], [r_yp])
                    fw.op("pool", lambda e, yp=yp, ix=ix, db=db: [e.indirect_dma_start(
                        out=ybufs[db], out_offset=bass.IndirectOffsetOnAxis(ap=ix[:, 1:2], axis=0),
                        in_=yp[:], in_offset=None, bounds_check=fw.bc_reg(e, 2 * OWN - 1), oob_is_err=False)],
                        [r_yp, r_ix], [], dma=1, sem_res=r_yp)
            if ex % 4 == 3:
                fw.emit()
        fw.end_phase()
        if stop_after == 5:
            return _finish(nc, fw, dbg, out, r_out)

        fw.begin_phase()
        ga2, r_ga2 = fw.buf("ga2", [128, D], F32)
        gfr, r_gfr = fw.buf("gfr", [128, D], F32)
        dma(fw, "sp", ga2[:], mod_all[0:1, 5 * D:6 * D].broadcast_to([128, D]), [], [r_ga2])
        dma(fw, "sp", gfr[:], gf_rep_in, [], [r_gfr])
        xr_ = fw.ring("xd", 2, [128, D], F32)
        y1r = fw.ring("y1", 2, [128, D], F32)
        y2r = fw.ring("y2", 2, [128, D], F32)
        junk, r_junk = fw.buf("junkd", [128, D], BF16)
        ssr = fw.ring("ssd", 2, [128, 1], F32)
        for tt in range(16):
            xr, r_xr = xr_.next()
            y1, r_y1 = y1r.next()
            y2, r_y2 = y2r.next()
            dma(fw, "sp", xr[:], xnew_d[tt * 128:(tt + 1) * 128, :], [], [r_xr])
            fw.op("sp", lambda e, y1=y1, tt=tt: [e.dma_start(out=y1[:, i * 1024:(i + 1) * 1024],
                                                             in_=ybufs[i][tt * 128:(tt + 1) * 128, :]) for i in range(4)],
                  [], [r_y1], dma=4)
            fw.op("sp", lambda e, y2=y2, tt=tt: [e.dma_start(out=y2[:, i * 1024:(i + 1) * 1024],
                                                             in_=ybufs[i][OWN + tt * 128:OWN + (tt + 1) * 128, :]) for i in range(4)],
                  [], [r_y2], dma=4)
            fw.op("pool", lambda e, y1=y1, y2=y2: e.tensor_tensor(out=y1[:], in0=y1[:], in1=y2[:], op=ALU.add), [r_y1, r_y2], [r_y1])
            fw.op("dve", lambda e, y1=y1: e.tensor_tensor(out=y1[:], in0=y1[:], in1=ga2[:], op=ALU.mult), [r_y1, r_ga2], [r_y1])
            fw.op("pool", lambda e, y1=y1, xr=xr: e.tensor_tensor(out=xr[:], in0=xr[:], in1=y1[:], op=ALU.add), [r_y1, r_xr], [r_xr])
            ss, r_ss = ssr.next()
            fw.op("act", lambda e, xr=xr, ss=ss: e.activation(out=junk[:], in_=xr[:], func=AF.Square, accum_out=ss[:]),
                  [r_xr], [r_junk, r_ss])
            fw.op("act", lambda e, ss=ss: e.activation(out=ss[:], in_=ss[:], func=AF.Sqrt, scale=1.0 / D, bias=epst[:, 0:1]),
                  [r_ss, r_eps], [r_ss])
            fw.op("dve", lambda e, ss=ss: e.reciprocal(out=ss[:], in_=ss[:]), [r_ss], [r_ss])
            fw.op("dve", lambda e, xr=xr, ss=ss, y2=y2: e.scalar_tensor_tensor(
                out=y2[:], in0=xr[:], scalar=ss[:, 0:1], in1=gfr[:], op0=ALU.mult, op1=ALU.mult), [r_xr, r_ss, r_gfr, r_y2], [r_y2])
            store(fw, "pool", out[tt * 128:(tt + 1) * 128, :], y2[:], r_y2)
        fw.end_phase()
        if debug:
            fw.begin_phase()
            def ddump(name, src, shape, dt):
                d = nc.dram_tensor(name, list(shape), dt, kind="ExternalOutput").ap()
                dma(fw, "sp", d, src, [], [fw.res(name, True)])
            ddump("dbg_qT", qT_d[:, :, 0:512], [NQH, 128, 512], BF16)
            ddump("dbg_uT", uT_d[0:2], [2, 128, UW], F32)
            ddump("dbg_gates", gates_d[0:2, :, 0:512], [2, 128, 512], F32)
            ddump("dbg_attnT", attnT_d[:, :, 0:512], [16, 128, 512], BF16)
            ddump("dbg_csT", csT_d[:, :, 0:512], [16, 128, 512], BF16)
            ddump("dbg_xnew", xnew_d[0:256, :], [256, D], F32)
            ddump("dbg_tab_i", tab_i, [NE * CAP, 2], I32)
            ddump("dbg_tab_w", tab_w, [NE * CAP, 2], F32)
            ddump("dbg_ybuf0", ybufs[0][0:128, :], [128, 1024], F32)
            ddump("dbg_ybuf1", ybufs[0][OWN:OWN + 128, :], [128, 1024], F32)
            fw.end_phase()
        return _finish(nc, fw, dbg, out, r_out)


def _dump(nc, fw, name, t, r_t, shape, dt):
    d = nc.dram_tensor(name, list(shape), dt, kind="ExternalOutput").ap()
    r_d = fw.res(name, True)
    dma(fw, "sp", d, t[:], [r_t], [r_d])
    return d


def _finish(nc, fw, dbg, out, r_out):
    return nc


def _rope_tables(hf):
    inv = (10000.0 ** (-np.arange(0, 64, 2, dtype=np.float32) / 64.0)).astype(np.float32)
    tok = np.concatenate([np.arange(hf * OWN, (hf + 1) * OWN), np.arange((1 - hf) * OWN, (2 - hf) * OWN)])
    row = (tok // 64).astype(np.float32)
    col = (tok % 64).astype(np.float32)
    C = np.ones((128, NKEY), np.float32)
    S = np.zeros((128, NKEY), np.float32)
    for d in range(128):
        a = d // 64
        r = d % 64
        j = r % 32
        half = r // 32
        pos = row if a == 0 else col
        ang = (pos * inv[j]).astype(np.float32)
        C[d, :SEQ] = np.cos(ang)
        S[d, :SEQ] = np.sin(ang) * (-1.0 if half == 0 else 1.0)
    return C, S


def _consts():
    ident = np.eye(128, dtype=np.float32)
    pm = np.zeros((128, 128), np.float32)
    for m in range(128):
        r = m % 64
        partner = m + 32 if (r // 32) == 0 else m - 32
        pm[partner, m] = 1.0
    tril = np.zeros((128, 128), np.float32)
    for k in range(128):
        tril[k, k + 1:] = 1.0
    iota_cap = np.tile((np.arange(NE, dtype=np.float32) * CAP)[None, :], (128, 1))
    tok_ent = np.zeros((128, 16, 2, 2), np.int32)
    for tt in range(16):
        t = tt * 128 + np.arange(128)
        for k in range(2):
            tok_ent[:, tt, k, 0] = t
            tok_ent[:, tt, k, 1] = k * OWN + t
    tab_i_init = np.zeros((NE * CAP, 2), np.int32)
    tab_i_init[:, 1] = 1 << 24
    tab_w_init = np.zeros((NE * CAP, 2), np.float32)
    return dict(ident=ident, pmat=pm, tril=tril, iota_cap=iota_cap, tok_ent=tok_ent,
                tab_i_init=tab_i_init, tab_w_init=tab_w_init)


_NC_CACHE = {}


def kernel(x, c, ctx, c_ctx, norm1_g, w_mod, b_mod, w_in, q_norm_g, k_norm_g, w_attn_out,
           conv_dw_w, conv_dw_b, conv_ln_g, conv_ln_b, w_conv_out, w_out, norm2_g,
           w_router_group, b_router_group, w_router_expert, b_router_expert,
           w_exp_gate, w_exp_up, w_exp_down, norm_f_g, _debug=False, _stop_after=None):
    f = lambda a: np.ascontiguousarray(np.asarray(a, dtype=np.float32))
    x, c, ctx, c_ctx = f(x), f(c), f(ctx), f(c_ctx)
    fm = lambda v: np.ascontiguousarray(f(v).reshape(-1, 128).T)
    consts = _consts()
    shared = dict(
        qk_g=np.ascontiguousarray(np.stack([f(q_norm_g)[0], f(k_norm_g)[0]], axis=1)),
        g1=fm(norm1_g[0]), g2=fm(norm2_g[0]),
        gf_rep=np.ascontiguousarray(np.tile(f(norm_f_g)[None, :], (128, 1))),
        bmod2=np.ascontiguousarray(np.tile(f(b_mod)[0][None, :], (2, 1))),
        w_mod=f(w_mod)[0], w_in=f(w_in)[0], w_attn_out=f(w_attn_out)[0], w_conv_out=f(w_conv_out)[0],
        w_out=f(w_out)[0],
        conv_w=np.ascontiguousarray(f(conv_dw_w)[0, :, 0, :].T.reshape(16, 128, TAPS).transpose(1, 0, 2)),
        conv_b=fm(conv_dw_b[0]), ln_g=fm(conv_ln_g[0]), ln_b=fm(conv_ln_b[0]),
        w_router=np.ascontiguousarray(np.concatenate([f(w_router_group)[0], f(w_router_expert)[0]], axis=1)),
        b_router_rep=np.ascontiguousarray(np.tile(np.concatenate([f(b_router_group)[0], f(b_router_expert)[0]])[None, :],
                                                  (128, 1))),
        w_exp_gate=f(w_exp_gate)[0], w_exp_up=f(w_exp_up)[0], w_exp_down=f(w_exp_down)[0],
        **consts,
    )
    in_maps = []
    for core in range(8):
        b, hf = core // 2, core % 2
        own = x[b, hf * OWN:(hf + 1) * OWN]
        oth = x[b, (1 - hf) * OWN:(2 - hf) * OWN]
        halo = np.zeros((2 * HALO, D), np.float32)
        mask = np.zeros((128, 2 * HALO), np.float32)
        if hf == 1:
            halo[:HALO] = x[b, OWN - HALO:OWN]
            mask[:, :HALO] = 1.0
        else:
            halo[HALO:] = x[b, OWN:OWN + HALO]
            mask[:, HALO:] = 1.0
        C, S = _rope_tables(hf)
        cv = np.ascontiguousarray(np.stack([c[b].reshape(KC, 128).T, c_ctx.reshape(KC, 128).T], axis=2))
        m = dict(shared)
        m.update(x_own=np.ascontiguousarray(own), x_oth=np.ascontiguousarray(oth), ctx_b=np.ascontiguousarray(ctx[b]),
                 x_halo=halo, halo_mask=mask, cvec=cv, rope_c=C, rope_s=S)
        in_maps.append(m)
    key = (_debug, _stop_after)
    if key not in _NC_CACHE:
        _NC_CACHE[key] = build_nc(debug=_debug, stop_after=_stop_after)
    nc = _NC_CACHE[key]
    res = run_bass_kernel_spmd(nc, in_maps, core_ids=list(range(8)))
    if _debug:
        return res
    outp = np.empty((4, SEQ, D), np.float32)
    for core in range(8):
        b, hf = core // 2, core % 2
        outp[b, hf * OWN:(hf + 1) * OWN] = np.asarray(res.results[core]["out"], dtype=np.float32)
    return outp
```

```python
import contextlib
import numpy as np
import concourse.bass as bass
import concourse.mybir as mybir
from concourse.bass_utils import run_bass_kernel_spmd

F32 = mybir.dt.float32
BF16 = mybir.dt.bfloat16
I32 = mybir.dt.int32
ALU = mybir.AluOpType
AF = mybir.ActivationFunctionType
AX = mybir.AxisListType

D = 4096
KC = D // 128
SEQ = 4096
OWN = 2048
NCTX = 256
NKEY = SEQ + NCTX
NKT = NKEY // 128
HD = 128
NQH = 16
NKVH = 4
CW = 2048
TAPS = 31
Q_OFF, K_OFF, V_OFF, GLU_OFF, GATE_OFF, IN_W = 0, 2048, 2560, 3072, 7168, 15360
NE = 32
FF = 1024
CAP = 512
NST = CAP // 128
EPS = 1e-6
HALO = 16
UW = OWN + 2 * HALO

EPOCH = 12000
SAME_ENGINE_SYNC = True


class Sem:
    def __init__(self, fw, name, step):
        self.fw, self.name, self.step = fw, name, step
        self.count = 0
        self.handles = []

    def _handle(self, ep):
        while len(self.handles) <= ep:
            h = self.fw.stack.enter_context(self.fw.nc.semaphore(f"{self.name}_{len(self.handles)}"))
            self.handles.append(h)
        return self.handles[ep]

    def next(self, n=1):
        ep = self.count // EPOCH
        if (self.count + n - 1) // EPOCH != ep:
            self.count = (ep + 1) * EPOCH
            ep += 1
        self.count += n
        idx = self.count - ep * EPOCH
        return self._handle(ep), (self, ep, idx * self.step)

    def last_token(self):
        if self.count == 0:
            return None
        ep = (self.count - 1) // EPOCH
        return (self, ep, (self.count - ep * EPOCH) * self.step)


class Res:
    def __init__(self, name, persistent):
        self.name = name
        self.persistent = persistent
        self.last_write = None
        self.readers = []
        self.dma_sem = None


class FW:
    ENGS = ("pe", "act", "dve", "pool", "sp")

    def __init__(self, nc, stack):
        self.nc, self.stack = nc, stack
        self.ops = {e: [] for e in self.ENGS}
        self.esem = {e: Sem(self, f"s_{e}", 1) for e in ("pe", "act", "dve", "pool")}
        self.waited = {e: {} for e in self.ENGS}
        self.sem_pool = []
        self.all_dma_sems = []
        self.phase_res = []
        self.ph = None

    def begin_phase(self):
        self.ph = contextlib.ExitStack()
        self.phase_res = []

    def end_phase(self):
        self.barrier()
        self.emit()
        for r in self.phase_res:
            if r.dma_sem is not None:
                self.sem_pool.append(r.dma_sem)
                r.dma_sem = None
        self.phase_res = []
        self.ph.close()
        self.ph = None

    def res(self, name="r", persistent=False):
        r = Res(name, persistent)
        if not persistent:
            self.phase_res.append(r)
        return r

    def sb(self, name, shape, dtype, persistent=False, stack=None):
        st = stack if stack is not None else (self.stack if persistent else self.ph)
        self.nsb = getattr(self, "nsb", 0) + 1
        t = st.enter_context(self.nc.sbuf_tensor(f"sb{self.nsb}_{name}", list(shape), dtype))
        return t

    def buf(self, name, shape, dtype, persistent=False, stack=None):
        return self.sb(name, shape, dtype, persistent, stack), self.res(name, persistent or stack is not None)

    def ring(self, name, n, shape, dtype):
        return Ring([self.buf(f"{name}{i}", shape, dtype) for i in range(n)])

    def _waits_for(self, eng, toks):
        out = []
        for tok in toks:
            if tok is None:
                continue
            sem, ep, val = tok
            key = (id(sem), ep)
            if self.waited[eng].get(key, 0) >= val:
                continue
            self.waited[eng][key] = val
            out.append((sem.handles[ep], val))
        return out

    def op(self, eng, fn, reads=(), writes=(), dma=0, sem_res=None):
        toks = []
        for r in reads:
            toks.append(r.last_write)
        for w in writes:
            toks.append(w.last_write)
            toks.extend(w.readers)
        if dma:
            anchor = sem_res if sem_res is not None else writes[0]
            if anchor.dma_sem is None:
                if self.sem_pool:
                    anchor.dma_sem = self.sem_pool.pop()
                else:
                    anchor.dma_sem = Sem(self, f"d{len(self.all_dma_sems)}", 16)
                    self.all_dma_sems.append(anchor.dma_sem)
            handle, tok = anchor.dma_sem.next(dma)
            own = None
        else:
            handle, tok = self.esem[eng].next(1)
            own = self.esem[eng]
        if own is not None and not (SAME_ENGINE_SYNC and eng in ("act", "dve", "pool")):
            toks = [t for t in toks if t is None or t[0] is not own]
        waits = self._waits_for(eng, toks)
        self.ops[eng].append((waits, fn, handle, dma))
        for r in reads:
            r.readers.append(tok)
        for w in writes:
            w.last_write = tok
            w.readers = []
        return tok

    def bc_reg(self, e, val):
        if val not in self._regs:
            self._regs[val] = e.to_reg(val)
        return self._regs[val]

    def barrier(self):
        toks = [s.last_token() for s in self.esem.values()]
        toks += [s.last_token() for s in self.all_dma_sems]
        for eng in self.ENGS:
            waits = self._waits_for(eng, toks)
            self.ops[eng].append((waits, None, None, 0))

    def emit(self):
        nc = self.nc
        ops = self.ops
        self.ops = {e: [] for e in self.ENGS}
        with nc.Block() as block:
            def run(engname):
                def body(e):
                    self._regs = {}
                    for waits, fn, handle, dma in ops[engname]:
                        for h, v in waits:
                            e.wait_ge(h, v)
                        if fn is None:
                            continue
                        r = fn(e)
                        if dma:
                            assert isinstance(r, (list, tuple)) and len(r) == dma, (len(r), dma)
                            for ins in r:
                                ins.then_inc(handle, 16)
                        else:
                            r.then_inc(handle, 1)
                return body
            block.tensor(run("pe"))
            block.scalar(run("act"))
            block.vector(run("dve"))
            block.gpsimd(run("pool"))
            block.sync(run("sp"))


class Ring:
    def __init__(self, items):
        self.items = items
        self.i = 0

    def next(self):
        it = self.items[self.i % len(self.items)]
        self.i += 1
        return it


def mm_group(fw, out_ap, pairs, reads, writes):
    pairs = list(pairs)

    def fn(e):
        n = len(pairs)
        last = None
        for i, (l, r) in enumerate(pairs):
            last = e.matmul(out_ap, lhsT=l, rhs=r, start=(i == 0), stop=(i == n - 1))
        return last
    return fw.op("pe", fn, reads, writes)


def dma(fw, eng, out, in_, reads, writes, sem_res=None):
    return fw.op(eng, lambda e: [e.dma_start(out=out, in_=in_)], reads, writes, dma=1, sem_res=sem_res)


def store(fw, eng, out, in_, r_src):
    return fw.op(eng, lambda e: [e.dma_start(out=out, in_=in_)], [r_src], [], dma=1, sem_res=r_src)


def build_nc(debug=False, stop_after=None):
    nc = bass.Bass("TRN2", target_bir_lowering=False)

    def din(name, shape, dt=F32):
        return nc.dram_tensor(name, list(shape), dt, kind="ExternalInput").ap()

    def dscr(name, shape, dt=F32):
        return nc.dram_tensor(name, list(shape), dt, kind="Internal").ap()

    x_own = din("x_own", [OWN, D])
    x_oth = din("x_oth", [OWN, D])
    ctx_b = din("ctx_b", [NCTX, D])
    x_halo = din("x_halo", [2 * HALO, D])
    halo_mask = din("halo_mask", [128, 2 * HALO])
    cvec = din("cvec", [128, KC, 2])
    rope_c = din("rope_c", [128, NKEY])
    rope_s = din("rope_s", [128, NKEY])
    qk_g = din("qk_g", [128, 2])
    ident_in = din("ident", [128, 128])
    pmat_in = din("pmat", [128, 128])
    tril_in = din("tril", [128, 128])
    g1_in = din("g1", [128, KC])
    g2_in = din("g2", [128, KC])
    gf_rep_in = din("gf_rep", [128, D])
    bmod2 = din("bmod2", [2, 6 * D])
    w_mod = din("w_mod", [D, 6 * D])
    w_in = din("w_in", [D, IN_W])
    w_ao = din("w_attn_out", [NQH * HD, D])
    w_co = din("w_conv_out", [CW, D])
    w_out = din("w_out", [D, D])
    cw_in = din("conv_w", [128, 16, TAPS])
    cb_in = din("conv_b", [128, 16])
    lng_in = din("ln_g", [128, 16])
    lnb_in = din("ln_b", [128, 16])
    w_r = din("w_router", [D, 36])
    b_r_rep = din("b_router_rep", [128, 36])
    iota_cap = din("iota_cap", [128, NE])
    tok_ent = din("tok_ent", [128, 16, 2, 2], I32)
    tab_i_init = din("tab_i_init", [NE * CAP, 2], I32)
    tab_w_init = din("tab_w_init", [NE * CAP, 2])
    w_eg = din("w_exp_gate", [NE, D, FF])
    w_eu = din("w_exp_up", [NE, D, FF])
    w_ed = din("w_exp_down", [NE, FF, D])
    out = nc.dram_tensor("out", [OWN, D], F32, kind="ExternalOutput").ap()

    mod_all = dscr("mod_all", [2, 6 * D])
    qT_d = dscr("qT_d", [NQH, 128, OWN], BF16)
    uT_d = dscr("uT_d", [16, 128, UW])
    gates_d = dscr("gates_d", [64, 128, OWN])
    attnT_d = dscr("attnT_d", [16, 128, OWN], BF16)
    csT_d = dscr("csT_d", [16, 128, OWN], BF16)
    xnew_d = dscr("xnew_d", [OWN, D])
    tab_i = dscr("tab_i", [NE * CAP, 2], I32)
    tab_w = dscr("tab_w", [NE * CAP, 2])
    ybufs = [dscr(f"ybuf{i}", [2 * OWN, 1024]) for i in range(4)]

    w_mod_v = w_mod.rearrange("(kc p) n -> p kc n", p=128)
    w_in_v = w_in.rearrange("(kc p) n -> p kc n", p=128)
    w_ao_v = w_ao.rearrange("(kc p) n -> p kc n", p=128)
    w_co_v = w_co.rearrange("(kc p) n -> p kc n", p=128)
    w_out_v = w_out.rearrange("(kc p) n -> p kc n", p=128)
    w_r_v = w_r.rearrange("(kc p) n -> p kc n", p=128)

    dbg = {}

    with contextlib.ExitStack() as st:
        fw = FW(nc, st)
        r_mod_all = fw.res("mod_all", True)
        r_qT_d = fw.res("qT_d", True)
        r_uT_d = fw.res("uT_d", True)
        r_gates_d = fw.res("gates_d", True)
        r_attnT_d = fw.res("attnT_d", True)
        r_csT_d = fw.res("csT_d", True)
        r_xnew_d = [fw.res(f"xnew_d{g}", True) for g in range(4)]
        r_tab_i = fw.res("tab_i", True)
        r_tab_w = fw.res("tab_w", True)
        r_ybuf = fw.res("ybuf", True)
        r_out = fw.res("out", True)

        ident, r_ident = fw.buf("ident", [128, 128], F32, True)
        identb, r_identb = fw.buf("identb", [128, 128], BF16, True)
        pmat, r_pmat = fw.buf("pmat", [128, 128], F32, True)
        ones_f, r_ones_f = fw.buf("ones_f", [128, 128], F32, True)
        ones_b, r_ones_b = fw.buf("ones_b", [128, 128], BF16, True)
        epst, r_eps = fw.buf("epst", [128, 1], F32, True)
        qkg, r_qkg = fw.buf("qkg", [128, 2], F32, True)
        modv, r_modv = fw.buf("modv", [128, 6, KC], F32, True)
        A1, SH1, A1C, SH1C, A2, SH2 = range(6)

        banks = []
        for i in range(8):
            t = st.enter_context(nc.psum_tensor(f"bank{i}", [128, 512], F32))
            banks.append((t, fw.res(f"bank{i}", True)))

        def norm_transpose(rows, r_rows, nrows, mv_a, mv_s, dst_fn, r_dst, sc, pbanks, evac_engs=("act", "dve")):
            junk, r_junk, ss, r_ss, rstd, r_rstd, dg, r_dg, ss2, r_ss2 = sc
            for hh in range(2):
                fw.op("act", lambda e, hh=hh: e.activation(out=junk[:nrows, :], in_=rows[:nrows, hh * 2048:(hh + 1) * 2048],
                                                           func=AF.Square, accum_out=ss2[:nrows, hh:hh + 1]),
                      [r_rows], [r_junk, r_ss2])
            fw.op("dve", lambda e: e.tensor_tensor(out=ss[:nrows, :], in0=ss2[:nrows, 0:1], in1=ss2[:nrows, 1:2], op=ALU.add),
                  [r_ss2], [r_ss])
            fw.op("act", lambda e: e.activation(out=rstd[:nrows, :], in_=ss[:nrows, :], func=AF.Sqrt,
                                                scale=1.0 / D, bias=epst[:nrows, 0:1]), [r_ss, r_eps], [r_rstd])
            fw.op("dve", lambda e: e.reciprocal(out=rstd[:nrows, :], in_=rstd[:nrows, :]), [r_rstd], [r_rstd])
            fw.op("dve", lambda e: e.tensor_scalar(out=dg[:nrows, :nrows], in0=ident[:nrows, :nrows],
                                                   scalar1=rstd[:nrows, 0:1], scalar2=None, op0=ALU.mult),
                  [r_rstd, r_ident], [r_dg])
            for q in range(KC // 4):
                bk, r_bk = pbanks[q % len(pbanks)]

                def fn(e, q=q, bk=bk):
                    last = None
                    for j in range(4):
                        kc = q * 4 + j
                        last = e.matmul(bk[:, j * 128:j * 128 + nrows], lhsT=rows[:nrows, kc * 128:(kc + 1) * 128],
                                        rhs=dg[:nrows, :nrows], start=True, stop=True)
                    return last
                fw.op("pe", fn, [r_rows, r_dg], [r_bk])
                for j in range(4):
                    kc = q * 4 + j
                    eng = evac_engs[kc % len(evac_engs)]
                    if eng == "act":
                        fw.op("act", lambda e, kc=kc, j=j, bk=bk: e.activation(
                            out=dst_fn(kc), in_=bk[:, j * 128:j * 128 + nrows], func=AF.Identity,
                            scale=modv[:, mv_a, kc:kc + 1], bias=modv[:, mv_s, kc:kc + 1]),
                            [r_bk, r_modv], [r_dst])
                    else:
                        fw.op("dve", lambda e, kc=kc, j=j, bk=bk: e.tensor_scalar(
                            out=dst_fn(kc), in0=bk[:, j * 128:j * 128 + nrows],
                            scalar1=modv[:, mv_a, kc:kc + 1], scalar2=modv[:, mv_s, kc:kc + 1],
                            op0=ALU.mult, op1=ALU.add), [r_bk, r_modv], [r_dst])

        def nt_scratch():
            junk, r_junk = fw.buf("nt_junk", [128, D // 2], BF16)
            ss, r_ss = fw.buf("nt_ss", [128, 1], F32)
            ss2, r_ss2 = fw.buf("nt_ss2", [128, 2], F32)
            rstd, r_rstd = fw.buf("nt_rstd", [128, 1], F32)
            dg, r_dg = fw.buf("nt_dg", [128, 128], F32)
            return (junk, r_junk, ss, r_ss, rstd, r_rstd, dg, r_dg, ss2, r_ss2)

        fw.begin_phase()
        dma(fw, "sp", ident[:], ident_in, [], [r_ident])
        dma(fw, "sp", pmat[:], pmat_in, [], [r_pmat])
        dma(fw, "sp", qkg[:], qk_g, [], [r_qkg])
        fw.op("dve", lambda e: e.memset(ones_f[:], 1.0), [], [r_ones_f])
        fw.op("dve", lambda e: e.memset(ones_b[:], 1.0), [], [r_ones_b])
        fw.op("dve", lambda e: e.memset(epst[:], EPS), [], [r_eps])
        fw.op("dve", lambda e: e.tensor_copy(out=identb[:], in_=ident[:]), [r_ident], [r_identb])
        dma(fw, "sp", tab_i, tab_i_init, [], [r_tab_i])
        dma(fw, "sp", tab_w, tab_w_init, [], [r_tab_w])

        cv, r_cv = fw.buf("cv", [128, KC, 2], F32)
        sg, r_sg = fw.buf("sgc", [128, KC, 2], F32)
        dma(fw, "sp", cv[:], cvec, [], [r_cv])
        fw.op("act", lambda e: e.activation(out=sg[:], in_=cv[:], func=AF.Sigmoid), [r_cv], [r_sg])
        fw.op("dve", lambda e: e.tensor_tensor(out=sg[:], in0=sg[:], in1=cv[:], op=ALU.mult), [r_sg, r_cv], [r_sg])
        bmr = fw.ring("bm", 2, [2, 512], F32)
        mrr = fw.ring("modrow", 2, [2, 512], F32)
        wring = fw.ring("wm", 3, [128, 8, 512], F32)
        NCH = 6 * D // 512
        for ch in range(NCH):
            bk, r_bk = banks[ch % 2]
            tiles = []
            for k4 in range(4):
                wt, r_wt = wring.next()
                dma(fw, "sp", wt[:], w_mod_v[:, k4 * 8:(k4 + 1) * 8, ch * 512:(ch + 1) * 512], [], [r_wt])
                tiles.append((wt, r_wt))

                def fn(e, k4=k4, wt=wt, bk=bk):
                    last = None
                    for j in range(8):
                        kc = k4 * 8 + j
                        last = e.matmul(bk[0:2, :], lhsT=sg[:, kc, :], rhs=wt[:, j, :],
                                        start=(kc == 0), stop=(kc == KC - 1))
                    return last
                fw.op("pe", fn, [r_sg, r_wt], [r_bk])
            bm, r_bm = bmr.next()
            dma(fw, "sp", bm[:], bmod2[:, ch * 512:(ch + 1) * 512], [], [r_bm])
            mr, r_mr = mrr.next()
            fw.op("dve", lambda e, bk=bk, bm=bm, mr=mr: e.tensor_tensor(out=mr[:], in0=bk[0:2, :], in1=bm[:], op=ALU.add),
                  [r_bk, r_bm], [r_mr])
            store(fw, "sp", mod_all[:, ch * 512:(ch + 1) * 512], mr[:], r_mr)
        fw.barrier()
        mraw, r_mraw = fw.buf("mraw", [128, 6, KC], F32)
        g12, r_g12 = fw.buf("g12", [128, 2, KC], F32)
        dma(fw, "sp", g12[:, 0, :], g1_in, [], [r_g12])
        dma(fw, "sp", g12[:, 1, :], g2_in, [r_g12], [r_g12])

        def ld_mod(slot, row, j):
            src = mod_all[row, j * D:(j + 1) * D].rearrange("(kc p) -> p kc", p=128)
            fw.op("sp", lambda e: [e.dma_start(out=mraw[:, slot, :], in_=src, allow_slow_non_contiguous=True)],
                  [r_mraw], [r_mraw], dma=1)
        ld_mod(0, 0, 1)
        ld_mod(1, 0, 0)
        ld_mod(2, 1, 1)
        ld_mod(3, 1, 0)
        ld_mod(4, 0, 4)
        ld_mod(5, 0, 3)
        for (dst_a, dst_s, s_sc, s_sh, gi) in ((A1, SH1, 0, 1, 0), (A1C, SH1C, 2, 3, 0), (A2, SH2, 4, 5, 1)):
            fw.op("dve", lambda e, dst_a=dst_a, s_sc=s_sc, gi=gi: e.scalar_tensor_tensor(
                out=modv[:, dst_a, :], in0=mraw[:, s_sc, :], scalar=1.0, in1=g12[:, gi, :],
                op0=ALU.add, op1=ALU.mult), [r_mraw, r_g12], [r_modv])
            fw.op("dve", lambda e, dst_s=dst_s, s_sh=s_sh: e.tensor_copy(out=modv[:, dst_s, :], in_=mraw[:, s_sh, :]),
                  [r_mraw], [r_modv])
        if debug:
            dbg["modv"] = _dump(nc, fw, "dbg_modv", modv, r_modv, [128, 6, KC], F32)
        fw.end_phase()
        if stop_after == 0:
            return _finish(nc, fw, dbg, out, r_out)

        kv_stack = contextlib.ExitStack()
        KT, r_KT = fw.buf("KT", [128, NKVH, NKEY], BF16, stack=kv_stack)
        VA, r_VA = fw.buf("VA", [128, NKT, NKVH, HD + 2], BF16, stack=kv_stack)
        fw.begin_phase()
        fw.op("pool", lambda e: e.memset(VA[:, :, :, HD:HD + 2], 1.0), [], [r_VA])
        sc = nt_scratch()
        xring = fw.ring("xrow", 1, [128, D], F32)
        hT, r_hT = fw.buf("hT", [128, KC, 512], BF16)
        wring = fw.ring("wb", 3, [128, KC, 256], BF16)
        rc, r_rc = fw.buf("ropec", [128, 512], F32)
        rs, r_rs = fw.buf("ropes", [128, 512], F32)
        qg, r_qg = fw.buf("qg", [128, 512], F32)
        sq, r_sq = fw.buf("sq", [128, 512], BF16)
        rr, r_rr = fw.buf("rr", [128, 512], F32)
        t1, r_t1 = fw.buf("t1", [128, 512], F32)
        t2, r_t2 = fw.buf("t2", [128, 512], F32)
        qst = fw.ring("qst", 2, [128, 512], BF16)
        ust = fw.ring("ust", 2, [128, 512], F32)
        gst = fw.ring("gst", 2, [128, 512], F32)
        sgt, r_sgt = fw.buf("sgt", [128, 512], F32)
        hm, r_hm = fw.buf("hm", [128, 2 * HALO], F32)
        dma(fw, "sp", hm[:], halo_mask, [], [r_hm])

        groups = []
        for g in range(4):
            groups.append(dict(src=x_own[g * 512:(g + 1) * 512, :], n=512, a=A1, s=SH1, key0=g * 512,
                               full=True, own0=g * 512))
        for g in range(4):
            groups.append(dict(src=x_oth[g * 512:(g + 1) * 512, :], n=512, a=A1, s=SH1, key0=OWN + g * 512,
                               full=False))
        groups.append(dict(src=ctx_b, n=NCTX, a=A1C, s=SH1C, key0=SEQ, full=False))
        groups.append(dict(src=x_halo, n=2 * HALO, a=A1, s=SH1, key0=None, full=False, halo=True))

        def load_w(c0):
            wt, r_wt = wring.next()
            dma(fw, "pool", wt[:], w_in_v[:, :, c0:c0 + 256], [], [r_wt])
            return wt, r_wt

        def proj_fm(wt, r_wt, sub, n, bk, r_bk):
            mm_group(fw, bk[:, 0:n], [(wt[:, kc, sub * 128:(sub + 1) * 128], hT[:, kc, 0:n]) for kc in range(KC)],
                     [r_wt, r_hT], [r_bk])

        bank_i = [0]

        def next_bank(lo=0, hi=4):
            b = banks[lo + bank_i[0] % (hi - lo)]
            bank_i[0] += 1
            return b

        def do_group(G):
                n = G["n"]
                ntile = (n + 127) // 128
                for tt in range(ntile):
                    nr = min(128, n - tt * 128)
                    xr, r_xr = xring.next()
                    dma(fw, "sp", xr[:nr, :], G["src"][tt * 128:tt * 128 + nr, :], [], [r_xr])
                    norm_transpose(xr, r_xr, nr, G["a"], G["s"],
                                   lambda kc, tt=tt, nr=nr: hT[:, kc, tt * 128:tt * 128 + nr], r_hT, sc, banks[4:8])
                halo = G.get("halo", False)
                if not halo:
                    key0 = G["key0"]
                    dma(fw, "sp", rc[:, 0:n], rope_c[:, key0:key0 + n], [], [r_rc])
                    dma(fw, "sp", rs[:, 0:n], rope_s[:, key0:key0 + n], [], [r_rs])
                    heads = []
                    if G["full"]:
                        heads += [("q", h) for h in range(NQH)]
                    heads += [("k", h) for h in range(NKVH)]
                    for hi in range(0, len(heads), 2):
                        kind, h0 = heads[hi]
                        c0 = (Q_OFF if kind == "q" else K_OFF) + h0 * HD
                        wt, r_wt = load_w(c0)
                        for sub in range(2):
                            kind, h = heads[hi + sub]
                            gcol = 0 if kind == "q" else 1
                            bk, r_bk = next_bank()
                            proj_fm(wt, r_wt, sub, n, bk, r_bk)
                            fw.op("act", lambda e, bk=bk, gcol=gcol: e.activation(
                                out=qg[:, 0:n], in_=bk[:, 0:n], func=AF.Identity, scale=qkg[:, gcol:gcol + 1]),
                                [r_bk, r_qkg], [r_qg])
                            fw.op("act", lambda e, bk=bk: e.activation(out=sq[:, 0:n], in_=bk[:, 0:n], func=AF.Square),
                                  [r_bk], [r_sq])
                            b2, r_b2 = next_bank()
                            mm_group(fw, b2[:, 0:n], [(ones_b[:], sq[:, 0:n])], [r_ones_b, r_sq], [r_b2])
                            b3, r_b3 = next_bank()
                            mm_group(fw, b3[:, 0:n], [(pmat[:], qg[:, 0:n])], [r_pmat, r_qg], [r_b3])
                            fw.op("act", lambda e, b2=b2: e.activation(out=rr[:, 0:n], in_=b2[:, 0:n], func=AF.Sqrt,
                                                                       scale=1.0 / HD, bias=epst[:, 0:1]),
                                  [r_b2, r_eps], [r_rr])
                            fw.op("dve", lambda e: e.reciprocal(out=rr[:, 0:n], in_=rr[:, 0:n]), [r_rr], [r_rr])
                            fw.op("dve", lambda e: e.tensor_tensor(out=t1[:, 0:n], in0=qg[:, 0:n], in1=rc[:, 0:n], op=ALU.mult),
                                  [r_qg, r_rc], [r_t1])
                            fw.op("dve", lambda e, b3=b3: e.tensor_tensor(out=t2[:, 0:n], in0=b3[:, 0:n], in1=rs[:, 0:n],
                                                                          op=ALU.mult), [r_b3, r_rs], [r_t2])
                            fw.op("dve", lambda e: e.tensor_tensor(out=t1[:, 0:n], in0=t1[:, 0:n], in1=t2[:, 0:n], op=ALU.add),
                                  [r_t1, r_t2], [r_t1])
                            if kind == "k":
                                fw.op("dve", lambda e, h=h, key0=key0: e.tensor_tensor(
                                    out=KT[:, h, key0:key0 + n], in0=t1[:, 0:n], in1=rr[:, 0:n], op=ALU.mult),
                                    [r_t1, r_rr], [r_KT])
                            else:
                                qs, r_qs = qst.next()
                                fw.op("dve", lambda e, qs=qs: e.tensor_tensor(out=qs[:, 0:n], in0=t1[:, 0:n], in1=rr[:, 0:n],
                                                                               op=ALU.mult), [r_t1, r_rr], [r_qs])
                                o0 = G["own0"]
                                store(fw, "sp", qT_d[h, :, o0:o0 + n], qs[:, 0:n], r_qs)
                    for vb in range(2):
                        wt, r_wt = load_w(V_OFF + vb * 256)
                        for tt in range(ntile):
                            bk, r_bk = next_bank()
                            mm_group(fw, bk[:, 0:256], [(hT[:, kc, tt * 128:(tt + 1) * 128], wt[:, kc, :]) for kc in range(KC)],
                                     [r_wt, r_hT], [r_bk])
                            kt = (key0 // 128) + tt
                            eng = "act" if tt % 2 == 0 else "dve"
                            src = bk[:, 0:256].rearrange("p (h d) -> p h d", h=2)
                            if eng == "act":
                                fw.op("act", lambda e, kt=kt, vb=vb, src=src: e.activation(
                                    out=VA[:, kt, vb * 2:vb * 2 + 2, 0:HD], in_=src, func=AF.Identity), [r_bk], [r_VA])
                            else:
                                fw.op("dve", lambda e, kt=kt, vb=vb, src=src: e.tensor_copy(
                                    out=VA[:, kt, vb * 2:vb * 2 + 2, 0:HD], in_=src), [r_bk], [r_VA])
                if G["full"] or halo:
                    if halo:
                        ucol0 = None
                    else:
                        ucol0 = HALO + G["own0"]
                    for cb in range(8):
                        wa, r_wa = load_w(GLU_OFF + cb * 256)
                        wg, r_wg = load_w(GLU_OFF + CW + cb * 256)
                        for sub in range(2):
                            cc = cb * 2 + sub
                            ba, r_ba = next_bank()
                            proj_fm(wa, r_wa, sub, n, ba, r_ba)
                            bg, r_bg = next_bank()
                            proj_fm(wg, r_wg, sub, n, bg, r_bg)
                            fw.op("act", lambda e, bg=bg: e.activation(out=sgt[:, 0:n], in_=bg[:, 0:n], func=AF.Sigmoid),
                                  [r_bg], [r_sgt])
                            us, r_us = ust.next()
                            fw.op("dve", lambda e, ba=ba, us=us: e.tensor_tensor(out=us[:, 0:n], in0=ba[:, 0:n], in1=sgt[:, 0:n],
                                                                               op=ALU.mult), [r_ba, r_sgt], [r_us])
                            if halo:
                                fw.op("dve", lambda e, us=us: e.tensor_tensor(out=us[:, 0:n], in0=us[:, 0:n], in1=hm[:, 0:n],
                                                                               op=ALU.mult), [r_us, r_hm], [r_us])
                                fw.op("sp", lambda e, us=us, cc=cc: [
                                    e.dma_start(out=uT_d[cc, :, 0:HALO], in_=us[:, 0:HALO]),
                                    e.dma_start(out=uT_d[cc, :, HALO + OWN:UW], in_=us[:, HALO:2 * HALO])],
                                    [r_us], [], dma=2, sem_res=r_us)
                            else:
                                store(fw, "sp", uT_d[cc, :, ucol0:ucol0 + n], us[:, 0:n], r_us)
                if G["full"]:
                    o0 = G["own0"]
                    for gb in range(32):
                        wt, r_wt = load_w(GATE_OFF + gb * 256)
                        for sub in range(2):
                            ch = gb * 2 + sub
                            bk, r_bk = next_bank()
                            proj_fm(wt, r_wt, sub, n, bk, r_bk)
                            gs, r_gs = gst.next()
                            fw.op("act", lambda e, bk=bk, gs=gs: e.activation(out=gs[:, 0:n], in_=bk[:, 0:n], func=AF.Sigmoid),
                                  [r_bk], [r_gs])
                            store(fw, "sp", gates_d[ch, :, o0:o0 + n], gs[:, 0:n], r_gs)

        for G in groups:
            do_group(G)
        if debug:
            dbg["KT"] = _dump(nc, fw, "dbg_KT", KT, r_KT, [128, NKVH, NKEY], BF16)
            dbg["VA"] = _dump(nc, fw, "dbg_VA", VA, r_VA, [128, NKT, NKVH, HD + 2], BF16)
        fw.end_phase()
        if stop_after == 1:
            kv_stack.close()
            return _finish(nc, fw, dbg, out, r_out)

        fw.begin_phase()
        qT, r_qT = fw.buf("qT", [128, NQH, 512], BF16)
        pring = fw.ring("pexp", 3, [128, 512], BF16)
        atm = fw.ring("atm", 2, [128, 128], BF16)
        rden = fw.ring("rden", 2, [128, 1], F32)
        aT, r_aT = fw.buf("aT", [128, NQH, 512], BF16)
        uin = fw.ring("uin", 2, [128, 512 + 2 * HALO], F32)
        yc, r_yc = fw.buf("yc", [128, 16, 512], F32)
        ysq, r_ysq = fw.buf("ysq", [128, 512], F32)
        mean, r_mean = fw.buf("mean", [128, 512], F32)
        var, r_var = fw.buf("var", [128, 512], F32)
        zt, r_zt = fw.buf("zt", [128, 512], F32)
        cst = fw.ring("cst", 2, [128, 512], BF16)
        cw, r_cw = fw.buf("cw", [128, 16, TAPS], F32)
        cbv, r_cbv = fw.buf("cbv", [128, 16], F32)
        lng, r_lng = fw.buf("lng", [128, 16], F32)
        lnb, r_lnb = fw.buf("lnb", [128, 16], F32)
        dma(fw, "sp", cw[:], cw_in, [], [r_cw])
        dma(fw, "sp", cbv[:], cb_in, [], [r_cbv])
        dma(fw, "sp", lng[:], lng_in, [], [r_lng])
        dma(fw, "sp", lnb[:], lnb_in, [], [r_lnb])
        SCALE = float(HD) ** -0.5
        s_banks = [banks[0], banks[1]]
        o_banks = [(banks[2], banks[3]), (banks[4], banks[5])]
        tr_bank = banks[6]
        st_bank = banks[7]

        def conv_chunk(cc, o0):
            ui, r_ui = uin.next()
            dma(fw, "sp", ui[:], uT_d[cc, :, o0:o0 + 512 + 2 * HALO], [], [r_ui])
            fw.op("dve", lambda e, ui=ui, cc=cc: e.tensor_scalar(
                out=yc[:, cc, :], in0=ui[:, 1:513], scalar1=cw[:, cc, 0:1], scalar2=cbv[:, cc:cc + 1],
                op0=ALU.mult, op1=ALU.add), [r_ui, r_cw, r_cbv], [r_yc])
            for k in range(1, TAPS):
                fw.op("dve", lambda e, ui=ui, cc=cc, k=k: e.scalar_tensor_tensor(
                    out=yc[:, cc, :], in0=ui[:, k + 1:k + 513], scalar=cw[:, cc, k:k + 1], in1=yc[:, cc, :],
                    op0=ALU.mult, op1=ALU.add), [r_ui, r_cw, r_yc], [r_yc])

        for g in range(4):
            o0 = g * 512
            dma(fw, "sp", qT[:], qT_d[:, :, o0:o0 + 512].rearrange("h p t -> p h t"), [], [r_qT])
            for h in range(NQH):
                kvh = h // (NQH // NKVH)
                ob = o_banks[h % 2]
                pend = None
                for kc in range(NKT + 1):
                    if kc < NKT:
                        sb_, r_sb = s_banks[kc % 2]
                        mm_group(fw, sb_[:, :], [(KT[:, kvh, kc * 128:(kc + 1) * 128], qT[:, h, :])], [r_KT, r_qT], [r_sb])
                        pb, r_pb = pring.next()
                        fw.op("act", lambda e, sb_=sb_, pb=pb: e.activation(out=pb[:], in_=sb_[:, :], func=AF.Exp,
                                                                             scale=SCALE), [r_sb], [r_pb])
                        cur = (kc, pb, r_pb)
                    else:
                        cur = None
                    if pend is not None:
                        pkc, ppb, r_ppb = pend

                        def fn(e, pkc=pkc, ppb=ppb, kvh=kvh, ob=ob):
                            last = None
                            for sub in range(4):
                                bk = ob[sub // 2][0]
                                c0 = (sub % 2) * 256
                                last = e.matmul(bk[:, c0:c0 + HD + 1], lhsT=ppb[:, sub * 128:(sub + 1) * 128],
                                                rhs=VA[:, pkc, kvh, 0:HD + 1], start=(pkc == 0), stop=(pkc == NKT - 1))
                            return last
                        fw.op("pe", fn, [r_ppb, r_VA], [ob[0][1], ob[1][1]])
                    pend = cur
                for sub in range(4):
                    bk, r_bk = ob[sub // 2]
                    c0 = (sub % 2) * 256
                    rd, r_rd = rden.next()
                    fw.op("dve", lambda e, bk=bk, c0=c0, rd=rd: e.reciprocal(out=rd[:], in_=bk[:, c0 + HD:c0 + HD + 1]),
                          [r_bk], [r_rd])
                    am, r_am = atm.next()
                    fw.op("dve", lambda e, bk=bk, c0=c0, rd=rd, am=am: e.tensor_scalar(
                        out=am[:], in0=bk[:, c0:c0 + HD], scalar1=rd[:, 0:1], scalar2=None, op0=ALU.mult),
                        [r_bk, r_rd], [r_am])
                    tb, r_tb = tr_bank
                    tbv = tb[:].bitcast(BF16)
                    fw.op("pe", lambda e, am=am, tbv=tbv: e.transpose(tbv[:, 0:128], am[:], identb[:]),
                          [r_am, r_identb], [r_tb])
                    fw.op("act", lambda e, tbv=tbv, h=h, sub=sub: e.activation(
                        out=aT[:, h, sub * 128:(sub + 1) * 128], in_=tbv[:, 0:128], func=AF.Identity), [r_tb], [r_aT])
                conv_chunk(h, o0)
            store(fw, "pool", attnT_d[:, :, o0:o0 + 512].rearrange("h p t -> p h t"), aT[:], r_aT)

            sbk, r_sbk = st_bank
            mm_group(fw, sbk[:, :], [(ones_f[:], yc[:, cc, :]) for cc in range(16)], [r_ones_f, r_yc], [r_sbk])
            fw.op("act", lambda e, sbk=sbk: e.activation(out=mean[:], in_=sbk[:, :], func=AF.Identity, scale=1.0 / CW),
                  [r_sbk], [r_mean])
            for cc in range(16):
                fw.op("dve", lambda e, cc=cc: e.tensor_tensor(out=yc[:, cc, :], in0=yc[:, cc, :], in1=mean[:], op=ALU.subtract),
                      [r_yc, r_mean], [r_yc])
            for cc in range(16):
                fw.op("act", lambda e, cc=cc: e.activation(out=ysq[:], in_=yc[:, cc, :], func=AF.Square), [r_yc], [r_ysq])
                fw.op("pe", lambda e, cc=cc, sbk=sbk: e.matmul(sbk[:, :], lhsT=ones_f[:], rhs=ysq[:], start=(cc == 0),
                                                            stop=(cc == 15)), [r_ones_f, r_ysq], [r_sbk])
            fw.op("act", lambda e, sbk=sbk: e.activation(out=var[:], in_=sbk[:, :], func=AF.Sqrt, scale=1.0 / CW,
                                                         bias=epst[:, 0:1]), [r_sbk, r_eps], [r_var])
            fw.op("dve", lambda e: e.reciprocal(out=var[:], in_=var[:]), [r_var], [r_var])
            for cc in range(16):
                fw.op("dve", lambda e, cc=cc: e.tensor_tensor(out=zt[:], in0=yc[:, cc, :], in1=var[:], op=ALU.mult),
                      [r_yc, r_var], [r_zt])
                cs, r_cs = cst.next()
                fw.op("act", lambda e, cc=cc, cs=cs: e.activation(out=cs[:], in_=zt[:], func=AF.Silu,
                                                                 scale=lng[:, cc:cc + 1], bias=lnb[:, cc:cc + 1]),
                      [r_zt, r_lng, r_lnb], [r_cs])
                store(fw, "pool", csT_d[cc, :, o0:o0 + 512], cs[:], r_cs)
        fw.end_phase()
        kv_stack.close()
        if stop_after == 2:
            return _finish(nc, fw, dbg, out, r_out)

        fw.begin_phase()
        aT, r_aT = fw.buf("aT3", [128, 16, 512], BF16)
        cT, r_cT = fw.buf("cT3", [128, 16, 512], BF16)
        mT, r_mT = fw.buf("mT", [128, KC, 512], BF16)
        wring = fw.ring("wb3", 3, [128, 8192], BF16)
        gin = fw.ring("gin", 4, [128, 512], F32)
        tm1, r_tm1 = fw.buf("tm1", [128, 512], F32)
        tm2, r_tm2 = fw.buf("tm2", [128, 512], F32)
        xin = fw.ring("xin", 2, [128, 4, 256], F32)
        xo = fw.ring("xo", 2, [128, 4, 256], F32)
        gar = fw.ring("gar", 2, [128, 256], F32)
        for g in range(4):
            o0 = g * 512
            dma(fw, "sp", aT[:], attnT_d[:, :, o0:o0 + 512].rearrange("h p t -> p h t"), [], [r_aT])
            dma(fw, "sp", cT[:], csT_d[:, :, o0:o0 + 512].rearrange("h p t -> p h t"), [], [r_cT])
            for ob4 in range(8):
                wa, r_wa = wring.next()
                wav = wa[:].rearrange("p (k n) -> p k n", k=16)
                dma(fw, "pool", wav, w_ao_v[:, :, ob4 * 512:(ob4 + 1) * 512], [], [r_wa])
                wc, r_wc = wring.next()
                wcv = wc[:].rearrange("p (k n) -> p k n", k=16)
                dma(fw, "pool", wcv, w_co_v[:, :, ob4 * 512:(ob4 + 1) * 512], [], [r_wc])
                for sub in range(4):
                    oc = ob4 * 4 + sub
                    ba, r_ba = banks[(oc * 2) % 4]
                    bc, r_bc = banks[(oc * 2 + 1) % 4]
                    mm_group(fw, ba[:, :], [(wav[:, kc, sub * 128:(sub + 1) * 128], aT[:, kc, :]) for kc in range(16)],
                             [r_wa, r_aT], [r_ba])
                    mm_group(fw, bc[:, :], [(wcv[:, kc, sub * 128:(sub + 1) * 128], cT[:, kc, :]) for kc in range(16)],
                             [r_wc, r_cT], [r_bc])
                    ga_, r_ga = gin.next()
                    gc_, r_gc = gin.next()
                    dma(fw, "sp", ga_[:], gates_d[oc, :, o0:o0 + 512], [], [r_ga])
                    dma(fw, "sp", gc_[:], gates_d[32 + oc, :, o0:o0 + 512], [], [r_gc])
                    fw.op("dve", lambda e, ba=ba, ga_=ga_: e.tensor_tensor(out=tm1[:], in0=ba[:, :], in1=ga_[:], op=ALU.mult),
                          [r_ba, r_ga], [r_tm1])
                    fw.op("dve", lambda e, bc=bc, gc_=gc_: e.tensor_tensor(out=tm2[:], in0=bc[:, :], in1=gc_[:], op=ALU.mult),
                          [r_bc, r_gc], [r_tm2])
                    fw.op("dve", lambda e, oc=oc: e.tensor_tensor(out=mT[:, oc, :], in0=tm1[:], in1=tm2[:], op=ALU.add),
                          [r_tm1, r_tm2], [r_mT])
            for nb in range(16):
                wo, r_wo = wring.next()
                wov = wo[:].rearrange("p (k n) -> p k n", k=KC)
                dma(fw, "pool", wov, w_out_v[:, :, nb * 256:(nb + 1) * 256], [], [r_wo])
                xi, r_xi = xin.next()
                dma(fw, "sp", xi[:], x_own[o0:o0 + 512, nb * 256:(nb + 1) * 256].rearrange("(t p) n -> p t n", p=128),
                    [], [r_xi])
                gr, r_gr = gar.next()
                dma(fw, "sp", gr[:], mod_all[0:1, 2 * D + nb * 256:2 * D + (nb + 1) * 256].broadcast_to([128, 256]),
                    [], [r_gr])
                xo_, r_xo = xo.next()
                for tt in range(4):
                    bk, r_bk = banks[4 + (nb * 4 + tt) % 4]
                    mm_group(fw, bk[:, 0:256], [(mT[:, kc, tt * 128:(tt + 1) * 128], wov[:, kc, :]) for kc in range(KC)],
                             [r_wo, r_mT], [r_bk])
                    fw.op("dve", lambda e, bk=bk, tt=tt, gr=gr, xo_=xo_: e.tensor_tensor(
                        out=xo_[:, tt, :], in0=bk[:, 0:256], in1=gr[:], op=ALU.mult), [r_bk, r_gr], [r_xo])
                    fw.op("dve", lambda e, tt=tt, xi=xi, xo_=xo_: e.tensor_tensor(
                        out=xo_[:, tt, :], in0=xo_[:, tt, :], in1=xi[:, tt, :], op=ALU.add), [r_xo, r_xi], [r_xo])
                store(fw, "act", xnew_d[o0:o0 + 512, nb * 256:(nb + 1) * 256].rearrange("(t p) n -> p t n", p=128), xo_[:],
                      r_xo)
        fw.end_phase()
        if stop_after == 3:
            return _finish(nc, fw, dbg, out, r_out)

        fw.begin_phase()
        sc = nt_scratch()
        xring = fw.ring("xrow4", 2, [128, D], F32)
        h2T, r_h2T = fw.buf("h2T", [128, KC, 128], F32)
        wr, r_wr = fw.buf("wr", [128, KC, 36], F32)
        brr, r_brr = fw.buf("brr", [128, 36], F32)
        iot, r_iot = fw.buf("iot", [128, NE], F32)
        tril, r_tril = fw.buf("tril", [128, 128], F32)
        trilb, r_trilb = fw.buf("trilb", [128, 128], BF16)
        tke, r_tke = fw.buf("tke", [128, 16, 2, 2], I32)
        Mb, r_Mb = fw.buf("Mb", [128, 16, NE], BF16)
        dma(fw, "sp", wr[:], w_r_v, [], [r_wr])
        dma(fw, "sp", brr[:], b_r_rep, [], [r_brr])
        dma(fw, "sp", iot[:], iota_cap, [], [r_iot])
        dma(fw, "sp", tril[:], tril_in, [], [r_tril])
        dma(fw, "sp", tke[:], tok_ent, [], [r_tke])
        fw.op("dve", lambda e: e.tensor_copy(out=trilb[:], in_=tril[:]), [r_tril], [r_trilb])

        def sbuf1(name, shape, dt=F32):
            return fw.buf(name, shape, dt)
        L, r_L = sbuf1("L", [128, 36])
        gmax, r_gmax = sbuf1("gmax", [128, 1])
        ngmax, r_ngmax = sbuf1("ngmax", [128, 1])
        gmask, r_gmask = sbuf1("gmask", [128, 4])
        gexp, r_gexp = sbuf1("gexp", [128, 4])
        gsum, r_gsum = sbuf1("gsum", [128, 1])
        pg, r_pg = sbuf1("pg", [128, 1])
        pen, r_pen = sbuf1("pen", [128, 4])
        em, r_em = sbuf1("em", [128, NE])
        em2, r_em2 = sbuf1("em2", [128, NE])
        m1, r_m1 = sbuf1("m1", [128, 1])
        m2, r_m2 = sbuf1("m2", [128, 1])
        mk1, r_mk1 = sbuf1("mk1", [128, NE])
        mk2, r_mk2 = sbuf1("mk2", [128, NE])
        dd, r_dd = sbuf1("dd", [128, 1])
        e2, r_e2 = sbuf1("e2", [128, 1])
        wts, r_wts = sbuf1("wts", [128, 2])
        Msum, r_Msum = sbuf1("Msum", [128, NE])
        cum, r_cum = sbuf1("cum", [128, NE])
        tmp32, r_tmp32 = sbuf1("tmp32", [128, NE])
        pos, r_pos = sbuf1("pos", [128, 2])
        eb, r_eb = sbuf1("eb", [128, 2])
        ovf, r_ovf = sbuf1("ovf", [128, 2])
        dstf, r_dstf = sbuf1("dstf", [128, 2])
        dring = fw.ring("dsti", 2, [128, 2], I32)
        wring2 = fw.ring("wts2", 2, [128, 4], F32)

        for tt in range(16):
            g = tt // 4
            xr, r_xr = xring.next()
            dma(fw, "sp", xr[:], xnew_d[tt * 128:(tt + 1) * 128, :], [], [r_xr])
            norm_transpose(xr, r_xr, 128, A2, SH2, lambda kc: h2T[:, kc, :], r_h2T, sc, banks[4:8])
            lb, r_lb = banks[tt % 2]
            mm_group(fw, lb[:, 0:36], [(h2T[:, kc, :], wr[:, kc, :]) for kc in range(KC)], [r_h2T, r_wr], [r_lb])
            V = "dve"
            fw.op(V, lambda e, lb=lb: e.tensor_tensor(out=L[:], in0=lb[:, 0:36], in1=brr[:], op=ALU.add), [r_lb, r_brr], [r_L])
            fw.op(V, lambda e: e.tensor_reduce(out=gmax[:], in_=L[:, 0:4], axis=AX.X, op=ALU.max), [r_L], [r_gmax])
            fw.op(V, lambda e: e.tensor_scalar(out=gmask[:], in0=L[:, 0:4], scalar1=gmax[:, 0:1], scalar2=None,
                                               op0=ALU.is_equal), [r_L, r_gmax], [r_gmask])
            fw.op(V, lambda e: e.tensor_scalar(out=ngmax[:], in0=gmax[:], scalar1=-1.0, scalar2=None, op0=ALU.mult),
                  [r_gmax], [r_ngmax])
            fw.op("act", lambda e: e.activation(out=gexp[:], in_=L[:, 0:4], func=AF.Exp, bias=ngmax[:, 0:1],
                                                accum_out=gsum[:]), [r_L, r_ngmax], [r_gexp, r_gsum])
            fw.op(V, lambda e: e.reciprocal(out=pg[:], in_=gsum[:]), [r_gsum], [r_pg])
            fw.op(V, lambda e: e.tensor_scalar(out=pen[:], in0=gmask[:], scalar1=-1.0, scalar2=1e30, op0=ALU.add,
                                               op1=ALU.mult), [r_gmask], [r_pen])
            fw.op(V, lambda e: e.tensor_tensor(out=em[:].rearrange("p (g k) -> p g k", g=4),
                                               in0=L[:, 4:36].rearrange("p (g k) -> p g k", g=4),
                                               in1=pen[:].unsqueeze(2).broadcast_to([128, 4, 8]), op=ALU.add),
                  [r_L, r_pen], [r_em])
            fw.op(V, lambda e: e.tensor_reduce(out=m1[:], in_=em[:], axis=AX.X, op=ALU.max), [r_em], [r_m1])
            fw.op(V, lambda e: e.tensor_scalar(out=mk1[:], in0=em[:], scalar1=m1[:, 0:1], scalar2=None, op0=ALU.is_equal),
                  [r_em, r_m1], [r_mk1])
            fw.op(V, lambda e: e.scalar_tensor_tensor(out=em2[:], in0=mk1[:], scalar=-1e30, in1=em[:], op0=ALU.mult,
                                                      op1=ALU.add), [r_mk1, r_em], [r_em2])
            fw.op(V, lambda e: e.tensor_reduce(out=m2[:], in_=em2[:], axis=AX.X, op=ALU.max), [r_em2], [r_m2])
            fw.op(V, lambda e: e.tensor_scalar(out=mk2[:], in0=em2[:], scalar1=m2[:, 0:1], scalar2=None, op0=ALU.is_equal),
                  [r_em2, r_m2], [r_mk2])
            fw.op(V, lambda e: e.tensor_tensor(out=dd[:], in0=m2[:], in1=m1[:], op=ALU.subtract), [r_m1, r_m2], [r_dd])
            fw.op("act", lambda e: e.activation(out=e2[:], in_=dd[:], func=AF.Exp), [r_dd], [r_e2])
            wt2, r_wt2 = wring2.next()
            fw.op(V, lambda e: e.tensor_scalar(out=e2[:], in0=e2[:], scalar1=1.0, scalar2=None, op0=ALU.add), [r_e2], [r_e2])
            fw.op(V, lambda e: e.reciprocal(out=e2[:], in_=e2[:]), [r_e2], [r_e2])
            fw.op(V, lambda e, wt2=wt2: e.memset(wt2[:], 0.0), [], [r_wt2])
            fw.op(V, lambda e, wt2=wt2: e.tensor_tensor(out=wt2[:, 0:1], in0=e2[:], in1=pg[:], op=ALU.mult), [r_e2, r_pg], [r_wt2])
            fw.op(V, lambda e, wt2=wt2: e.tensor_tensor(out=wt2[:, 2:3], in0=pg[:], in1=wt2[:, 0:1], op=ALU.subtract),
                  [r_pg, r_wt2], [r_wt2])
            fw.op(V, lambda e: e.tensor_tensor(out=Msum[:], in0=mk1[:], in1=mk2[:], op=ALU.add), [r_mk1, r_mk2], [r_Msum])
            fw.op(V, lambda e, tt=tt: e.tensor_copy(out=Mb[:, tt, :], in_=Msum[:]), [r_Msum], [r_Mb])
            cb_, r_cb = banks[2 + tt % 2]
            pairs = [(trilb[:], Mb[:, tt, :])] + [(ones_b[:], Mb[:, j, :]) for j in range(tt)]
            mm_group(fw, cb_[:, 0:NE], pairs, [r_trilb, r_ones_b, r_Mb], [r_cb])
            fw.op(V, lambda e, cb_=cb_: e.tensor_copy(out=cum[:], in_=cb_[:, 0:NE]), [r_cb], [r_cum])
            for k, (mk, r_mk) in enumerate(((mk1, r_mk1), (mk2, r_mk2))):
                fw.op(V, lambda e, mk=mk: e.tensor_tensor(out=tmp32[:], in0=mk[:], in1=cum[:], op=ALU.mult), [r_mk, r_cum], [r_tmp32])
                fw.op(V, lambda e, k=k: e.tensor_reduce(out=pos[:, k:k + 1], in_=tmp32[:], axis=AX.X, op=ALU.add), [r_tmp32], [r_pos])
                fw.op(V, lambda e, mk=mk: e.tensor_tensor(out=tmp32[:], in0=mk[:], in1=iot[:], op=ALU.mult), [r_mk, r_iot], [r_tmp32])
                fw.op(V, lambda e, k=k: e.tensor_reduce(out=eb[:, k:k + 1], in_=tmp32[:], axis=AX.X, op=ALU.add), [r_tmp32], [r_eb])
            fw.op(V, lambda e: e.tensor_scalar(out=ovf[:], in0=pos[:], scalar1=float(CAP) - 0.5, scalar2=1.0e6, op0=ALU.is_gt,
                                               op1=ALU.mult), [r_pos], [r_ovf])
            fw.op(V, lambda e: e.tensor_tensor(out=dstf[:], in0=pos[:], in1=eb[:], op=ALU.add), [r_pos, r_eb], [r_dstf])
            fw.op(V, lambda e: e.tensor_tensor(out=dstf[:], in0=dstf[:], in1=ovf[:], op=ALU.add), [r_dstf, r_ovf], [r_dstf])
            di, r_di = dring.next()
            fw.op(V, lambda e, di=di: e.tensor_copy(out=di[:], in_=dstf[:]), [r_dstf], [r_di])
            for k in range(2):
                fw.op("pool", lambda e, di=di, k=k, tt=tt: [e.indirect_dma_start(
                    out=tab_i, out_offset=bass.IndirectOffsetOnAxis(ap=di[:, k:k + 1], axis=0),
                    in_=tke[:, tt, k, :], in_offset=None, bounds_check=fw.bc_reg(e, NE * CAP - 1), oob_is_err=False)],
                    [r_di, r_tke], [r_tab_i], dma=1)
                fw.op("pool", lambda e, di=di, k=k, wt2=wt2: [e.indirect_dma_start(
                    out=tab_w, out_offset=bass.IndirectOffsetOnAxis(ap=di[:, k:k + 1], axis=0),
                    in_=wt2[:, 2 * k:2 * k + 2], in_offset=None, bounds_check=fw.bc_reg(e, NE * CAP - 1), oob_is_err=False)],
                    [r_di, r_wt2], [r_tab_w], dma=1)
            if tt % 4 == 3:
                fw.emit()
        fw.end_phase()
        if stop_after == 4:
            return _finish(nc, fw, dbg, out, r_out)

        fw.begin_phase()
        sc = nt_scratch()
        xg = fw.ring("xg", 2, [128, D], F32)
        gT, r_gT = fw.buf("gT", [128, KC, CAP], BF16)
        actT, r_actT = fw.buf("actT", [128, FF // 128, CAP], BF16)
        wring = fw.ring("wbC", 3, [128, 8192], BF16)
        idxr = fw.ring("idx", 2 * NST, [128, 2], I32)
        wtr = fw.ring("wtc", 2 * NST, [128, 2], F32)
        sgr = fw.ring("sgr", 2, [128, CAP], F32)
        ypc = fw.ring("ypc", 3, [128, 1024], F32)
        for ex in range(NE):
            idxs = []
            for stl in range(NST):
                ix, r_ix = idxr.next()
                w1, r_w1 = wtr.next()
                r0 = ex * CAP + stl * 128
                dma(fw, "sp", ix[:], tab_i[r0:r0 + 128, :], [r_tab_i], [r_ix])
                dma(fw, "sp", w1[:], tab_w[r0:r0 + 128, :], [r_tab_w], [r_w1])
                idxs.append((ix, r_ix, w1, r_w1))
                xr, r_xr = xg.next()
                fw.op("pool", lambda e, xr=xr, ix=ix: [e.indirect_dma_start(
                    out=xr[:], out_offset=None, in_=xnew_d,
                    in_offset=bass.IndirectOffsetOnAxis(ap=ix[:, 0:1], axis=0))],
                    [r_ix], [r_xr], dma=1)
                norm_transpose(xr, r_xr, 128, A2, SH2, lambda kc, stl=stl: gT[:, kc, stl * 128:(stl + 1) * 128], r_gT,
                               sc, banks[4:8])
            for fb in range(4):
                wg_, r_wg = wring.next()
                wgv = wg_[:].rearrange("p (k n) -> p k n", k=KC)
                dma(fw, "pool", wgv, w_eg[ex].rearrange("(kc p) n -> p kc n", p=128)[:, :, fb * 256:(fb + 1) * 256], [], [r_wg])
                wu_, r_wu = wring.next()
                wuv = wu_[:].rearrange("p (k n) -> p k n", k=KC)
                dma(fw, "pool", wuv, w_eu[ex].rearrange("(kc p) n -> p kc n", p=128)[:, :, fb * 256:(fb + 1) * 256], [], [r_wu])
                for sub in range(2):
                    fc = fb * 2 + sub
                    bg, r_bg = banks[(fc * 2) % 4]
                    bu, r_bu = banks[(fc * 2 + 1) % 4]
                    mm_group(fw, bg[:, 0:CAP], [(wgv[:, kc, sub * 128:(sub + 1) * 128], gT[:, kc, :]) for kc in range(KC)],
                             [r_wg, r_gT], [r_bg])
                    mm_group(fw, bu[:, 0:CAP], [(wuv[:, kc, sub * 128:(sub + 1) * 128], gT[:, kc, :]) for kc in range(KC)],
                             [r_wu, r_gT], [r_bu])
                    sg_, r_sg_ = sgr.next()
                    fw.op("act", lambda e, bg=bg, sg_=sg_: e.activation(out=sg_[:], in_=bg[:, 0:CAP], func=AF.Silu), [r_bg], [r_sg_])
                    fw.op("dve", lambda e, bu=bu, sg_=sg_, fc=fc: e.tensor_tensor(out=actT[:, fc, :], in0=bu[:, 0:CAP], in1=sg_[:],
                                                                                op=ALU.mult), [r_bu, r_sg_], [r_actT])
            for db in range(4):
                wd_, r_wd = wring.next()
                wdv = wd_[:].rearrange("p (k n) -> p k n", k=FF // 128)
                dma(fw, "pool", wdv, w_ed[ex].rearrange("(kc p) n -> p kc n", p=128)[:, :, db * 1024:(db + 1) * 1024], [], [r_wd])
                for stl in range(NST):
                    ix, r_ix, w1, r_w1 = idxs[stl]
                    yp, r_yp = ypc.next()
                    for hf in range(2):
                        bk, r_bk = banks[4 + (db * 2 * NST + stl * 2 + hf) % 4]
                        mm_group(fw, bk[:, :], [(actT[:, kc, stl * 128:(stl + 1) * 128], wdv[:, kc, hf * 512:(hf + 1) * 512])
                                               for kc in range(FF // 128)], [r_wd, r_actT], [r_bk])
                        if hf == 0:
                            fw.op("act", lambda e, bk=bk, yp=yp, w1=w1: e.activation(
                                out=yp[:, 0:512], in_=bk[:, :], func=AF.Identity, scale=w1[:, 0:1]), [r_bk, r_w1], [r_yp])
                        else:
                            fw.op("dve", lambda e, bk=bk, yp=yp, w1=w1: e.tensor_scalar(
                                out=yp[:, 512:1024], in0=bk[:, :], scalar1=w1[:, 0:1], scalar2=None, op0=ALU.mult),
                                [r_bk, r_w1], [r_yp])
                    fw.op("pool", lambda e, yp=yp, ix=ix, db=db: [e.indirect_dma_start(
                        out=ybufs[db], out_offset=bass.IndirectOffsetOnAxis(ap=ix[:, 1:2], axis=0),
                        in_=yp[:], in_offset=None, bounds_check=fw.bc_reg(e, 2 * OWN - 1), oob_is_err=False)],
                        [r_yp, r_ix], [], dma=1, sem_res=r_yp)
            if ex % 4 == 3:
                fw.emit()
        fw.end_phase()
        if stop_after == 5:
            return _finish(nc, fw, dbg, out, r_out)

        fw.begin_phase()
        ga2, r_ga2 = fw.buf("ga2", [128, D], F32)
        gfr, r_gfr = fw.buf("gfr", [128, D], F32)
        dma(fw, "sp", ga2[:], mod_all[0:1, 5 * D:6 * D].broadcast_to([128, D]), [], [r_ga2])
        dma(fw, "sp", gfr[:], gf_rep_in, [], [r_gfr])
        xr_ = fw.ring("xd", 2, [128, D], F32)
        y1r = fw.ring("y1", 2, [128, D], F32)
        y2r = fw.ring("y2", 2, [128, D], F32)
        junk, r_junk = fw.buf("junkd", [128, D], BF16)
        ssr = fw.ring("ssd", 2, [128, 1], F32)
        for tt in range(16):
            xr, r_xr = xr_.next()
            y1, r_y1 = y1r.next()
            y2, r_y2 = y2r.next()
            dma(fw, "sp", xr[:], xnew_d[tt * 128:(tt + 1) * 128, :], [], [r_xr])
            fw.op("sp", lambda e, y1=y1, tt=tt: [e.dma_start(out=y1[:, i * 1024:(i + 1) * 1024],
                                                             in_=ybufs[i][tt * 128:(tt + 1) * 128, :]) for i in range(4)],
                  [], [r_y1], dma=4)
            fw.op("sp", lambda e, y2=y2, tt=tt: [e.dma_start(out=y2[:, i * 1024:(i + 1) * 1024],
                                                             in_=ybufs[i][OWN + tt * 128:OWN + (tt + 1) * 128, :]) for i in range(4)],
                  [], [r_y2], dma=4)
            fw.op("pool", lambda e, y1=y1, y2=y2: e.tensor_tensor(out=y1[:], in0=y1[:], in1=y2[:], op=ALU.add), [r_y1, r_y2], [r_y1])
            fw.op("dve", lambda e, y1=y1: e.tensor_tensor(out=y1[:], in0=y1[:], in1=ga2[:], op=ALU.mult), [r_y1, r_ga2], [r_y1])
            fw.op("pool", lambda e, y1=y1, xr=xr: e.tensor_tensor(out=xr[:], in0=xr[:], in1=y1[:], op=ALU.add), [r_y1, r_xr], [r_xr])
            ss, r_ss = ssr.next()
            fw.op("act", lambda e, xr=xr, ss=ss: e.activation(out=junk[:], in_=xr[:], func=AF.Square, accum_out=ss[:]),
                  [r_xr], [r_junk, r_ss])
            fw.op("act", lambda e, ss=ss: e.activation(out=ss[:], in_=ss[:], func=AF.Sqrt, scale=1.0 / D, bias=epst[:, 0:1]),
                  [r_ss, r_eps], [r_ss])
            fw.op("dve", lambda e, ss=ss: e.reciprocal(out=ss[:], in_=ss[:]), [r_ss], [r_ss])
            fw.op("dve", lambda e, xr=xr, ss=ss, y2=y2: e.scalar_tensor_tensor(
                out=y2[:], in0=xr[:], scalar=ss[:, 0:1], in1=gfr[:], op0=ALU.mult, op1=ALU.mult), [r_xr, r_ss, r_gfr, r_y2], [r_y2])
            store(fw, "pool", out[tt * 128:(tt + 1) * 128, :], y2[:], r_y2)
        fw.end_phase()
        if debug:
            fw.begin_phase()
            def ddump(name, src, shape, dt):
                d = nc.dram_tensor(name, list(shape), dt, kind="ExternalOutput").ap()
                dma(fw, "sp", d, src, [], [fw.res(name, True)])
            ddump("dbg_qT", qT_d[:, :, 0:512], [NQH, 128, 512], BF16)
            ddump("dbg_uT", uT_d[0:2], [2, 128, UW], F32)
            ddump("dbg_gates", gates_d[0:2, :, 0:512], [2, 128, 512], F32)
            ddump("dbg_attnT", attnT_d[:, :, 0:512], [16, 128, 512], BF16)
            ddump("dbg_csT", csT_d[:, :, 0:512], [16, 128, 512], BF16)
            ddump("dbg_xnew", xnew_d[0:256, :], [256, D], F32)
            ddump("dbg_tab_i", tab_i, [NE * CAP, 2], I32)
            ddump("dbg_tab_w", tab_w, [NE * CAP, 2], F32)
            ddump("dbg_ybuf0", ybufs[0][0:128, :], [128, 1024], F32)
            ddump("dbg_ybuf1", ybufs[0][OWN:OWN + 128, :], [128, 1024], F32)
            fw.end_phase()
        return _finish(nc, fw, dbg, out, r_out)


def _dump(nc, fw, name, t, r_t, shape, dt):
    d = nc.dram_tensor(name, list(shape), dt, kind="ExternalOutput").ap()
    r_d = fw.res(name, True)
    dma(fw, "sp", d, t[:], [r_t], [r_d])
    return d


def _finish(nc, fw, dbg, out, r_out):
    return nc


def _rope_tables(hf):
    inv = (10000.0 ** (-np.arange(0, 64, 2, dtype=np.float32) / 64.0)).astype(np.float32)
    tok = np.concatenate([np.arange(hf * OWN, (hf + 1) * OWN), np.arange((1 - hf) * OWN, (2 - hf) * OWN)])
    row = (tok // 64).astype(np.float32)
    col = (tok % 64).astype(np.float32)
    C = np.ones((128, NKEY), np.float32)
    S = np.zeros((128, NKEY), np.float32)
    for d in range(128):
        a = d // 64
        r = d % 64
        j = r % 32
        half = r // 32
        pos = row if a == 0 else col
        ang = (pos * inv[j]).astype(np.float32)
        C[d, :SEQ] = np.cos(ang)
        S[d, :SEQ] = np.sin(ang) * (-1.0 if half == 0 else 1.0)
    return C, S


def _consts():
    ident = np.eye(128, dtype=np.float32)
    pm = np.zeros((128, 128), np.float32)
    for m in range(128):
        r = m % 64
        partner = m + 32 if (r // 32) == 0 else m - 32
        pm[partner, m] = 1.0
    tril = np.zeros((128, 128), np.float32)
    for k in range(128):
        tril[k, k + 1:] = 1.0
    iota_cap = np.tile((np.arange(NE, dtype=np.float32) * CAP)[None, :], (128, 1))
    tok_ent = np.zeros((128, 16, 2, 2), np.int32)
    for tt in range(16):
        t = tt * 128 + np.arange(128)
        for k in range(2):
            tok_ent[:, tt, k, 0] = t
            tok_ent[:, tt, k, 1] = k * OWN + t
    tab_i_init = np.zeros((NE * CAP, 2), np.int32)
    tab_i_init[:, 1] = 1 << 24
    tab_w_init = np.zeros((NE * CAP, 2), np.float32)
    return dict(ident=ident, pmat=pm, tril=tril, iota_cap=iota_cap, tok_ent=tok_ent,
                tab_i_init=tab_i_init, tab_w_init=tab_w_init)


_NC_CACHE = {}


def kernel(x, c, ctx, c_ctx, norm1_g, w_mod, b_mod, w_in, q_norm_g, k_norm_g, w_attn_out,
           conv_dw_w, conv_dw_b, conv_ln_g, conv_ln_b, w_conv_out, w_out, norm2_g,
           w_router_group, b_router_group, w_router_expert, b_router_expert,
           w_exp_gate, w_exp_up, w_exp_down, norm_f_g, _debug=False, _stop_after=None):
    f = lambda a: np.ascontiguousarray(np.asarray(a, dtype=np.float32))
    x, c, ctx, c_ctx = f(x), f(c), f(ctx), f(c_ctx)
    fm = lambda v: np.ascontiguousarray(f(v).reshape(-1, 128).T)
    consts = _consts()
    shared = dict(
        qk_g=np.ascontiguousarray(np.stack([f(q_norm_g)[0], f(k_norm_g)[0]], axis=1)),
        g1=fm(norm1_g[0]), g2=fm(norm2_g[0]),
        gf_rep=np.ascontiguousarray(np.tile(f(norm_f_g)[None, :], (128, 1))),
        bmod2=np.ascontiguousarray(np.tile(f(b_mod)[0][None, :], (2, 1))),
        w_mod=f(w_mod)[0], w_in=f(w_in)[0], w_attn_out=f(w_attn_out)[0], w_conv_out=f(w_conv_out)[0],
        w_out=f(w_out)[0],
        conv_w=np.ascontiguousarray(f(conv_dw_w)[0, :, 0, :].T.reshape(16, 128, TAPS).transpose(1, 0, 2)),
        conv_b=fm(conv_dw_b[0]), ln_g=fm(conv_ln_g[0]), ln_b=fm(conv_ln_b[0]),
        w_router=np.ascontiguousarray(np.concatenate([f(w_router_group)[0], f(w_router_expert)[0]], axis=1)),
        b_router_rep=np.ascontiguousarray(np.tile(np.concatenate([f(b_router_group)[0], f(b_router_expert)[0]])[None, :],
                                                  (128, 1))),
        w_exp_gate=f(w_exp_gate)[0], w_exp_up=f(w_exp_up)[0], w_exp_down=f(w_exp_down)[0],
        **consts,
    )
    in_maps = []
    for core in range(8):
        b, hf = core // 2, core % 2
        own = x[b, hf * OWN:(hf + 1) * OWN]
        oth = x[b, (1 - hf) * OWN:(2 - hf) * OWN]
        halo = np.zeros((2 * HALO, D), np.float32)
        mask = np.zeros((128, 2 * HALO), np.float32)
        if hf == 1:
            halo[:HALO] = x[b, OWN - HALO:OWN]
            mask[:, :HALO] = 1.0
        else:
            halo[HALO:] = x[b, OWN:OWN + HALO]
            mask[:, HALO:] = 1.0
        C, S = _rope_tables(hf)
        cv = np.ascontiguousarray(np.stack([c[b].reshape(KC, 128).T, c_ctx.reshape(KC, 128).T], axis=2))
        m = dict(shared)
        m.update(x_own=np.ascontiguousarray(own), x_oth=np.ascontiguousarray(oth), ctx_b=np.ascontiguousarray(ctx[b]),
                 x_halo=halo, halo_mask=mask, cvec=cv, rope_c=C, rope_s=S)
        in_maps.append(m)
    key = (_debug, _stop_after)
    if key not in _NC_CACHE:
        _NC_CACHE[key] = build_nc(debug=_debug, stop_after=_stop_after)
    nc = _NC_CACHE[key]
    res = run_bass_kernel_spmd(nc, in_maps, core_ids=list(range(8)))
    if _debug:
        return res
    outp = np.empty((4, SEQ, D), np.float32)
    for core in range(8):
        b, hf = core // 2, core % 2
        outp[b, hf * OWN:(hf + 1) * OWN] = np.asarray(res.results[core]["out"], dtype=np.float32)
    return outp
```

```python
import contextlib
import numpy as np
import concourse.bass as bass
import concourse.mybir as mybir
from concourse.bass_utils import run_bass_kernel_spmd

F32 = mybir.dt.float32
BF16 = mybir.dt.bfloat16
I32 = mybir.dt.int32
ALU = mybir.AluOpType
AF = mybir.ActivationFunctionType
AX = mybir.AxisListType

D = 4096
KC = D // 128
SEQ = 4096
OWN = 2048
NCTX = 256
NKEY = SEQ + NCTX
NKT = NKEY // 128
HD = 128
NQH = 16
NKVH = 4
CW = 2048
TAPS = 31
Q_OFF, K_OFF, V_OFF, GLU_OFF, GATE_OFF, IN_W = 0, 2048, 2560, 3072, 7168, 15360
NE = 32
FF = 1024
CAP = 512
NST = CAP // 128
EPS = 1e-6
HALO = 16
UW = OWN + 2 * HALO

EPOCH = 12000
SAME_ENGINE_SYNC = True


class Sem:
    def __init__(self, fw, name, step):
        self.fw, self.name, self.step = fw, name, step
        self.count = 0
        self.handles = []

    def _handle(self, ep):
        while len(self.handles) <= ep:
            h = self.fw.stack.enter_context(self.fw.nc.semaphore(f"{self.name}_{len(self.handles)}"))
            self.handles.append(h)
        return self.handles[ep]

    def next(self, n=1):
        ep = self.count // EPOCH
        if (self.count + n - 1) // EPOCH != ep:
            self.count = (ep + 1) * EPOCH
            ep += 1
        self.count += n
        idx = self.count - ep * EPOCH
        return self._handle(ep), (self, ep, idx * self.step)

    def last_token(self):
        if self.count == 0:
            return None
        ep = (self.count - 1) // EPOCH
        return (self, ep, (self.count - ep * EPOCH) * self.step)


class Res:
    def __init__(self, name, persistent):
        self.name = name
        self.persistent = persistent
        self.last_write = None
        self.readers = []
        self.dma_sem = None


class FW:
    ENGS = ("pe", "act", "dve", "pool", "sp")

    def __init__(self, nc, stack):
        self.nc, self.stack = nc, stack
        self.ops = {e: [] for e in self.ENGS}
        self.esem = {e: Sem(self, f"s_{e}", 1) for e in ("pe", "act", "dve", "pool")}
        self.waited = {e: {} for e in self.ENGS}
        self.sem_pool = []
        self.all_dma_sems = []
        self.phase_res = []
        self.ph = None

    def begin_phase(self):
        self.ph = contextlib.ExitStack()
        self.phase_res = []

    def end_phase(self):
        self.barrier()
        self.emit()
        for r in self.phase_res:
            if r.dma_sem is not None:
                self.sem_pool.append(r.dma_sem)
                r.dma_sem = None
        self.phase_res = []
        self.ph.close()
        self.ph = None

    def res(self, name="r", persistent=False):
        r = Res(name, persistent)
        if not persistent:
            self.phase_res.append(r)
        return r

    def sb(self, name, shape, dtype, persistent=False, stack=None):
        st = stack if stack is not None else (self.stack if persistent else self.ph)
        self.nsb = getattr(self, "nsb", 0) + 1
        t = st.enter_context(self.nc.sbuf_tensor(f"sb{self.nsb}_{name}", list(shape), dtype))
        return t

    def buf(self, name, shape, dtype, persistent=False, stack=None):
        return self.sb(name, shape, dtype, persistent, stack), self.res(name, persistent or stack is not None)

    def ring(self, name, n, shape, dtype):
        return Ring([self.buf(f"{name}{i}", shape, dtype) for i in range(n)])

    def _waits_for(self, eng, toks):
        out = []
        for tok in toks:
            if tok is None:
                continue
            sem, ep, val = tok
            key = (id(sem), ep)
            if self.waited[eng].get(key, 0) >= val:
                continue
            self.waited[eng][key] = val
            out.append((sem.handles[ep], val))
        return out

    def op(self, eng, fn, reads=(), writes=(), dma=0, sem_res=None):
        toks = []
        for r in reads:
            toks.append(r.last_write)
        for w in writes:
            toks.append(w.last_write)
            toks.extend(w.readers)
        if dma:
            anchor = sem_res if sem_res is not None else writes[0]
            if anchor.dma_sem is None:
                if self.sem_pool:
                    anchor.dma_sem = self.sem_pool.pop()
                else:
                    anchor.dma_sem = Sem(self, f"d{len(self.all_dma_sems)}", 16)
                    self.all_dma_sems.append(anchor.dma_sem)
            handle, tok = anchor.dma_sem.next(dma)
            own = None
        else:
            handle, tok = self.esem[eng].next(1)
            own = self.esem[eng]
        if own is not None and not (SAME_ENGINE_SYNC and eng in ("act", "dve", "pool")):
            toks = [t for t in toks if t is None or t[0] is not own]
        waits = self._waits_for(eng, toks)
        self.ops[eng].append((waits, fn, handle, dma))
        for r in reads:
            r.readers.append(tok)
        for w in writes:
            w.last_write = tok
            w.readers = []
        return tok

    def bc_reg(self, e, val):
        if val not in self._regs:
            self._regs[val] = e.to_reg(val)
        return self._regs[val]

    def barrier(self):
        toks = [s.last_token() for s in self.esem.values()]
        toks += [s.last_token() for s in self.all_dma_sems]
        for eng in self.ENGS:
            waits = self._waits_for(eng, toks)
            self.ops[eng].append((waits, None, None, 0))

    def emit(self):
        nc = self.nc
        ops = self.ops
        self.ops = {e: [] for e in self.ENGS}
        with nc.Block() as block:
            def run(engname):
                def body(e):
                    self._regs = {}
                    for waits, fn, handle, dma in ops[engname]:
                        for h, v in waits:
                            e.wait_ge(h, v)
                        if fn is None:
                            continue
                        r = fn(e)
                        if dma:
                            assert isinstance(r, (list, tuple)) and len(r) == dma, (len(r), dma)
                            for ins in r:
                                ins.then_inc(handle, 16)
                        else:
                            r.then_inc(handle, 1)
                return body
            block.tensor(run("pe"))
            block.scalar(run("act"))
            block.vector(run("dve"))
            block.gpsimd(run("pool"))
            block.sync(run("sp"))


class Ring:
    def __init__(self, items):
        self.items = items
        self.i = 0

    def next(self):
        it = self.items[self.i % len(self.items)]
        self.i += 1
        return it


def mm_group(fw, out_ap, pairs, reads, writes):
    pairs = list(pairs)

    def fn(e):
        n = len(pairs)
        last = None
        for i, (l, r) in enumerate(pairs):
            last = e.matmul(out_ap, lhsT=l, rhs=r, start=(i == 0), stop=(i == n - 1))
        return last
    return fw.op("pe", fn, reads, writes)


def dma(fw, eng, out, in_, reads, writes, sem_res=None):
    return fw.op(eng, lambda e: [e.dma_start(out=out, in_=in_)], reads, writes, dma=1, sem_res=sem_res)


def store(fw, eng, out, in_, r_src):
    return fw.op(eng, lambda e: [e.dma_start(out=out, in_=in_)], [r_src], [], dma=1, sem_res=r_src)


def build_nc(debug=False, stop_after=None):
    nc = bass.Bass("TRN2", target_bir_lowering=False)

    def din(name, shape, dt=F32):
        return nc.dram_tensor(name, list(shape), dt, kind="ExternalInput").ap()

    def dscr(name, shape, dt=F32):
        return nc.dram_tensor(name, list(shape), dt, kind="Internal").ap()

    x_own = din("x_own", [OWN, D])
    x_oth = din("x_oth", [OWN, D])
    ctx_b = din("ctx_b", [NCTX, D])
    x_halo = din("x_halo", [2 * HALO, D])
    halo_mask = din("halo_mask", [128, 2 * HALO])
    cvec = din("cvec", [128, KC, 2])
    rope_c = din("rope_c", [128, NKEY])
    rope_s = din("rope_s", [128, NKEY])
    qk_g = din("qk_g", [128, 2])
    ident_in = din("ident", [128, 128])
    pmat_in = din("pmat", [128, 128])
    tril_in = din("tril", [128, 128])
    g1_in = din("g1", [128, KC])
    g2_in = din("g2", [128, KC])
    gf_rep_in = din("gf_rep", [128, D])
    bmod2 = din("bmod2", [2, 6 * D])
    w_mod = din("w_mod", [D, 6 * D])
    w_in = din("w_in", [D, IN_W])
    w_ao = din("w_attn_out", [NQH * HD, D])
    w_co = din("w_conv_out", [CW, D])
    w_out = din("w_out", [D, D])
    cw_in = din("conv_w", [128, 16, TAPS])
    cb_in = din("conv_b", [128, 16])
    lng_in = din("ln_g", [128, 16])
    lnb_in = din("ln_b", [128, 16])
    w_r = din("w_router", [D, 36])
    b_r_rep = din("b_router_rep", [128, 36])
    iota_cap = din("iota_cap", [128, NE])
    tok_ent = din("tok_ent", [128, 16, 2, 2], I32)
    tab_i_init = din("tab_i_init", [NE * CAP, 2], I32)
    tab_w_init = din("tab_w_init", [NE * CAP, 2])
    w_eg = din("w_exp_gate", [NE, D, FF])
    w_eu = din("w_exp_up", [NE, D, FF])
    w_ed = din("w_exp_down", [NE, FF, D])
    out = nc.dram_tensor("out", [OWN, D], F32, kind="ExternalOutput").ap()

    mod_all = dscr("mod_all", [2, 6 * D])
    qT_d = dscr("qT_d", [NQH, 128, OWN], BF16)
    uT_d = dscr("uT_d", [16, 128, UW])
    gates_d = dscr("gates_d", [64, 128, OWN])
    attnT_d = dscr("attnT_d", [16, 128, OWN], BF16)
    csT_d = dscr("csT_d", [16, 128, OWN], BF16)
    xnew_d = dscr("xnew_d", [OWN, D])
    tab_i = dscr("tab_i", [NE * CAP, 2], I32)
    tab_w = dscr("tab_w", [NE * CAP, 2])
    ybufs = [dscr(f"ybuf{i}", [2 * OWN, 1024]) for i in range(4)]

    w_mod_v = w_mod.rearrange("(kc p) n -> p kc n", p=128)
    w_in_v = w_in.rearrange("(kc p) n -> p kc n", p=128)
    w_ao_v = w_ao.rearrange("(kc p) n -> p kc n", p=128)
    w_co_v = w_co.rearrange("(kc p) n -> p kc n", p=128)
    w_out_v = w_out.rearrange("(kc p) n -> p kc n", p=128)
    w_r_v = w_r.rearrange("(kc p) n -> p kc n", p=128)

    dbg = {}

    with contextlib.ExitStack() as st:
        fw = FW(nc, st)
        r_mod_all = fw.res("mod_all", True)
        r_qT_d = fw.res("qT_d", True)
        r_uT_d = fw.res("uT_d", True)
        r_gates_d = fw.res("gates_d", True)
        r_attnT_d = fw.res("attnT_d", True)
        r_csT_d = fw.res("csT_d", True)
        r_xnew_d = [fw.res(f"xnew_d{g}", True) for g in range(4)]
        r_tab_i = fw.res("tab_i", True)
        r_tab_w = fw.res("tab_w", True)
        r_ybuf = fw.res("ybuf", True)
        r_out = fw.res("out", True)

        ident, r_ident = fw.buf("ident", [128, 128], F32, True)
        identb, r_identb = fw.buf("identb", [128, 128], BF16, True)
        pmat, r_pmat = fw.buf("pmat", [128, 128], F32, True)
        ones_f, r_ones_f = fw.buf("ones_f", [128, 128], F32, True)
        ones_b, r_ones_b = fw.buf("ones_b", [128, 128], BF16, True)
        epst, r_eps = fw.buf("epst", [128, 1], F32, True)
        qkg, r_qkg = fw.buf("qkg", [128, 2], F32, True)
        modv, r_modv = fw.buf("modv", [128, 6, KC], F32, True)
        A1, SH1, A1C, SH1C, A2, SH2 = range(6)

        banks = []
        for i in range(8):
            t = st.enter_context(nc.psum_tensor(f"bank{i}", [128, 512], F32))
            banks.append((t, fw.res(f"bank{i}", True)))

        def norm_transpose(rows, r_rows, nrows, mv_a, mv_s, dst_fn, r_dst, sc, pbanks, evac_engs=("act", "dve")):
            junk, r_junk, ss, r_ss, rstd, r_rstd, dg, r_dg, ss2, r_ss2 = sc
            for hh in range(2):
                fw.op("act", lambda e, hh=hh: e.activation(out=junk[:nrows, :], in_=rows[:nrows, hh * 2048:(hh + 1) * 2048],
                                                           func=AF.Square, accum_out=ss2[:nrows, hh:hh + 1]),
                      [r_rows], [r_junk, r_ss2])
            fw.op("dve", lambda e: e.tensor_tensor(out=ss[:nrows, :], in0=ss2[:nrows, 0:1], in1=ss2[:nrows, 1:2], op=ALU.add),
                  [r_ss2], [r_ss])
            fw.op("act", lambda e: e.activation(out=rstd[:nrows, :], in_=ss[:nrows, :], func=AF.Sqrt,
                                                scale=1.0 / D, bias=epst[:nrows, 0:1]), [r_ss, r_eps], [r_rstd])
            fw.op("dve", lambda e: e.reciprocal(out=rstd[:nrows, :], in_=rstd[:nrows, :]), [r_rstd], [r_rstd])
            fw.op("dve", lambda e: e.tensor_scalar(out=dg[:nrows, :nrows], in0=ident[:nrows, :nrows],
                                                   scalar1=rstd[:nrows, 0:1], scalar2=None, op0=ALU.mult),
                  [r_rstd, r_ident], [r_dg])
            for q in range(KC // 4):
                bk, r_bk = pbanks[q % len(pbanks)]

                def fn(e, q=q, bk=bk):
                    last = None
                    for j in range(4):
                        kc = q * 4 + j
                        last = e.matmul(bk[:, j * 128:j * 128 + nrows], lhsT=rows[:nrows, kc * 128:(kc + 1) * 128],
                                        rhs=dg[:nrows, :nrows], start=True, stop=True)
                    return last
                fw.op("pe", fn, [r_rows, r_dg], [r_bk])
                for j in range(4):
                    kc = q * 4 + j
                    eng = evac_engs[kc % len(evac_engs)]
                    if eng == "act":
                        fw.op("act", lambda e, kc=kc, j=j, bk=bk: e.activation(
                            out=dst_fn(kc), in_=bk[:, j * 128:j * 128 + nrows], func=AF.Identity,
                            scale=modv[:, mv_a, kc:kc + 1], bias=modv[:, mv_s, kc:kc + 1]),
                            [r_bk, r_modv], [r_dst])
                    else:
                        fw.op("dve", lambda e, kc=kc, j=j, bk=bk: e.tensor_scalar(
                            out=dst_fn(kc), in0=bk[:, j * 128:j * 128 + nrows],
                            scalar1=modv[:, mv_a, kc:kc + 1], scalar2=modv[:, mv_s, kc:kc + 1],
                            op0=ALU.mult, op1=ALU.add), [r_bk, r_modv], [r_dst])

        def nt_scratch():
            junk, r_junk = fw.buf("nt_junk", [128, D // 2], BF16)
            ss, r_ss = fw.buf("nt_ss", [128, 1], F32)
            ss2, r_ss2 = fw.buf("nt_ss2", [128, 2], F32)
            rstd, r_rstd = fw.buf("nt_rstd", [128, 1], F32)
            dg, r_dg = fw.buf("nt_dg", [128, 128], F32)
            return (junk, r_junk, ss, r_ss, rstd, r_rstd, dg, r_dg, ss2, r_ss2)

        fw.begin_phase()
        dma(fw, "sp", ident[:], ident_in, [], [r_ident])
        dma(fw, "sp", pmat[:], pmat_in, [], [r_pmat])
        dma(fw, "sp", qkg[:], qk_g, [], [r_qkg])
        fw.op("dve", lambda e: e.memset(ones_f[:], 1.0), [], [r_ones_f])
        fw.op("dve", lambda e: e.memset(ones_b[:], 1.0), [], [r_ones_b])
        fw.op("dve", lambda e: e.memset(epst[:], EPS), [], [r_eps])
        fw.op("dve", lambda e: e.tensor_copy(out=identb[:], in_=ident[:]), [r_ident], [r_identb])
        dma(fw, "sp", tab_i, tab_i_init, [], [r_tab_i])
        dma(fw, "sp", tab_w, tab_w_init, [], [r_tab_w])

        cv, r_cv = fw.buf("cv", [128, KC, 2], F32)
        sg, r_sg = fw.buf("sgc", [128, KC, 2], F32)
        dma(fw, "sp", cv[:], cvec, [], [r_cv])
        fw.op("act", lambda e: e.activation(out=sg[:], in_=cv[:], func=AF.Sigmoid), [r_cv], [r_sg])
        fw.op("dve", lambda e: e.tensor_tensor(out=sg[:], in0=sg[:], in1=cv[:], op=ALU.mult), [r_sg, r_cv], [r_sg])
        bmr = fw.ring("bm", 2, [2, 512], F32)
        mrr = fw.ring("modrow", 2, [2, 512], F32)
        wring = fw.ring("wm", 3, [128, 8, 512], F32)
        NCH = 6 * D // 512
        for ch in range(NCH):
            bk, r_bk = banks[ch % 2]
            tiles = []
            for k4 in range(4):
                wt, r_wt = wring.next()
                dma(fw, "sp", wt[:], w_mod_v[:, k4 * 8:(k4 + 1) * 8, ch * 512:(ch + 1) * 512], [], [r_wt])
                tiles.append((wt, r_wt))

                def fn(e, k4=k4, wt=wt, bk=bk):
                    last = None
                    for j in range(8):
                        kc = k4 * 8 + j
                        last = e.matmul(bk[0:2, :], lhsT=sg[:, kc, :], rhs=wt[:, j, :],
                                        start=(kc == 0), stop=(kc == KC - 1))
                    return last
                fw.op("pe", fn, [r_sg, r_wt], [r_bk])
            bm, r_bm = bmr.next()
            dma(fw, "sp", bm[:], bmod2[:, ch * 512:(ch + 1) * 512], [], [r_bm])
            mr, r_mr = mrr.next()
            fw.op("dve", lambda e, bk=bk, bm=bm, mr=mr: e.tensor_tensor(out=mr[:], in0=bk[0:2, :], in1=bm[:], op=ALU.add),
                  [r_bk, r_bm], [r_mr])
            store(fw, "sp", mod_all[:, ch * 512:(ch + 1) * 512], mr[:], r_mr)
        fw.barrier()
        mraw, r_mraw = fw.buf("mraw", [128, 6, KC], F32)
        g12, r_g12 = fw.buf("g12", [128, 2, KC], F32)
        dma(fw, "sp", g12[:, 0, :], g1_in, [], [r_g12])
        dma(fw, "sp", g12[:, 1, :], g2_in, [r_g12], [r_g12])

        def ld_mod(slot, row, j):
            src = mod_all[row, j * D:(j + 1) * D].rearrange("(kc p) -> p kc", p=128)
            fw.op("sp", lambda e: [e.dma_start(out=mraw[:, slot, :], in_=src, allow_slow_non_contiguous=True)],
                  [r_mraw], [r_mraw], dma=1)
        ld_mod(0, 0, 1)
        ld_mod(1, 0, 0)
        ld_mod(2, 1, 1)
        ld_mod(3, 1, 0)
        ld_mod(4, 0, 4)
        ld_mod(5, 0, 3)
        for (dst_a, dst_s, s_sc, s_sh, gi) in ((A1, SH1, 0, 1, 0), (A1C, SH1C, 2, 3, 0), (A2, SH2, 4, 5, 1)):
            fw.op("dve", lambda e, dst_a=dst_a, s_sc=s_sc, gi=gi: e.scalar_tensor_tensor(
                out=modv[:, dst_a, :], in0=mraw[:, s_sc, :], scalar=1.0, in1=g12[:, gi, :],
                op0=ALU.add, op1=ALU.mult), [r_mraw, r_g12], [r_modv])
            fw.op("dve", lambda e, dst_s=dst_s, s_sh=s_sh: e.tensor_copy(out=modv[:, dst_s, :], in_=mraw[:, s_sh, :]),
                  [r_mraw], [r_modv])
        if debug:
            dbg["modv"] = _dump(nc, fw, "dbg_modv", modv, r_modv, [128, 6, KC], F32)
        fw.end_phase()
        if stop_after == 0:
            return _finish(nc, fw, dbg, out, r_out)

        kv_stack = contextlib.ExitStack()
        KT, r_KT = fw.buf("KT", [128, NKVH, NKEY], BF16, stack=kv_stack)
        VA, r_VA = fw.buf("VA", [128, NKT, NKVH, HD + 2], BF16, stack=kv_stack)
        fw.begin_phase()
        fw.op("pool", lambda e: e.memset(VA[:, :, :, HD:HD + 2], 1.0), [], [r_VA])
        sc = nt_scratch()
        xring = fw.ring("xrow", 1, [128, D], F32)
        hT, r_hT = fw.buf("hT", [128, KC, 512], BF16)
        wring = fw.ring("wb", 3, [128, KC, 256], BF16)
        rc, r_rc = fw.buf("ropec", [128, 512], F32)
        rs, r_rs = fw.buf("ropes", [128, 512], F32)
        qg, r_qg = fw.buf("qg", [128, 512], F32)
        sq, r_sq = fw.buf("sq", [128, 512], BF16)
        rr, r_rr = fw.buf("rr", [128, 512], F32)
        t1, r_t1 = fw.buf("t1", [128, 512], F32)
        t2, r_t2 = fw.buf("t2", [128, 512], F32)
        qst = fw.ring("qst", 2, [128, 512], BF16)
        ust = fw.ring("ust", 2, [128, 512], F32)
        gst = fw.ring("gst", 2, [128, 512], F32)
        sgt, r_sgt = fw.buf("sgt", [128, 512], F32)
        hm, r_hm = fw.buf("hm", [128, 2 * HALO], F32)
        dma(fw, "sp", hm[:], halo_mask, [], [r_hm])

        groups = []
        for g in range(4):
            groups.append(dict(src=x_own[g * 512:(g + 1) * 512, :], n=512, a=A1, s=SH1, key0=g * 512,
                               full=True, own0=g * 512))
        for g in range(4):
            groups.append(dict(src=x_oth[g * 512:(g + 1) * 512, :], n=512, a=A1, s=SH1, key0=OWN + g * 512,
                               full=False))
        groups.append(dict(src=ctx_b, n=NCTX, a=A1C, s=SH1C, key0=SEQ, full=False))
        groups.append(dict(src=x_halo, n=2 * HALO, a=A1, s=SH1, key0=None, full=False, halo=True))

        def load_w(c0):
            wt, r_wt = wring.next()
            dma(fw, "pool", wt[:], w_in_v[:, :, c0:c0 + 256], [], [r_wt])
            return wt, r_wt

        def proj_fm(wt, r_wt, sub, n, bk, r_bk):
            mm_group(fw, bk[:, 0:n], [(wt[:, kc, sub * 128:(sub + 1) * 128], hT[:, kc, 0:n]) for kc in range(KC)],
                     [r_wt, r_hT], [r_bk])

        bank_i = [0]

        def next_bank(lo=0, hi=4):
            b = banks[lo + bank_i[0] % (hi - lo)]
            bank_i[0] += 1
            return b

        def do_group(G):
                n = G["n"]
                ntile = (n + 127) // 128
                for tt in range(ntile):
                    nr = min(128, n - tt * 128)
                    xr, r_xr = xring.next()
                    dma(fw, "sp", xr[:nr, :], G["src"][tt * 128:tt * 128 + nr, :], [], [r_xr])
                    norm_transpose(xr, r_xr, nr, G["a"], G["s"],
                                   lambda kc, tt=tt, nr=nr: hT[:, kc, tt * 128:tt * 128 + nr], r_hT, sc, banks[4:8])
                halo = G.get("halo", False)
                if not halo:
                    key0 = G["key0"]
                    dma(fw, "sp", rc[:, 0:n], rope_c[:, key0:key0 + n], [], [r_rc])
                    dma(fw, "sp", rs[:, 0:n], rope_s[:, key0:key0 + n], [], [r_rs])
                    heads = []
                    if G["full"]:
                        heads += [("q", h) for h in range(NQH)]
                    heads += [("k", h) for h in range(NKVH)]
                    for hi in range(0, len(heads), 2):
                        kind, h0 = heads[hi]
                        c0 = (Q_OFF if kind == "q" else K_OFF) + h0 * HD
                        wt, r_wt = load_w(c0)
                        for sub in range(2):
                            kind, h = heads[hi + sub]
                            gcol = 0 if kind == "q" else 1
                            bk, r_bk = next_bank()
                            proj_fm(wt, r_wt, sub, n, bk, r_bk)
                            fw.op("act", lambda e, bk=bk, gcol=gcol: e.activation(
                                out=qg[:, 0:n], in_=bk[:, 0:n], func=AF.Identity, scale=qkg[:, gcol:gcol + 1]),
                                [r_bk, r_qkg], [r_qg])
                            fw.op("act", lambda e, bk=bk: e.activation(out=sq[:, 0:n], in_=bk[:, 0:n], func=AF.Square),
                                  [r_bk], [r_sq])
                            b2, r_b2 = next_bank()
                            mm_group(fw, b2[:, 0:n], [(ones_b[:], sq[:, 0:n])], [r_ones_b, r_sq], [r_b2])
                            b3, r_b3 = next_bank()
                            mm_group(fw, b3[:, 0:n], [(pmat[:], qg[:, 0:n])], [r_pmat, r_qg], [r_b3])
                            fw.op("act", lambda e, b2=b2: e.activation(out=rr[:, 0:n], in_=b2[:, 0:n], func=AF.Sqrt,
                                                                       scale=1.0 / HD, bias=epst[:, 0:1]),
                                  [r_b2, r_eps], [r_rr])
                            fw.op("dve", lambda e: e.reciprocal(out=rr[:, 0:n], in_=rr[:, 0:n]), [r_rr], [r_rr])
                            fw.op("dve", lambda e: e.tensor_tensor(out=t1[:, 0:n], in0=qg[:, 0:n], in1=rc[:, 0:n], op=ALU.mult),
                                  [r_qg, r_rc], [r_t1])
                            fw.op("dve", lambda e, b3=b3: e.tensor_tensor(out=t2[:, 0:n], in0=b3[:, 0:n], in1=rs[:, 0:n],
                                                                          op=ALU.mult), [r_b3, r_rs], [r_t2])
                            fw.op("dve", lambda e: e.tensor_tensor(out=t1[:, 0:n], in0=t1[:, 0:n], in1=t2[:, 0:n], op=ALU.add),
                                  [r_t1, r_t2], [r_t1])
                            if kind == "k":
                                fw.op("dve", lambda e, h=h, key0=key0: e.tensor_tensor(
                                    out=KT[:, h, key0:key0 + n], in0=t1[:, 0:n], in1=rr[:, 0:n], op=ALU.mult),
                                    [r_t1, r_rr], [r_KT])
                            else:
                                qs, r_qs = qst.next()
                                fw.op("dve", lambda e, qs=qs: e.tensor_tensor(out=qs[:, 0:n], in0=t1[:, 0:n], in1=rr[:, 0:n],
                                                                               op=ALU.mult), [r_t1, r_rr], [r_qs])
                                o0 = G["own0"]
                                store(fw, "sp", qT_d[h, :, o0:o0 + n], qs[:, 0:n], r_qs)
                    for vb in range(2):
                        wt, r_wt = load_w(V_OFF + vb * 256)
                        for tt in range(ntile):
                            bk, r_bk = next_bank()
                            mm_group(fw, bk[:, 0:256], [(hT[:, kc, tt * 128:(tt + 1) * 128], wt[:, kc, :]) for kc in range(KC)],
                                     [r_wt, r_hT], [r_bk])
                            kt = (key0 // 128) + tt
                            eng = "act" if tt % 2 == 0 else "dve"
                            src = bk[:, 0:256].rearrange("p (h d) -> p h d", h=2)
                            if eng == "act":
                                fw.op("act", lambda e, kt=kt, vb=vb, src=src: e.activation(
                                    out=VA[:, kt, vb * 2:vb * 2 + 2, 0:HD], in_=src, func=AF.Identity), [r_bk], [r_VA])
                            else:
                                fw.op("dve", lambda e, kt=kt, vb=vb, src=src: e.tensor_copy(
                                    out=VA[:, kt, vb * 2:vb * 2 + 2, 0:HD], in_=src), [r_bk], [r_VA])
                if G["full"] or halo:
                    if halo:
                        ucol0 = None
                    else:
                        ucol0 = HALO + G["own0"]
                    for cb in range(8):
                        wa, r_wa = load_w(GLU_OFF + cb * 256)
                        wg, r_wg = load_w(GLU_OFF + CW + cb * 256)
                        for sub in range(2):
                            cc = cb * 2 + sub
                            ba, r_ba = next_bank()
                            proj_fm(wa, r_wa, sub, n, ba, r_ba)
                            bg, r_bg = next_bank()
                            proj_fm(wg, r_wg, sub, n, bg, r_bg)
                            fw.op("act", lambda e, bg=bg: e.activation(out=sgt[:, 0:n], in_=bg[:, 0:n], func=AF.Sigmoid),
                                  [r_bg], [r_sgt])
                            us, r_us = ust.next()
                            fw.op("dve", lambda e, ba=ba, us=us: e.tensor_tensor(out=us[:, 0:n], in0=ba[:, 0:n], in1=sgt[:, 0:n],
                                                                               op=ALU.mult), [r_ba, r_sgt], [r_us])
                            if halo:
                                fw.op("dve", lambda e, us=us: e.tensor_tensor(out=us[:, 0:n], in0=us[:, 0:n], in1=hm[:, 0:n],
                                                                               op=ALU.mult), [r_us, r_hm], [r_us])
                                fw.op("sp", lambda e, us=us, cc=cc: [
                                    e.dma_start(out=uT_d[cc, :, 0:HALO], in_=us[:, 0:HALO]),
                                    e.dma_start(out=uT_d[cc, :, HALO + OWN:UW], in_=us[:, HALO:2 * HALO])],
                                    [r_us], [], dma=2, sem_res=r_us)
                            else:
                                store(fw, "sp", uT_d[cc, :, ucol0:ucol0 + n], us[:, 0:n], r_us)
                if G["full"]:
                    o0 = G["own0"]
                    for gb in range(32):
                        wt, r_wt = load_w(GATE_OFF + gb * 256)
                        for sub in range(2):
                            ch = gb * 2 + sub
                            bk, r_bk = next_bank()
                            proj_fm(wt, r_wt, sub, n, bk, r_bk)
                            gs, r_gs = gst.next()
                            fw.op("act", lambda e, bk=bk, gs=gs: e.activation(out=gs[:, 0:n], in_=bk[:, 0:n], func=AF.Sigmoid),
                                  [r_bk], [r_gs])
                            store(fw, "sp", gates_d[ch, :, o0:o0 + n], gs[:, 0:n], r_gs)

        for G in groups:
            do_group(G)
        if debug:
            dbg["KT"] = _dump(nc, fw, "dbg_KT", KT, r_KT, [128, NKVH, NKEY], BF16)
            dbg["VA"] = _dump(nc, fw, "dbg_VA", VA, r_VA, [128, NKT, NKVH, HD + 2], BF16)
        fw.end_phase()
        if stop_after == 1:
            kv_stack.close()
            return _finish(nc, fw, dbg, out, r_out)

        fw.begin_phase()
        qT, r_qT = fw.buf("qT", [128, NQH, 512], BF16)
        pring = fw.ring("pexp", 3, [128, 512], BF16)
        atm = fw.ring("atm", 2, [128, 128], BF16)
        rden = fw.ring("rden", 2, [128, 1], F32)
        aT, r_aT = fw.buf("aT", [128, NQH, 512], BF16)
        uin = fw.ring("uin", 2, [128, 512 + 2 * HALO], F32)
        yc, r_yc = fw.buf("yc", [128, 16, 512], F32)
        ysq, r_ysq = fw.buf("ysq", [128, 512], F32)
        mean, r_mean = fw.buf("mean", [128, 512], F32)
        var, r_var = fw.buf("var", [128, 512], F32)
        zt, r_zt = fw.buf("zt", [128, 512], F32)
        cst = fw.ring("cst", 2, [128, 512], BF16)
        cw, r_cw = fw.buf("cw", [128, 16, TAPS], F32)
        cbv, r_cbv = fw.buf("cbv", [128, 16], F32)
        lng, r_lng = fw.buf("lng", [128, 16], F32)
        lnb, r_lnb = fw.buf("lnb", [128, 16], F32)
        dma(fw, "sp", cw[:], cw_in, [], [r_cw])
        dma(fw, "sp", cbv[:], cb_in, [], [r_cbv])
        dma(fw, "sp", lng[:], lng_in, [], [r_lng])
        dma(fw, "sp", lnb[:], lnb_in, [], [r_lnb])
        SCALE = float(HD) ** -0.5
        s_banks = [banks[0], banks[1]]
        o_banks = [(banks[2], banks[3]), (banks[4], banks[5])]
        tr_bank = banks[6]
        st_bank = banks[7]

        def conv_chunk(cc, o0):
            ui, r_ui = uin.next()
            dma(fw, "sp", ui[:], uT_d[cc, :, o0:o0 + 512 + 2 * HALO], [], [r_ui])
            fw.op("dve", lambda e, ui=ui, cc=cc: e.tensor_scalar(
                out=yc[:, cc, :], in0=ui[:, 1:513], scalar1=cw[:, cc, 0:1], scalar2=cbv[:, cc:cc + 1],
                op0=ALU.mult, op1=ALU.add), [r_ui, r_cw, r_cbv], [r_yc])
            for k in range(1, TAPS):
                fw.op("dve", lambda e, ui=ui, cc=cc, k=k: e.scalar_tensor_tensor(
                    out=yc[:, cc, :], in0=ui[:, k + 1:k + 513], scalar=cw[:, cc, k:k + 1], in1=yc[:, cc, :],
                    op0=ALU.mult, op1=ALU.add), [r_ui, r_cw, r_yc], [r_yc])

        for g in range(4):
            o0 = g * 512
            dma(fw, "sp", qT[:], qT_d[:, :, o0:o0 + 512].rearrange("h p t -> p h t"), [], [r_qT])
            for h in range(NQH):
                kvh = h // (NQH // NKVH)
                ob = o_banks[h % 2]
                pend = None
                for kc in range(NKT + 1):
                    if kc < NKT:
                        sb_, r_sb = s_banks[kc % 2]
                        mm_group(fw, sb_[:, :], [(KT[:, kvh, kc * 128:(kc + 1) * 128], qT[:, h, :])], [r_KT, r_qT], [r_sb])
                        pb, r_pb = pring.next()
                        fw.op("act", lambda e, sb_=sb_, pb=pb: e.activation(out=pb[:], in_=sb_[:, :], func=AF.Exp,
                                                                             scale=SCALE), [r_sb], [r_pb])
                        cur = (kc, pb, r_pb)
                    else:
                        cur = None
                    if pend is not None:
                        pkc, ppb, r_ppb = pend

                        def fn(e, pkc=pkc, ppb=ppb, kvh=kvh, ob=ob):
                            last = None
                            for sub in range(4):
                                bk = ob[sub // 2][0]
                                c0 = (sub % 2) * 256
                                last = e.matmul(bk[:, c0:c0 + HD + 1], lhsT=ppb[:, sub * 128:(sub + 1) * 128],
                                                rhs=VA[:, pkc, kvh, 0:HD + 1], start=(pkc == 0), stop=(pkc == NKT - 1))
                            return last
                        fw.op("pe", fn, [r_ppb, r_VA], [ob[0][1], ob[1][1]])
                    pend = cur
                for sub in range(4):
                    bk, r_bk = ob[sub // 2]
                    c0 = (sub % 2) * 256
                    rd, r_rd = rden.next()
                    fw.op("dve", lambda e, bk=bk, c0=c0, rd=rd: e.reciprocal(out=rd[:], in_=bk[:, c0 + HD:c0 + HD + 1]),
                          [r_bk], [r_rd])
                    am, r_am = atm.next()
                    fw.op("dve", lambda e, bk=bk, c0=c0, rd=rd, am=am: e.tensor_scalar(
                        out=am[:], in0=bk[:, c0:c0 + HD], scalar1=rd[:, 0:1], scalar2=None, op0=ALU.mult),
                        [r_bk, r_rd], [r_am])
                    tb, r_tb = tr_bank
                    tbv = tb[:].bitcast(BF16)
                    fw.op("pe", lambda e, am=am, tbv=tbv: e.transpose(tbv[:, 0:128], am[:], identb[:]),
                          [r_am, r_identb], [r_tb])
                    fw.op("act", lambda e, tbv=tbv, h=h, sub=sub: e.activation(
                        out=aT[:, h, sub * 128:(sub + 1) * 128], in_=tbv[:, 0:128], func=AF.Identity), [r_tb], [r_aT])
                conv_chunk(h, o0)
            store(fw, "pool", attnT_d[:, :, o0:o0 + 512].rearrange("h p t -> p h t"), aT[:], r_aT)

            sbk, r_sbk = st_bank
            mm_group(fw, sbk[:, :], [(ones_f[:], yc[:, cc, :]) for cc in range(16)], [r_ones_f, r_yc], [r_sbk])
            fw.op("act", lambda e, sbk=sbk: e.activation(out=mean[:], in_=sbk[:, :], func=AF.Identity, scale=1.0 / CW),
                  [r_sbk], [r_mean])
            for cc in range(16):
                fw.op("dve", lambda e, cc=cc: e.tensor_tensor(out=yc[:, cc, :], in0=yc[:, cc, :], in1=mean[:], op=ALU.subtract),
                      [r_yc, r_mean], [r_yc])
            for cc in range(16):
                fw.op("act", lambda e, cc=cc: e.activation(out=ysq[:], in_=yc[:, cc, :], func=AF.Square), [r_yc], [r_ysq])
                fw.op("pe", lambda e, cc=cc, sbk=sbk: e.matmul(sbk[:, :], lhsT=ones_f[:], rhs=ysq[:], start=(cc == 0),
                                                            stop=(cc == 15)), [r_ones_f, r_ysq], [r_sbk])
            fw.op("act", lambda e, sbk=sbk: e.activation(out=var[:], in_=sbk[:, :], func=AF.Sqrt, scale=1.0 / CW,
                                                         bias=epst[:, 0:1]), [r_sbk, r_eps], [r_var])
            fw.op("dve", lambda e: e.reciprocal(out=var[:], in_=var[:]), [r_var], [r_var])
            for cc in range(16):
                fw.op("dve", lambda e, cc=cc: e.tensor_tensor(out=zt[:], in0=yc[:, cc, :], in1=var[:], op=ALU.mult),
                      [r_yc, r_var], [r_zt])
                cs, r_cs = cst.next()
                fw.op("act", lambda e, cc=cc, cs=cs: e.activation(out=cs[:], in_=zt[:], func=AF.Silu,
                                                                 scale=lng[:, cc:cc + 1], bias=lnb[:, cc:cc + 1]),
                      [r_zt, r_lng, r_lnb], [r_cs])
                store(fw, "pool", csT_d[cc, :, o0:o0 + 512], cs[:], r_cs)
        fw.end_phase()
        kv_stack.close()
        if stop_after == 2:
            return _finish(nc, fw, dbg, out, r_out)

        fw.begin_phase()
        aT, r_aT = fw.buf("aT3", [128, 16, 512], BF16)
        cT, r_cT = fw.buf("cT3", [128, 16, 512], BF16)
        mT, r_mT = fw.buf("mT", [128, KC, 512], BF16)
        wring = fw.ring("wb3", 3, [128, 8192], BF16)
        gin = fw.ring("gin", 4, [128, 512], F32)
        tm1, r_tm1 = fw.buf("tm1", [128, 512], F32)
        tm2, r_tm2 = fw.buf("tm2", [128, 512], F32)
        xin = fw.ring("xin", 2, [128, 4, 256], F32)
        xo = fw.ring("xo", 2, [128, 4, 256], F32)
        gar = fw.ring("gar", 2, [128, 256], F32)
        for g in range(4):
            o0 = g * 512
            dma(fw, "sp", aT[:], attnT_d[:, :, o0:o0 + 512].rearrange("h p t -> p h t"), [], [r_aT])
            dma(fw, "sp", cT[:], csT_d[:, :, o0:o0 + 512].rearrange("h p t -> p h t"), [], [r_cT])
            for ob4 in range(8):
                wa, r_wa = wring.next()
                wav = wa[:].rearrange("p (k n) -> p k n", k=16)
                dma(fw, "pool", wav, w_ao_v[:, :, ob4 * 512:(ob4 + 1) * 512], [], [r_wa])
                wc, r_wc = wring.next()
                wcv = wc[:].rearrange("p (k n) -> p k n", k=16)
                dma(fw, "pool", wcv, w_co_v[:, :, ob4 * 512:(ob4 + 1) * 512], [], [r_wc])
                for sub in range(4):
                    oc = ob4 * 4 + sub
                    ba, r_ba = banks[(oc * 2) % 4]
                    bc, r_bc = banks[(oc * 2 + 1) % 4]
                    mm_group(fw, ba[:, :], [(wav[:, kc, sub * 128:(sub + 1) * 128], aT[:, kc, :]) for kc in range(16)],
                             [r_wa, r_aT], [r_ba])
                    mm_group(fw, bc[:, :], [(wcv[:, kc, sub * 128:(sub + 1) * 128], cT[:, kc, :]) for kc in range(16)],
                             [r_wc, r_cT], [r_bc])
                    ga_, r_ga = gin.next()
                    gc_, r_gc = gin.next()
                    dma(fw, "sp", ga_[:], gates_d[oc, :, o0:o0 + 512], [], [r_ga])
                    dma(fw, "sp", gc_[:], gates_d[32 + oc, :, o0:o0 + 512], [], [r_gc])
                    fw.op("dve", lambda e, ba=ba, ga_=ga_: e.tensor_tensor(out=tm1[:], in0=ba[:, :], in1=ga_[:], op=ALU.mult),
                          [r_ba, r_ga], [r_tm1])
                    fw.op("dve", lambda e, bc=bc, gc_=gc_: e.tensor_tensor(out=tm2[:], in0=bc[:, :], in1=gc_[:], op=ALU.mult),
                          [r_bc, r_gc], [r_tm2])
                    fw.op("dve", lambda e, oc=oc: e.tensor_tensor(out=mT[:, oc, :], in0=tm1[:], in1=tm2[:], op=ALU.add),
                          [r_tm1, r_tm2], [r_mT])
            for nb in range(16):
                wo, r_wo = wring.next()
                wov = wo[:].rearrange("p (k n) -> p k n", k=KC)
                dma(fw, "pool", wov, w_out_v[:, :, nb * 256:(nb + 1) * 256], [], [r_wo])
                xi, r_xi = xin.next()
                dma(fw, "sp", xi[:], x_own[o0:o0 + 512, nb * 256:(nb + 1) * 256].rearrange("(t p) n -> p t n", p=128),
                    [], [r_xi])
                gr, r_gr = gar.next()
                dma(fw, "sp", gr[:], mod_all[0:1, 2 * D + nb * 256:2 * D + (nb + 1) * 256].broadcast_to([128, 256]),
                    [], [r_gr])
                xo_, r_xo = xo.next()
                for tt in range(4):
                    bk, r_bk = banks[4 + (nb * 4 + tt) % 4]
                    mm_group(fw, bk[:, 0:256], [(mT[:, kc, tt * 128:(tt + 1) * 128], wov[:, kc, :]) for kc in range(KC)],
                             [r_wo, r_mT], [r_bk])
                    fw.op("dve", lambda e, bk=bk, tt=tt, gr=gr, xo_=xo_: e.tensor_tensor(
                        out=xo_[:, tt, :], in0=bk[:, 0:256], in1=gr[:], op=ALU.mult), [r_bk, r_gr], [r_xo])
                    fw.op("dve", lambda e, tt=tt, xi=xi, xo_=xo_: e.tensor_tensor(
                        out=xo_[:, tt, :], in0=xo_[:, tt, :], in1=xi[:, tt, :], op=ALU.add), [r_xo, r_xi], [r_xo])
                store(fw, "act", xnew_d[o0:o0 + 512, nb * 256:(nb + 1) * 256].rearrange("(t p) n -> p t n", p=128), xo_[:],
                      r_xo)
        fw.end_phase()
        if stop_after == 3:
            return _finish(nc, fw, dbg, out, r_out)

        fw.begin_phase()
        sc = nt_scratch()
        xring = fw.ring("xrow4", 2, [128, D], F32)
        h2T, r_h2T = fw.buf("h2T", [128, KC, 128], F32)
        wr, r_wr = fw.buf("wr", [128, KC, 36], F32)
        brr, r_brr = fw.buf("brr", [128, 36], F32)
        iot, r_iot = fw.buf("iot", [128, NE], F32)
        tril, r_tril = fw.buf("tril", [128, 128], F32)
        trilb, r_trilb = fw.buf("trilb", [128, 128], BF16)
        tke, r_tke = fw.buf("tke", [128, 16, 2, 2], I32)
        Mb, r_Mb = fw.buf("Mb", [128, 16, NE], BF16)
        dma(fw, "sp", wr[:], w_r_v, [], [r_wr])
        dma(fw, "sp", brr[:], b_r_rep, [], [r_brr])
        dma(fw, "sp", iot[:], iota_cap, [], [r_iot])
        dma(fw, "sp", tril[:], tril_in, [], [r_tril])
        dma(fw, "sp", tke[:], tok_ent, [], [r_tke])
        fw.op("dve", lambda e: e.tensor_copy(out=trilb[:], in_=tril[:]), [r_tril], [r_trilb])

        def sbuf1(name, shape, dt=F32):
            return fw.buf(name, shape, dt)
        L, r_L = sbuf1("L", [128, 36])
        gmax, r_gmax = sbuf1("gmax", [128, 1])
        ngmax, r_ngmax = sbuf1("ngmax", [128, 1])
        gmask, r_gmask = sbuf1("gmask", [128, 4])
        gexp, r_gexp = sbuf1("gexp", [128, 4])
        gsum, r_gsum = sbuf1("gsum", [128, 1])
        pg, r_pg = sbuf1("pg", [128, 1])
        pen, r_pen = sbuf1("pen", [128, 4])
        em, r_em = sbuf1("em", [128, NE])
        em2, r_em2 = sbuf1("em2", [128, NE])
        m1, r_m1 = sbuf1("m1", [128, 1])
        m2, r_m2 = sbuf1("m2", [128, 1])
        mk1, r_mk1 = sbuf1("mk1", [128, NE])
        mk2, r_mk2 = sbuf1("mk2", [128, NE])
        dd, r_dd = sbuf1("dd", [128, 1])
        e2, r_e2 = sbuf1("e2", [128, 1])
        wts, r_wts = sbuf1("wts", [128, 2])
        Msum, r_Msum = sbuf1("Msum", [128, NE])
        cum, r_cum = sbuf1("cum", [128, NE])
        tmp32, r_tmp32 = sbuf1("tmp32", [128, NE])
        pos, r_pos = sbuf1("pos", [128, 2])
        eb, r_eb = sbuf1("eb", [128, 2])
        ovf, r_ovf = sbuf1("ovf", [128, 2])
        dstf, r_dstf = sbuf1("dstf", [128, 2])
        dring = fw.ring("dsti", 2, [128, 2], I32)
        wring2 = fw.ring("wts2", 2, [128, 4], F32)

        for tt in range(16):
            g = tt // 4
            xr, r_xr = xring.next()
            dma(fw, "sp", xr[:], xnew_d[tt * 128:(tt + 1) * 128, :], [], [r_xr])
            norm_transpose(xr, r_xr, 128, A2, SH2, lambda kc: h2T[:, kc, :], r_h2T, sc, banks[4:8])
            lb, r_lb = banks[tt % 2]
            mm_group(fw, lb[:, 0:36], [(h2T[:, kc, :], wr[:, kc, :]) for kc in range(KC)], [r_h2T, r_wr], [r_lb])
            V = "dve"
            fw.op(V, lambda e, lb=lb: e.tensor_tensor(out=L[:], in0=lb[:, 0:36], in1=brr[:], op=ALU.add), [r_lb, r_brr], [r_L])
            fw.op(V, lambda e: e.tensor_reduce(out=gmax[:], in_=L[:, 0:4], axis=AX.X, op=ALU.max), [r_L], [r_gmax])
            fw.op(V, lambda e: e.tensor_scalar(out=gmask[:], in0=L[:, 0:4], scalar1=gmax[:, 0:1], scalar2=None,
                                               op0=ALU.is_equal), [r_L, r_gmax], [r_gmask])
            fw.op(V, lambda e: e.tensor_scalar(out=ngmax[:], in0=gmax[:], scalar1=-1.0, scalar2=None, op0=ALU.mult),
                  [r_gmax], [r_ngmax])
            fw.op("act", lambda e: e.activation(out=gexp[:], in_=L[:, 0:4], func=AF.Exp, bias=ngmax[:, 0:1],
                                                accum_out=gsum[:]), [r_L, r_ngmax], [r_gexp, r_gsum])
            fw.op(V, lambda e: e.reciprocal(out=pg[:], in_=gsum[:]), [r_gsum], [r_pg])
            fw.op(V, lambda e: e.tensor_scalar(out=pen[:], in0=gmask[:], scalar1=-1.0, scalar2=1e30, op0=ALU.add,
                                               op1=ALU.mult), [r_gmask], [r_pen])
            fw.op(V, lambda e: e.tensor_tensor(out=em[:].rearrange("p (g k) -> p g k", g=4),
                                               in0=L[:, 4:36].rearrange("p (g k) -> p g k", g=4),
                                               in1=pen[:].unsqueeze(2).broadcast_to([128, 4, 8]), op=ALU.add),
                  [r_L, r_pen], [r_em])
            fw.op(V, lambda e: e.tensor_reduce(out=m1[:], in_=em[:], axis=AX.X, op=ALU.max), [r_em], [r_m1])
            fw.op(V, lambda e: e.tensor_scalar(out=mk1[:], in0=em[:], scalar1=m1[:, 0:1], scalar2=None, op0=ALU.is_equal),
                  [r_em, r_m1], [r_mk1])
            fw.op(V, lambda e: e.scalar_tensor_tensor(out=em2[:], in0=mk1[:], scalar=-1e30, in1=em[:], op0=ALU.mult,
                                                      op1=ALU.add), [r_mk1, r_em], [r_em2])
            fw.op(V, lambda e: e.tensor_reduce(out=m2[:], in_=em2[:], axis=AX.X, op=ALU.max), [r_em2], [r_m2])
            fw.op(V, lambda e: e.tensor_scalar(out=mk2[:], in0=em2[:], scalar1=m2[:, 0:1], scalar2=None, op0=ALU.is_equal),
                  [r_em2, r_m2], [r_mk2])
            fw.op(V, lambda e: e.tensor_tensor(out=dd[:], in0=m2[:], in1=m1[:], op=ALU.subtract), [r_m1, r_m2], [r_dd])
            fw.op("act", lambda e: e.activation(out=e2[:], in_=dd[:], func=AF.Exp), [r_dd], [r_e2])
            wt2, r_wt2 = wring2.next()
            fw.op(V, lambda e: e.tensor_scalar(out=e2[:], in0=e2[:], scalar1=1.0, scalar2=None, op0=ALU.add), [r_e2], [r_e2])
            fw.op(V, lambda e: e.reciprocal(out=e2[:], in_=e2[:]), [r_e2], [r_e2])
            fw.op(V, lambda e, wt2=wt2: e.memset(wt2[:], 0.0), [], [r_wt2])
            fw.op(V, lambda e, wt2=wt2: e.tensor_tensor(out=wt2[:, 0:1], in0=e2[:], in1=pg[:], op=ALU.mult), [r_e2, r_pg], [r_wt2])
            fw.op(V, lambda e, wt2=wt2: e.tensor_tensor(out=wt2[:, 2:3], in0=pg[:], in1=wt2[:, 0:1], op=ALU.subtract),
                  [r_pg, r_wt2], [r_wt2])
            fw.op(V, lambda e: e.tensor_tensor(out=Msum[:], in0=mk1[:], in1=mk2[:], op=ALU.add), [r_mk1, r_mk2], [r_Msum])
            fw.op(V, lambda e, tt=tt: e.tensor_copy(out=Mb[:, tt, :], in_=Msum[:]), [r_Msum], [r_Mb])
            cb_, r_cb = banks[2 + tt % 2]
            pairs = [(trilb[:], Mb[:, tt, :])] + [(ones_b[:], Mb[:, j, :]) for j in range(tt)]
            mm_group(fw, cb_[:, 0:NE], pairs, [r_trilb, r_ones_b, r_Mb], [r_cb])
            fw.op(V, lambda e, cb_=cb_: e.tensor_copy(out=cum[:], in_=cb_[:, 0:NE]), [r_cb], [r_cum])
            for k, (mk, r_mk) in enumerate(((mk1, r_mk1), (mk2, r_mk2))):
                fw.op(V, lambda e, mk=mk: e.tensor_tensor(out=tmp32[:], in0=mk[:], in1=cum[:], op=ALU.mult), [r_mk, r_cum], [r_tmp32])
                fw.op(V, lambda e, k=k: e.tensor_reduce(out=pos[:, k:k + 1], in_=tmp32[:], axis=AX.X, op=ALU.add), [r_tmp32], [r_pos])
                fw.op(V, lambda e, mk=mk: e.tensor_tensor(out=tmp32[:], in0=mk[:], in1=iot[:], op=ALU.mult), [r_mk, r_iot], [r_tmp32])
                fw.op(V, lambda e, k=k: e.tensor_reduce(out=eb[:, k:k + 1], in_=tmp32[:], axis=AX.X, op=ALU.add), [r_tmp32], [r_eb])
            fw.op(V, lambda e: e.tensor_scalar(out=ovf[:], in0=pos[:], scalar1=float(CAP) - 0.5, scalar2=1.0e6, op0=ALU.is_gt,
                                               op1=ALU.mult), [r_pos], [r_ovf])
            fw.op(V, lambda e: e.tensor_tensor(out=dstf[:], in0=pos[:], in1=eb[:], op=ALU.add), [r_pos, r_eb], [r_dstf])
            fw.op(V, lambda e: e.tensor_tensor(out=dstf[:], in0=dstf[:], in1=ovf[:], op=ALU.add), [r_dstf, r_ovf], [r_dstf])
            di, r_di = dring.next()
            fw.op(V, lambda e, di=di: e.tensor_copy(out=di[:], in_=dstf[:]), [r_dstf], [r_di])
            for k in range(2):
                fw.op("pool", lambda e, di=di, k=k, tt=tt: [e.indirect_dma_start(
                    out=tab_i, out_offset=bass.IndirectOffsetOnAxis(ap=di[:, k:k + 1], axis=0),
                    in_=tke[:, tt, k, :], in_offset=None, bounds_check=fw.bc_reg(e, NE * CAP - 1), oob_is_err=False)],
                    [r_di, r_tke], [r_tab_i], dma=1)
                fw.op("pool", lambda e, di=di, k=k, wt2=wt2: [e.indirect_dma_start(
                    out=tab_w, out_offset=bass.IndirectOffsetOnAxis(ap=di[:, k:k + 1], axis=0),
                    in_=wt2[:, 2 * k:2 * k + 2], in_offset=None, bounds_check=fw.bc_reg(e, NE * CAP - 1), oob_is_err=False)],
                    [r_di, r_wt2], [r_tab_w], dma=1)
            if tt % 4 == 3:
                fw.emit()
        fw.end_phase()
        if stop_after == 4:
            return _finish(nc, fw, dbg, out, r_out)

        fw.begin_phase()
        sc = nt_scratch()
        xg = fw.ring("xg", 2, [128, D], F32)
        gT, r_gT = fw.buf("gT", [128, KC, CAP], BF16)
        actT, r_actT = fw.buf("actT", [128, FF // 128, CAP], BF16)
        wring = fw.ring("wbC", 3, [128, 8192], BF16)
        idxr = fw.ring("idx", 2 * NST, [128, 2], I32)
        wtr = fw.ring("wtc", 2 * NST, [128, 2], F32)
        sgr = fw.ring("sgr", 2, [128, CAP], F32)
        ypc = fw.ring("ypc", 3, [128, 1024], F32)
        for ex in range(NE):
            idxs = []
            for stl in range(NST):
                ix, r_ix = idxr.next()
                w1, r_w1 = wtr.next()
                r0 = ex * CAP + stl * 128
                dma(fw, "sp", ix[:], tab_i[r0:r0 + 128, :], [r_tab_i], [r_ix])
                dma(fw, "sp", w1[:], tab_w[r0:r0 + 128, :], [r_tab_w], [r_w1])
                idxs.append((ix, r_ix, w1, r_w1))
                xr, r_xr = xg.next()
                fw.op("pool", lambda e, xr=xr, ix=ix: [e.indirect_dma_start(
                    out=xr[:], out_offset=None, in_=xnew_d,
                    in_offset=bass.IndirectOffsetOnAxis(ap=ix[:, 0:1], axis=0))],
                    [r_ix], [r_xr], dma=1)
                norm_transpose(xr, r_xr, 128, A2, SH2, lambda kc, stl=stl: gT[:, kc, stl * 128:(stl + 1) * 128], r_gT,
                               sc, banks[4:8])
            for fb in range(4):
                wg_, r_wg = wring.next()
                wgv = wg_[:].rearrange("p (k n) -> p k n", k=KC)
                dma(fw, "pool", wgv, w_eg[ex].rearrange("(kc p) n -> p kc n", p=128)[:, :, fb * 256:(fb + 1) * 256], [], [r_wg])
                wu_, r_wu = wring.next()
                wuv = wu_[:].rearrange("p (k n) -> p k n", k=KC)
                dma(fw, "pool", wuv, w_eu[ex].rearrange("(kc p) n -> p kc n", p=128)[:, :, fb * 256:(fb + 1) * 256], [], [r_wu])
                for sub in range(2):
                    fc = fb * 2 + sub
                    bg, r_bg = banks[(fc * 2) % 4]
                    bu, r_bu = banks[(fc * 2 + 1) % 4]
                    mm_group(fw, bg[:, 0:CAP], [(wgv[:, kc, sub * 128:(sub + 1) * 128], gT[:, kc, :]) for kc in range(KC)],
                             [r_wg, r_gT], [r_bg])
                    mm_group(fw, bu[:, 0:CAP], [(wuv[:, kc, sub * 128:(sub + 1) * 128], gT[:, kc, :]) for kc in range(KC)],
                             [r_wu, r_gT], [r_bu])
                    sg_, r_sg_ = sgr.next()
                    fw.op("act", lambda e, bg=bg, sg_=sg_: e.activation(out=sg_[:], in_=bg[:, 0:CAP], func=AF.Silu), [r_bg], [r_sg_])
                    fw.op("dve", lambda e, bu=bu, sg_=sg_, fc=fc: e.tensor_tensor(out=actT[:, fc, :], in0=bu[:, 0:CAP], in1=sg_[:],
                                                                                op=ALU.mult), [r_bu, r_sg_], [r_actT])
            def load_wd(db, ex=ex):
                wd_, r_wd = wring.next()
                wdv = wd_[:].rearrange("p (k n) -> p k n", k=FF // 128)
                dma(fw, "pool", wdv, w_ed[ex].rearrange("(kc p) n -> p kc n", p=128)[:, :, db * 1024:(db + 1) * 1024], [], [r_wd])
                return wdv, r_wd
            nxt_wd = load_wd(0)
            for db in range(4):
                wdv, r_wd = nxt_wd
                if db + 1 < 4:
                    nxt_wd = load_wd(db + 1)
                for stl in range(NST):
                    ix, r_ix, w1, r_w1 = idxs[stl]
                    yp, r_yp = ypc.next()
                    for hf in range(2):
                        bk, r_bk = banks[4 + (db * 2 * NST + stl * 2 + hf) % 4]
                        mm_group(fw, bk[:, :], [(actT[:, kc, stl * 128:(stl + 1) * 128], wdv[:, kc, hf * 512:(hf + 1) * 512])
                                               for kc in range(FF // 128)], [r_wd, r_actT], [r_bk])
                        if hf == 0:
                            fw.op("act", lambda e, bk=bk, yp=yp, w1=w1: e.activation(
                                out=yp[:, 0:512], in_=bk[:, :], func=AF.Identity, scale=w1[:, 0:1]), [r_bk, r_w1], [r_yp])
                        else:
                            fw.op("dve", lambda e, bk=bk, yp=yp, w1=w1: e.tensor_scalar(
                                out=yp[:, 512:1024], in0=bk[:, :], scalar1=w1[:, 0:1], scalar2=None, op0=ALU.mult),
                                [r_bk, r_w1], [r_yp])
                    fw.op("pool", lambda e, yp=yp, ix=ix, db=db: [e.indirect_dma_start(
                        out=ybufs[db], out_offset=bass.IndirectOffsetOnAxis(ap=ix[:, 1:2], axis=0),
                        in_=yp[:], in_offset=None, bounds_check=fw.bc_reg(e, 2 * OWN - 1), oob_is_err=False)],
                        [r_yp, r_ix], [], dma=1, sem_res=r_yp)
            if ex % 4 == 3:
                fw.emit()
        fw.end_phase()
        if stop_after == 5:
            return _finish(nc, fw, dbg, out, r_out)

        fw.begin_phase()
        ga2, r_ga2 = fw.buf("ga2", [128, D], F32)
        gfr, r_gfr = fw.buf("gfr", [128, D], F32)
        dma(fw, "sp", ga2[:], mod_all[0:1, 5 * D:6 * D].broadcast_to([128, D]), [], [r_ga2])
        dma(fw, "sp", gfr[:], gf_rep_in, [], [r_gfr])
        xr_ = fw.ring("xd", 2, [128, D], F32)
        y1r = fw.ring("y1", 2, [128, D], F32)
        y2r = fw.ring("y2", 2, [128, D], F32)
        junk, r_junk = fw.buf("junkd", [128, D], BF16)
        ssr = fw.ring("ssd", 2, [128, 1], F32)
        for tt in range(16):
            xr, r_xr = xr_.next()
            y1, r_y1 = y1r.next()
            y2, r_y2 = y2r.next()
            dma(fw, "sp", xr[:], xnew_d[tt * 128:(tt + 1) * 128, :], [], [r_xr])
            fw.op("sp", lambda e, y1=y1, tt=tt: [e.dma_start(out=y1[:, i * 1024:(i + 1) * 1024],
                                                             in_=ybufs[i][tt * 128:(tt + 1) * 128, :]) for i in range(4)],
                  [], [r_y1], dma=4)
            fw.op("sp", lambda e, y2=y2, tt=tt: [e.dma_start(out=y2[:, i * 1024:(i + 1) * 1024],
                                                             in_=ybufs[i][OWN + tt * 128:OWN + (tt + 1) * 128, :]) for i in range(4)],
                  [], [r_y2], dma=4)
            fw.op("pool", lambda e, y1=y1, y2=y2: e.tensor_tensor(out=y1[:], in0=y1[:], in1=y2[:], op=ALU.add), [r_y1, r_y2], [r_y1])
            fw.op("dve", lambda e, y1=y1: e.tensor_tensor(out=y1[:], in0=y1[:], in1=ga2[:], op=ALU.mult), [r_y1, r_ga2], [r_y1])
            fw.op("pool", lambda e, y1=y1, xr=xr: e.tensor_tensor(out=xr[:], in0=xr[:], in1=y1[:], op=ALU.add), [r_y1, r_xr], [r_xr])
            ss, r_ss = ssr.next()
            fw.op("act", lambda e, xr=xr, ss=ss: e.activation(out=junk[:], in_=xr[:], func=AF.Square, accum_out=ss[:]),
                  [r_xr], [r_junk, r_ss])
            fw.op("act", lambda e, ss=ss: e.activation(out=ss[:], in_=ss[:], func=AF.Sqrt, scale=1.0 / D, bias=epst[:, 0:1]),
                  [r_ss, r_eps], [r_ss])
            fw.op("dve", lambda e, ss=ss: e.reciprocal(out=ss[:], in_=ss[:]), [r_ss], [r_ss])
            fw.op("dve", lambda e, xr=xr, ss=ss, y2=y2: e.scalar_tensor_tensor(
                out=y2[:], in0=xr[:], scalar=ss[:, 0:1], in1=gfr[:], op0=ALU.mult, op1=ALU.mult), [r_xr, r_ss, r_gfr, r_y2], [r_y2])
            store(fw, "pool", out[tt * 128:(tt + 1) * 128, :], y2[:], r_y2)
        fw.end_phase()
        if debug:
            fw.begin_phase()
            def ddump(name, src, shape, dt):
                d = nc.dram_tensor(name, list(shape), dt, kind="ExternalOutput").ap()
                dma(fw, "sp", d, src, [], [fw.res(name, True)])
            ddump("dbg_qT", qT_d[:, :, 0:512], [NQH, 128, 512], BF16)
            ddump("dbg_uT", uT_d[0:2], [2, 128, UW], F32)
            ddump("dbg_gates", gates_d[0:2, :, 0:512], [2, 128, 512], F32)
            ddump("dbg_attnT", attnT_d[:, :, 0:512], [16, 128, 512], BF16)
            ddump("dbg_csT", csT_d[:, :, 0:512], [16, 128, 512], BF16)
            ddump("dbg_xnew", xnew_d[0:256, :], [256, D], F32)
            ddump("dbg_tab_i", tab_i, [NE * CAP, 2], I32)
            ddump("dbg_tab_w", tab_w, [NE * CAP, 2], F32)
            ddump("dbg_ybuf0", ybufs[0][0:128, :], [128, 1024], F32)
            ddump("dbg_ybuf1", ybufs[0][OWN:OWN + 128, :], [128, 1024], F32)
            fw.end_phase()
        return _finish(nc, fw, dbg, out, r_out)


def _dump(nc, fw, name, t, r_t, shape, dt):
    d = nc.dram_tensor(name, list(shape), dt, kind="ExternalOutput").ap()
    r_d = fw.res(name, True)
    dma(fw, "sp", d, t[:], [r_t], [r_d])
    return d


def _finish(nc, fw, dbg, out, r_out):
    return nc


def _rope_tables(hf):
    inv = (10000.0 ** (-np.arange(0, 64, 2, dtype=np.float32) / 64.0)).astype(np.float32)
    tok = np.concatenate([np.arange(hf * OWN, (hf + 1) * OWN), np.arange((1 - hf) * OWN, (2 - hf) * OWN)])
    row = (tok // 64).astype(np.float32)
    col = (tok % 64).astype(np.float32)
    C = np.ones((128, NKEY), np.float32)
    S = np.zeros((128, NKEY), np.float32)
    for d in range(128):
        a = d // 64
        r = d % 64
        j = r % 32
        half = r // 32
        pos = row if a == 0 else col
        ang = (pos * inv[j]).astype(np.float32)
        C[d, :SEQ] = np.cos(ang)
        S[d, :SEQ] = np.sin(ang) * (-1.0 if half == 0 else 1.0)
    return C, S


def _consts():
    ident = np.eye(128, dtype=np.float32)
    pm = np.zeros((128, 128), np.float32)
    for m in range(128):
        r = m % 64
        partner = m + 32 if (r // 32) == 0 else m - 32
        pm[partner, m] = 1.0
    tril = np.zeros((128, 128), np.float32)
    for k in range(128):
        tril[k, k + 1:] = 1.0
    iota_cap = np.tile((np.arange(NE, dtype=np.float32) * CAP)[None, :], (128, 1))
    tok_ent = np.zeros((128, 16, 2, 2), np.int32)
    for tt in range(16):
        t = tt * 128 + np.arange(128)
        for k in range(2):
            tok_ent[:, tt, k, 0] = t
            tok_ent[:, tt, k, 1] = k * OWN + t
    tab_i_init = np.zeros((NE * CAP, 2), np.int32)
    tab_i_init[:, 1] = 1 << 24
    tab_w_init = np.zeros((NE * CAP, 2), np.float32)
    return dict(ident=ident, pmat=pm, tril=tril, iota_cap=iota_cap, tok_ent=tok_ent,
                tab_i_init=tab_i_init, tab_w_init=tab_w_init)


_NC_CACHE = {}


def kernel(x, c, ctx, c_ctx, norm1_g, w_mod, b_mod, w_in, q_norm_g, k_norm_g, w_attn_out,
           conv_dw_w, conv_dw_b, conv_ln_g, conv_ln_b, w_conv_out, w_out, norm2_g,
           w_router_group, b_router_group, w_router_expert, b_router_expert,
           w_exp_gate, w_exp_up, w_exp_down, norm_f_g, _debug=False, _stop_after=None):
    f = lambda a: np.ascontiguousarray(np.asarray(a, dtype=np.float32))
    x, c, ctx, c_ctx = f(x), f(c), f(ctx), f(c_ctx)
    fm = lambda v: np.ascontiguousarray(f(v).reshape(-1, 128).T)
    consts = _consts()
    shared = dict(
        qk_g=np.ascontiguousarray(np.stack([f(q_norm_g)[0], f(k_norm_g)[0]], axis=1)),
        g1=fm(norm1_g[0]), g2=fm(norm2_g[0]),
        gf_rep=np.ascontiguousarray(np.tile(f(norm_f_g)[None, :], (128, 1))),
        bmod2=np.ascontiguousarray(np.tile(f(b_mod)[0][None, :], (2, 1))),
        w_mod=f(w_mod)[0], w_in=f(w_in)[0], w_attn_out=f(w_attn_out)[0], w_conv_out=f(w_conv_out)[0],
        w_out=f(w_out)[0],
        conv_w=np.ascontiguousarray(f(conv_dw_w)[0, :, 0, :].T.reshape(16, 128, TAPS).transpose(1, 0, 2)),
        conv_b=fm(conv_dw_b[0]), ln_g=fm(conv_ln_g[0]), ln_b=fm(conv_ln_b[0]),
        w_router=np.ascontiguousarray(np.concatenate([f(w_router_group)[0], f(w_router_expert)[0]], axis=1)),
        b_router_rep=np.ascontiguousarray(np.tile(np.concatenate([f(b_router_group)[0], f(b_router_expert)[0]])[None, :],
                                                  (128, 1))),
        w_exp_gate=f(w_exp_gate)[0], w_exp_up=f(w_exp_up)[0], w_exp_down=f(w_exp_down)[0],
        **consts,
    )
    in_maps = []
    for core in range(8):
        b, hf = core // 2, core % 2
        own = x[b, hf * OWN:(hf + 1) * OWN]
        oth = x[b, (1 - hf) * OWN:(2 - hf) * OWN]
        halo = np.zeros((2 * HALO, D), np.float32)
        mask = np.zeros((128, 2 * HALO), np.float32)
        if hf == 1:
            halo[:HALO] = x[b, OWN - HALO:OWN]
            mask[:, :HALO] = 1.0
        else:
            halo[HALO:] = x[b, OWN:OWN + HALO]
            mask[:, HALO:] = 1.0
        C, S = _rope_tables(hf)
        cv = np.ascontiguousarray(np.stack([c[b].reshape(KC, 128).T, c_ctx.reshape(KC, 128).T], axis=2))
        m = dict(shared)
        m.update(x_own=np.ascontiguousarray(own), x_oth=np.ascontiguousarray(oth), ctx_b=np.ascontiguousarray(ctx[b]),
                 x_halo=halo, halo_mask=mask, cvec=cv, rope_c=C, rope_s=S)
        in_maps.append(m)
    key = (_debug, _stop_after)
    if key not in _NC_CACHE:
        _NC_CACHE[key] = build_nc(debug=_debug, stop_after=_stop_after)
    nc = _NC_CACHE[key]
    res = run_bass_kernel_spmd(nc, in_maps, core_ids=list(range(8)))
    if _debug:
        return res
    outp = np.empty((4, SEQ, D), np.float32)
    for core in range(8):
        b, hf = core // 2, core % 2
        outp[b, hf * OWN:(hf + 1) * OWN] = np.asarray(res.results[core]["out"], dtype=np.float32)
    return outp
```
